# Optimizing a Trainium2 kernel written in Bass

```python
import math
import jax
import jax.numpy as jnp
from jax import lax
import numpy as np

D_MODEL = 4096
BATCH = 2
SEQ = 8192
DEPTH = 1

BRANCH_WIDTH = D_MODEL // 2
N_BRANCHES = 3
ATTN_HEADS = 16
ATTN_HEAD_DIM = BRANCH_WIDTH // ATTN_HEADS
MOBA_BLOCK = 256
MOBA_TOP_K = 3
Q_CHUNK = 32
SSM_GROUP = 16
SSM_GROUPS = BRANCH_WIDTH // SSM_GROUP
SSM_STATE = 64
DT_MIN = 1e-3
DT_MAX = 1e-1
MEM_LEN = 256
MEM_HEADS = 4
MEM_HEAD_DIM = BRANCH_WIDTH // MEM_HEADS

EPS = 1e-6
NEG = -1e30

kernel_name = 'hybrid_moba_s5_memory_gated_block'


def _rms_norm(x, gain):
    xf = x.astype(jnp.float32)
    y = xf * lax.rsqrt(jnp.mean(xf * xf, axis=-1, keepdims=True) + EPS)
    return (y * gain.astype(jnp.float32)).astype(x.dtype)


def _alibi_slopes(n_heads):
    return jnp.asarray([2.0 ** (-8.0 * (i + 1) / n_heads) for i in range(n_heads)], dtype=jnp.float32)


def _moba_attention(q, k, v):
    bsz, n_heads, seq, hd = q.shape
    n_blocks = -(-seq // MOBA_BLOCK)
    pad = n_blocks * MOBA_BLOCK - seq
    k_pad = jnp.pad(k, ((0, 0), (0, 0), (0, pad), (0, 0)))
    v_pad = jnp.pad(v, ((0, 0), (0, 0), (0, pad), (0, 0)))
    k_blk = k_pad.reshape(bsz, n_heads, n_blocks, MOBA_BLOCK, hd)
    v_blk = v_pad.reshape(bsz, n_heads, n_blocks, MOBA_BLOCK, hd)
    k_mean = jnp.mean(k_blk.astype(jnp.float32), axis=3)
    n_sel = min(MOBA_TOP_K, n_blocks)
    n_g = n_sel * MOBA_BLOCK
    slopes = _alibi_slopes(n_heads)[None, :, None, None]
    scale = hd ** -0.5
    b_idx = jnp.arange(bsz)[:, None, None, None]
    h_idx = jnp.arange(n_heads)[None, :, None, None]
    blk_ids = jnp.arange(n_blocks)
    key_off = jnp.arange(MOBA_BLOCK)

    def one_chunk(c):
        start = c * Q_CHUNK
        own = start // MOBA_BLOCK
        q_c = lax.dynamic_slice_in_dim(q, start, Q_CHUNK, axis=2)
        q_pos = start + jnp.arange(Q_CHUNK)
        gate = jnp.einsum('bhqd,bhnd->bhqn', q_c.astype(jnp.float32), k_mean)
        gate = jnp.where(blk_ids < own, gate, NEG)
        _, sel = lax.top_k(gate, n_sel)
        valid = sel < own
        k_sel = k_blk[b_idx, h_idx, sel]
        v_sel = v_blk[b_idx, h_idx, sel]
        s_sel = jnp.einsum('bhqd,bhqnkd->bhqnk', q_c, k_sel).astype(jnp.float32) * scale
        dist_sel = (q_pos[None, None, :, None, None] - (sel[..., None] * MOBA_BLOCK + key_off)).astype(jnp.float32)
        s_sel = jnp.where(valid[..., None], s_sel - slopes[..., None] * dist_sel, NEG)
        s_sel = s_sel.reshape(bsz, n_heads, Q_CHUNK, n_g)
        k_own = lax.dynamic_slice_in_dim(k_pad, own * MOBA_BLOCK, MOBA_BLOCK, axis=2)
        v_own = lax.dynamic_slice_in_dim(v_pad, own * MOBA_BLOCK, MOBA_BLOCK, axis=2)
        s_own = jnp.einsum('bhqd,bhkd->bhqk', q_c, k_own).astype(jnp.float32) * scale
        dist_own = q_pos[:, None] - (own * MOBA_BLOCK + key_off)[None, :]
        s_own = jnp.where(dist_own >= 0, s_own - slopes * dist_own.astype(jnp.float32), NEG)
        p = jax.nn.softmax(jnp.concatenate([s_sel, s_own], axis=-1), axis=-1).astype(v.dtype)
        out = jnp.einsum('bhqk,bhqkd->bhqd', p[..., :n_g], v_sel.reshape(bsz, n_heads, Q_CHUNK, n_g, hd))
        out = out + jnp.einsum('bhqk,bhkd->bhqd', p[..., n_g:], v_own)
        return out

    chunks = lax.map(one_chunk, jnp.arange(seq // Q_CHUNK))
    return chunks.transpose(1, 2, 0, 3, 4).reshape(bsz, n_heads, seq, hd)


def _cplx_combine(e1, e2):
    a1r, a1i, b1r, b1i = e1
    a2r, a2i, b2r, b2i = e2
    return (a2r * a1r - a2i * a1i,
            a2r * a1i + a2i * a1r,
            a2r * b1r - a2i * b1i + b2r,
            a2r * b1i + a2i * b1r + b2i)


def _s5_branch(u, a_re, a_im, log_dt, b_re, b_im, c_re, c_im, d_skip):
    bsz, seq, width = u.shape
    ug = u.astype(jnp.float32).reshape(bsz, seq, SSM_GROUPS, SSM_GROUP)
    dt = jnp.exp(log_dt.astype(jnp.float32))[:, None]
    ar = a_re.astype(jnp.float32)
    ai = a_im.astype(jnp.float32)
    mag = jnp.exp(dt * ar)
    abar_re = mag * jnp.cos(dt * ai)
    abar_im = mag * jnp.sin(dt * ai)
    den = ar * ar + ai * ai
    nr = abar_re - 1.0
    f_re = (nr * ar + abar_im * ai) / den
    f_im = (abar_im * ar - nr * ai) / den
    br = b_re.astype(jnp.float32)
    bi = b_im.astype(jnp.float32)
    bbar_re = f_re[..., None] * br - f_im[..., None] * bi
    bbar_im = f_re[..., None] * bi + f_im[..., None] * br
    cr = c_re.astype(jnp.float32)
    ci = c_im.astype(jnp.float32)
    d = d_skip.astype(jnp.float32).reshape(SSM_GROUPS, SSM_GROUP)

    def per_example(u_e):
        bu_re = jnp.einsum('sgc,gnc->sgn', u_e, bbar_re)
        bu_im = jnp.einsum('sgc,gnc->sgn', u_e, bbar_im)
        a_r = jnp.broadcast_to(abar_re, bu_re.shape)
        a_i = jnp.broadcast_to(abar_im, bu_im.shape)
        _, _, x_re, x_im = lax.associative_scan(_cplx_combine, (a_r, a_i, bu_re, bu_im), axis=0)
        return (jnp.einsum('sgn,gcn->sgc', x_re, cr)
                - jnp.einsum('sgn,gcn->sgc', x_im, ci)
                + d * u_e)

    y = lax.map(per_example, ug)
    return y.reshape(bsz, seq, width).astype(u.dtype)


def _memory_attention(q, k, v):
    scale = q.shape[-1] ** -0.5
    s = jnp.einsum('bqhd,bkhd->bhqk', q, k).astype(jnp.float32) * scale
    p = jax.nn.softmax(s, axis=-1).astype(v.dtype)
    return jnp.einsum('bhqk,bkhd->bqhd', p, v)


def setup_inputs(seed: int = 0) -> dict:
    key = jax.random.key(seed)
    ks = jax.random.split(key, 24)
    W = BRANCH_WIDTH
    n_in = 8 * W + N_BRANCHES * D_MODEL

    def nrm(k, shape, scale):
        return jax.random.normal(k, shape, jnp.float32) * scale

    n_idx = jnp.arange(SSM_STATE, dtype=jnp.float32)
    return {
        'x': nrm(ks[0], (BATCH, SEQ, D_MODEL), 1.0),
        'mem': nrm(ks[1], (BATCH, MEM_LEN, D_MODEL), 1.0),
        'w_in': nrm(ks[2], (D_MODEL, n_in), D_MODEL ** -0.5),
        'g_norm': 1.0 + nrm(ks[3], (D_MODEL,), 0.02),
        'g_mem': 1.0 + nrm(ks[4], (D_MODEL,), 0.02),
        'w_mem_kv': nrm(ks[5], (D_MODEL, 2 * W), D_MODEL ** -0.5),
        'q_gain_a': 1.0 + nrm(ks[6], (ATTN_HEAD_DIM,), 0.02),
        'k_gain_a': 1.0 + nrm(ks[7], (ATTN_HEAD_DIM,), 0.02),
        'q_gain_c': 1.0 + nrm(ks[8], (MEM_HEAD_DIM,), 0.02),
        'k_gain_c': 1.0 + nrm(ks[9], (MEM_HEAD_DIM,), 0.02),
        'ssm_a_re': -0.5 + nrm(ks[10], (SSM_GROUPS, SSM_STATE), 0.01),
        'ssm_a_im': math.pi * n_idx[None, :] + nrm(ks[11], (SSM_GROUPS, SSM_STATE), 0.01),
        'ssm_log_dt': jax.random.uniform(ks[12], (SSM_GROUPS,), jnp.float32, math.log(DT_MIN), math.log(DT_MAX)),
        'ssm_b_re': nrm(ks[13], (SSM_GROUPS, SSM_STATE, SSM_GROUP), (2 * SSM_GROUP) ** -0.5),
        'ssm_b_im': nrm(ks[14], (SSM_GROUPS, SSM_STATE, SSM_GROUP), (2 * SSM_GROUP) ** -0.5),
        'ssm_c_re': nrm(ks[15], (SSM_GROUPS, SSM_GROUP, SSM_STATE), SSM_STATE ** -0.5),
        'ssm_c_im': nrm(ks[16], (SSM_GROUPS, SSM_GROUP, SSM_STATE), SSM_STATE ** -0.5),
        'ssm_d': nrm(ks[17], (W,), 1.0),
        'w_glu': nrm(ks[18], (W, W), W ** -0.5),
        'b_glu': nrm(ks[19], (W,), 0.01),
        'w_br_a': nrm(ks[20], (W, D_MODEL), W ** -0.5),
        'w_br_s': nrm(ks[21], (W, D_MODEL), W ** -0.5),
        'w_br_c': nrm(ks[22], (W, D_MODEL), W ** -0.5),
        'w_out': nrm(ks[23], (D_MODEL, D_MODEL), D_MODEL ** -0.5),
    }


def reference(x, mem, w_in, g_norm, g_mem, w_mem_kv, q_gain_a, k_gain_a, q_gain_c, k_gain_c,
              ssm_a_re, ssm_a_im, ssm_log_dt, ssm_b_re, ssm_b_im, ssm_c_re, ssm_c_im, ssm_d,
              w_glu, b_glu, w_br_a, w_br_s, w_br_c, w_out):
    bsz, seq, _ = x.shape
    W = BRANCH_WIDTH
    for _layer in range(DEPTH):
        h = _rms_norm(x, g_norm)
        proj = h @ w_in
        splits = [W * i for i in range(1, 9)] + [8 * W + D_MODEL, 8 * W + 2 * D_MODEL]
        q_a, k_a, v_a, z_a, u_s, z_s, q_c, z_c, g_a, g_s, g_c = jnp.split(proj, splits, axis=-1)

        qa = _rms_norm(q_a.reshape(bsz, seq, ATTN_HEADS, ATTN_HEAD_DIM), q_gain_a).transpose(0, 2, 1, 3)
        ka = _rms_norm(k_a.reshape(bsz, seq, ATTN_HEADS, ATTN_HEAD_DIM), k_gain_a).transpose(0, 2, 1, 3)
        va = v_a.reshape(bsz, seq, ATTN_HEADS, ATTN_HEAD_DIM).transpose(0, 2, 1, 3)
        o_a = _moba_attention(qa, ka, va).transpose(0, 2, 1, 3).reshape(bsz, seq, W)
        y_a = o_a * jax.nn.silu(z_a)

        y_s = _s5_branch(u_s, ssm_a_re, ssm_a_im, ssm_log_dt, ssm_b_re, ssm_b_im, ssm_c_re, ssm_c_im, ssm_d)
        y_s = jax.nn.gelu(y_s)
        y_s = y_s * jax.nn.sigmoid(y_s @ w_glu + b_glu)
        y_s = y_s * jax.nn.silu(z_s)

        m = _rms_norm(mem, g_mem)
        k_m, v_m = jnp.split(m @ w_mem_kv, 2, axis=-1)
        n_mem = mem.shape[1]
        qc = _rms_norm(q_c.reshape(bsz, seq, MEM_HEADS, MEM_HEAD_DIM), q_gain_c)
        kc = _rms_norm(k_m.reshape(bsz, n_mem, MEM_HEADS, MEM_HEAD_DIM), k_gain_c)
        vc = v_m.reshape(bsz, n_mem, MEM_HEADS, MEM_HEAD_DIM)
        o_c = _memory_attention(qc, kc, vc).reshape(bsz, seq, W)
        y_c = o_c * jax.nn.silu(z_c)

        merged = (jax.nn.sigmoid(g_a) * (y_a @ w_br_a)
                  + jax.nn.sigmoid(g_s) * (y_s @ w_br_s)
                  + jax.nn.sigmoid(g_c) * (y_c @ w_br_c))
        x = x + merged @ w_out
    return x
```

```python
import math
import os
import numpy as np
from contextlib import ExitStack
import concourse.bass as bass
import concourse.mybir as mybir
from concourse.bass_utils import run_bass_kernel_spmd

F32 = mybir.dt.float32
BF16 = mybir.dt.bfloat16
AF = mybir.ActivationFunctionType
ALU = mybir.AluOpType
AX = mybir.AxisListType

D = 4096
W = 2048
NIN = 28672
OWN = 2048
TT = 512
NTT = OWN // TT
EPS = 1e-6
MEM = 256

DBG = int(os.environ.get("K_DBG", "0"))
ENGS = ("tensor", "vector", "scalar", "gpsimd", "sync")

ENABLE_A = True
ENABLE_S = True
S_USE_POOL = False
CTX = 8192
OFF = CTX - OWN
NCT = CTX // TT
FT = 256
SLOPES = [2.0 ** (-8.0 * (i + 1) / 16) for i in range(16)]


class Buf:
    __slots__ = ("name", "w", "r", "dsem", "dcount")

    def __init__(self, name=""):
        self.name = name
        self.w = None
        self.r = {}
        self.dsem = None
        self.dcount = 0


class Trk:
    def __init__(self, nc, stack):
        self.nc = nc
        self.stack = stack
        self.ops = {e: [] for e in ENGS}
        self.cnt = {e: 0 for e in ENGS}
        self.sem = {e: stack.enter_context(nc.semaphore(f"s_{e}")) for e in ENGS}
        self.seen = {e: {} for e in ENGS}
        self.dsems = []

    def _dsem(self, buf):
        if buf.dsem is None:
            buf.dsem = self.stack.enter_context(self.nc.semaphore(f"d{len(self.dsems)}"))
            self.dsems.append(buf)
        return buf.dsem

    def _need(self, eng, dep, waits):
        if dep is None:
            return
        kind, key, count = dep
        k = (kind, id(key) if kind == 'd' else key)
        if self.seen[eng].get(k, 0) >= count:
            return
        self.seen[eng][k] = count
        waits.append((self.sem[key] if kind == 'e' else key, count))

    def _deps(self, eng, reads, writes):
        waits = []
        for b in reads:
            self._need(eng, b.w, waits)
        for b in writes:
            self._need(eng, b.w, waits)
            for d in b.r.values():
                self._need(eng, d, waits)
        return waits

    def op(self, eng, fn, *args, reads=(), writes=(), inc=True, **kw):
        waits = self._deps(eng, reads, writes)
        if inc:
            self.cnt[eng] += 1
            c = self.cnt[eng]
        else:
            c = self.cnt[eng] + 1
        me = ('e', eng, c)
        for b in reads:
            b.r[eng] = me
        if inc:
            for b in writes:
                b.w = me
                b.r = {}
        self.ops[eng].append((fn, args, kw, waits, (self.sem[eng], 1) if inc else None))

    def dma(self, eng, out, in_, reads=(), writes=()):
        waits = self._deps(eng, reads, writes)
        anchor = writes[0] if writes else reads[0]
        sem = self._dsem(anchor)
        anchor.dcount += 16
        me = ('d', sem, anchor.dcount)
        for b in reads:
            b.r[('d', id(sem))] = me
        for b in writes:
            b.w = me
            b.r = {}
        fn = getattr(self.nc, eng).dma_start
        self.ops[eng].append((fn, (), dict(out=out, in_=in_), waits, (sem, 16)))

    def wait_all(self, eng, bufs):
        waits = []
        for b in bufs:
            self._need(eng, b.w, waits)
            for d in b.r.values():
                self._need(eng, d, waits)
        self.ops[eng].append((None, (), {}, waits, None))

    def barrier(self):
        snap = [('e', e, self.cnt[e]) for e in ENGS if self.cnt[e] > 0]
        snap += [('d', b.dsem, b.dcount) for b in self.dsems if b.dcount > 0]
        for e in ENGS:
            waits = []
            for d in snap:
                if d[0] == 'e' and d[1] == e:
                    continue
                self._need(e, d, waits)
            self.ops[e].append((None, (), {}, waits, None))

    def replay(self, block):
        for e in ENGS:
            ops = self.ops[e]
            if not ops:
                continue

            def body(engine, ops=ops):
                for fn, args, kw, waits, inc in ops:
                    for s, v in waits:
                        engine.wait_ge(s, v)
                    if fn is not None:
                        ins = fn(*args, **kw)
                        if inc is not None:
                            ins.then_inc(inc[0], inc[1])
            getattr(block, e)(body)


DRAM_NAMES = []


def build_nc():
    nc = bass.Bass("TRN2", target_bir_lowering=False)

    def din(name, shape, dt=F32):
        DRAM_NAMES.append(name)
        return nc.dram_tensor(name, list(shape), dt, kind="ExternalInput").ap()

    def dscr(name, shape, dt):
        DRAM_NAMES.append(name)
        return nc.dram_tensor(name, list(shape), dt, kind="Internal").ap()

    x = din("x", [OWN, D])
    mem = din("mem", [MEM, D])
    w_in = din("w_in", [D, NIN])
    w_kv = din("w_mem_kv", [D, 2 * W])
    w_br = [din(n, [W, D]) for n in ("w_br_a", "w_br_s", "w_br_c")]
    w_out = din("w_out", [D, D])
    gnB = din("gnB", [128, D])
    gmB = din("gmB", [128, D])
    qgc = din("qgc", [128, 4])
    kgc = din("kgc", [128, 4])
    xprev = din("xprev", [OFF, D])
    vbB = din("vbB", [128, 32])
    qga = din("qga", [128, 1])
    kga = din("kga", [128, 1])
    s_arep = din("s_arep", [128, 8192]); s_irep = din("s_irep", [128, 8192]); s_lrep = din("s_lrep", [128, 8192])
    s_acol = din("s_acol", [128, 64]); s_icol = din("s_icol", [128, 64]); s_lcol = din("s_lcol", [128, 64])
    s_bre = din("s_bre", [128, 8192]); s_bim = din("s_bim", [128, 8192])
    s_cre = din("s_cre", [128, 8192]); s_cim = din("s_cim", [128, 8192])
    s_dcol = din("s_dcol", [128, 16]); s_bglu = din("s_bglu", [128, 16])
    w_glu = din("w_glu", [W, W])
    out = nc.dram_tensor("out", [OWN, D], F32, kind="ExternalOutput").ap()

    hTd = dscr("hTd", [32, 128, CTX], BF16)
    kTd = dscr("kTd", [16, 128, CTX], BF16)
    qTd = dscr("qTd", [16, 128, OWN], BF16)
    Vd = dscr("Vd", [CTX, W], BF16)
    uTd = dscr("uTd", [16, 128, CTX], BF16)
    y1d = dscr("y1d", [16, 128, OWN], F32)
    y1bd = dscr("y1bd", [16, 128, OWN], BF16)
    qcTd = dscr("qcTd", [16, 128, OWN], BF16)
    szd = [dscr(f"szd{i}", [16, 128, OWN], F32) for i in range(3)]
    sgd = [dscr(f"sgd{i}", [32, 128, OWN], F32) for i in range(3)]
    yTd = [dscr(f"yTd{i}", [16, 128, OWN], BF16) for i in range(3)]

    with ExitStack() as st:
        t = Trk(nc, st)
        sb = lambda name, shape, dt, stk: stk.enter_context(nc.sbuf_tensor(name, list(shape), dt))
        ps = lambda name, shape, dt, stk: stk.enter_context(nc.psum_tensor(name, list(shape), dt))

        identf = sb("identf", [128, 128], F32, st)
        ident = sb("ident", [128, 128], BF16, st)
        onesb = sb("onesb", [128, 128], BF16, st)
        epst = sb("epst", [128, 1], F32, st)
        qgct = sb("qgct", [128, 4], F32, st)
        kgct = sb("kgct", [128, 4], F32, st)
        KcT = sb("KcT", [128, 16, MEM], BF16, st)
        Vc = sb("Vc", [128, 2, W], BF16, st)
        qgat = sb("qgat", [128, 1], F32, st)
        kgat = sb("kgat", [128, 1], F32, st)
        kmT = sb("kmT", [128, 16, 32], F32, st)
        b_kmT = Buf("kmT")
        b_const = Buf("const")
        b_KcT = Buf("KcT")
        b_Vc = Buf("Vc")
        t.op("gpsimd", nc.gpsimd.memset, identf[:], 0.0, writes=[b_const])
        t.op("gpsimd", nc.gpsimd.affine_select, out=identf[:], in_=identf[:], pattern=[[-1, 128]],
             compare_op=ALU.not_equal, fill=1.0, base=0, channel_multiplier=1,
             reads=[b_const], writes=[b_const])
        t.op("vector", nc.vector.tensor_copy, ident[:], identf[:], reads=[b_const], writes=[b_const])
        t.op("vector", nc.vector.memset, onesb[:], 1.0, reads=[b_const], writes=[b_const])
        t.op("vector", nc.vector.memset, epst[:], EPS, reads=[b_const], writes=[b_const])
        if DBG:
            t.op("vector", nc.vector.memset, kmT[:], 0.0, writes=[b_kmT])
        b_g = Buf("gains")
        t.dma("sync", qgct[:], qgc[:, :], writes=[b_g])
        t.dma("sync", kgct[:], kgc[:, :], writes=[b_g])
        t.dma("sync", qgat[:], qga[:, :], writes=[b_g])
        t.dma("sync", kgat[:], kga[:, :], writes=[b_g])
        t.op("vector", nc.vector.tensor_scalar, out=qgat[:], in0=qgat[:], scalar1=float(128 ** -0.5),
             scalar2=None, op0=ALU.mult, reads=[b_g], writes=[b_g])
        t.op("vector", nc.vector.tensor_scalar, out=qgct[:], in0=qgct[:], scalar1=float(512 ** -0.5),
             scalar2=None, op0=ALU.mult, reads=[b_g], writes=[b_g])

        wbuf = [None, None]
        b_w = [Buf(f"w{i}") for i in range(2)]
        wctr = [0]
        wgen = [0]

        def mk_w(stk):
            wgen[0] += 1
            for i in range(2):
                wbuf[i] = sb(f"wbuf{wgen[0]}_{i}", [128, 32, 512], BF16, stk)

        def load_w(src, nkt, c0, ncols=512):
            i = wctr[0] % 2
            wctr[0] += 1
            v = src.rearrange("(kt p) c -> p kt c", p=128)
            half = nkt // 2
            t.dma("gpsimd", wbuf[i][:, 0:half, 0:ncols], v[:, 0:half, c0:c0 + ncols], writes=[b_w[i]])
            t.dma("gpsimd", wbuf[i][:, half:nkt, 0:ncols], v[:, half:nkt, c0:c0 + ncols], writes=[b_w[i]])
            return wbuf[i], b_w[i]

        b_hTd = Buf("hTd")
        with ExitStack() as p0:
            hT = [sb(f"p0hT{i}", [128, 32, TT], BF16, p0) for i in range(2)]
            b_hT = [Buf(), Buf()]
            gB = sb("p0gB", [128, D], F32, p0)
            xt = [sb(f"p0xt{i}", [128, D], F32, p0) for i in range(2)]
            hb = sb("p0hb", [128, D], BF16, p0)
            stat = sb("p0stat", [128, 4], F32, p0)
            tp = [ps(f"p0tp{i}", [128, 1024], BF16, p0) for i in range(4)]
            b_gB, b_hb, b_junk, b_stat = Buf(), Buf(), Buf(), Buf()
            b_xt = [Buf(), Buf()]
            b_tp = [Buf() for _ in range(4)]
            t.dma("sync", gB[:], gnB[:, :], writes=[b_gB])
            for i in range(CTX // 128):
                tt, s = divmod(i, 4)
                xi, bx = xt[i % 2], b_xt[i % 2]
                src = xprev[i * 128:(i + 1) * 128, :] if i < OFF // 128 else x[i * 128 - OFF:(i + 1) * 128 - OFF, :]
                t.dma("sync", xi[:, 0:D // 2], src[:, 0:D // 2], writes=[bx])
                t.dma("sync", xi[:, D // 2:D], src[:, D // 2:D], writes=[bx])
                t.op("scalar", nc.scalar.activation, out=hb[:], in_=xi[:], func=AF.Square,
                     accum_out=stat[:, 0:1], reads=[bx], writes=[b_hb, b_stat])
                t.op("scalar", nc.scalar.activation, out=stat[:, 1:2], in_=stat[:, 0:1], func=AF.Sqrt,
                     bias=epst[:, 0:1], scale=1.0 / D, reads=[b_stat, b_const], writes=[b_stat])
                t.op("vector", nc.vector.reciprocal, stat[:, 2:3], stat[:, 1:2], reads=[b_stat], writes=[b_stat])
                t.op("vector", nc.vector.scalar_tensor_tensor, out=hb[:], in0=xi[:], scalar=stat[:, 2:3],
                     in1=gB[:], op0=ALU.mult, op1=ALU.mult, reads=[bx, b_stat, b_gB], writes=[b_hb])
                for q in range(4):
                    for k in range(8):
                        kt = q * 8 + k
                        t.op("tensor", nc.tensor.transpose, tp[q][:, k * 128:(k + 1) * 128],
                             hb[:, kt * 128:(kt + 1) * 128], ident[:],
                             reads=[b_hb, b_const], writes=[b_tp[q]], inc=(k == 7))
                    dst = hT[tt % 2][:, q * 8:(q + 1) * 8, s * 128:(s + 1) * 128]
                    srcp = tp[q][:].rearrange("p (k c) -> p k c", k=8)
                    if q % 2 == 0:
                        t.op("vector", nc.vector.tensor_copy, dst, srcp, reads=[b_tp[q]], writes=[b_hT[tt % 2]])
                    else:
                        t.op("scalar", nc.scalar.copy, dst, srcp, reads=[b_tp[q]], writes=[b_hT[tt % 2]])
                if s == 3:
                    t.dma("sync", hTd[:, :, tt * TT:(tt + 1) * TT].rearrange("kt p t -> p kt t"),
                          hT[tt % 2][:], reads=[b_hT[tt % 2]], writes=[b_hTd])
            t.barrier()

        with ExitStack() as pc:
            mT = sb("pcmT", [128, 32, MEM], BF16, pc)
            b_mT = Buf("mT")
            pca = pc.enter_context(ExitStack())
            gB = sb("pcgB", [128, D], F32, pca)
            xt = [sb(f"pcxt{i}", [128, D], F32, pca) for i in range(2)]
            hb = sb("pchb", [128, D], BF16, pca)
            junk = sb("pcjunk", [128, D], BF16, pca)
            stat = sb("pcstat", [128, 4], F32, pca)
            tp = [ps(f"pctp{i}", [128, 1024], BF16, pca) for i in range(4)]
            b_gB, b_hb, b_junk, b_stat = Buf(), Buf(), Buf(), Buf()
            b_xt = [Buf(), Buf()]
            b_tp = [Buf() for _ in range(4)]
            t.dma("sync", gB[:], gmB[:, :], writes=[b_gB])
            for i in range(MEM // 128):
                xi, bx = xt[i % 2], b_xt[i % 2]
                src = mem[i * 128:(i + 1) * 128, :]
                t.dma("sync", xi[:], src, writes=[bx])
                t.op("scalar", nc.scalar.activation, out=junk[:], in_=xi[:], func=AF.Square,
                     accum_out=stat[:, 0:1], reads=[bx], writes=[b_junk, b_stat])
                t.op("scalar", nc.scalar.activation, out=stat[:, 1:2], in_=stat[:, 0:1], func=AF.Sqrt,
                     bias=epst[:, 0:1], scale=1.0 / D, reads=[b_stat, b_const], writes=[b_stat])
                t.op("vector", nc.vector.reciprocal, stat[:, 2:3], stat[:, 1:2], reads=[b_stat], writes=[b_stat])
                t.op("vector", nc.vector.scalar_tensor_tensor, out=hb[:], in0=xi[:], scalar=stat[:, 2:3],
                     in1=gB[:], op0=ALU.mult, op1=ALU.mult, reads=[bx, b_stat, b_gB], writes=[b_hb])
                for q in range(4):
                    for k in range(8):
                        kt = q * 8 + k
                        t.op("tensor", nc.tensor.transpose, tp[q][:, k * 128:(k + 1) * 128],
                             hb[:, kt * 128:(kt + 1) * 128], ident[:],
                             reads=[b_hb, b_const], writes=[b_tp[q]], inc=(k == 7))
                    dst = mT[:, q * 8:(q + 1) * 8, i * 128:(i + 1) * 128]
                    srcp = tp[q][:].rearrange("p (k c) -> p k c", k=8)
                    t.op("vector", nc.vector.tensor_copy, dst, srcp, reads=[b_tp[q]], writes=[b_mT])
            t.barrier()
            pca.close()
            mk_w(pc)
            kps = [ps(f"pckps{i}", [128, 512], F32, pc) for i in range(4)]
            ssp = ps("pcssp", [128, 512], F32, pc)
            vps = [ps(f"pcvps{i}", [128, 512], F32, pc) for i in range(2)]
            sq = sb("pcsq", [128, 4, MEM], BF16, pc)
            rstd = sb("pcrstd", [128, MEM], F32, pc)
            b_kps = [Buf() for _ in range(4)]
            b_ssp, b_sq, b_rstd = Buf(), Buf(), Buf()
            b_vps = [Buf(), Buf()]
            for hd in range(4):
                wt, bw = load_w(w_kv, 32, hd * 512)
                for j in range(4):
                    for kt in range(32):
                        t.op("tensor", nc.tensor.matmul, kps[j][:, 0:MEM], lhsT=wt[:, kt, j * 128:(j + 1) * 128],
                             rhs=mT[:, kt, :], start=(kt == 0), stop=(kt == 31),
                             reads=[bw, b_mT], writes=[b_kps[j]], inc=(kt == 31))
                    t.op("scalar", nc.scalar.activation, out=sq[:, j, :], in_=kps[j][:, 0:MEM], func=AF.Square,
                         reads=[b_kps[j]], writes=[b_sq])
                for j in range(4):
                    t.op("tensor", nc.tensor.matmul, ssp[:, 0:MEM], lhsT=onesb[:], rhs=sq[:, j, :],
                         start=(j == 0), stop=(j == 3), reads=[b_sq, b_const], writes=[b_ssp], inc=(j == 3))
                t.op("scalar", nc.scalar.activation, out=rstd[:], in_=ssp[:, 0:MEM], func=AF.Sqrt,
                     bias=epst[:, 0:1], scale=1.0 / 512, reads=[b_ssp, b_const], writes=[b_rstd])
                t.op("vector", nc.vector.reciprocal, rstd[:], rstd[:], reads=[b_rstd], writes=[b_rstd])
                for j in range(4):
                    t.op("vector", nc.vector.scalar_tensor_tensor, out=KcT[:, hd * 4 + j, :], in0=kps[j][:, 0:MEM],
                         scalar=kgct[:, j:j + 1], in1=rstd[:], op0=ALU.mult, op1=ALU.mult,
                         reads=[b_kps[j], b_g, b_rstd], writes=[b_KcT])
            for cb in range(4):
                wt, bw = load_w(w_kv, 32, W + cb * 512)
                for mt in range(2):
                    for kt in range(32):
                        t.op("tensor", nc.tensor.matmul, vps[mt][:], lhsT=mT[:, kt, mt * 128:(mt + 1) * 128],
                             rhs=wt[:, kt, :], start=(kt == 0), stop=(kt == 31),
                             reads=[bw, b_mT], writes=[b_vps[mt]], inc=(kt == 31))
                    t.op("vector", nc.vector.tensor_copy, Vc[:, mt, cb * 512:(cb + 1) * 512], vps[mt][:],
                         reads=[b_vps[mt]], writes=[b_Vc])
            t.barrier()

        b_qcTd = Buf("qcTd")
        b_szd = [Buf() for _ in range(3)]
        b_sgd = [Buf() for _ in range(3)]
        with ExitStack() as p1:
            mk_w(p1)
            hT = [sb(f"p1hT{i}", [128, 32, TT], BF16, p1) for i in range(2)]
            b_hT = [Buf(), Buf()]
            acc = [ps(f"p1acc{i}", [128, 512], F32, p1) for i in range(4)]
            b_acc = [Buf() for _ in range(4)]
            ssp = ps("p1ssp", [128, 512], F32, p1)
            b_ssp = Buf()
            sq = sb("p1sq", [128, 4, TT], BF16, p1)
            rstd = sb("p1rstd", [128, TT], F32, p1)
            b_sq, b_rstd = Buf(), Buf()
            ob32 = [sb(f"p1ob32_{i}", [128, TT], F32, p1) for i in range(2)]
            ob16 = [sb(f"p1ob16_{i}", [128, 4, TT], BF16, p1) for i in range(2)]
            b_ob32 = [Buf(), Buf()]
            b_ob16 = [Buf(), Buf()]
            hctr = [0]
            octr = [0]

            def load_hT(tt):
                i = hctr[0] % 2
                hctr[0] += 1
                src = hTd[:, :, tt * TT:(tt + 1) * TT].rearrange("kt p t -> p kt t")
                t.dma("sync", hT[i][:, 0:16, :], src[:, 0:16, :], reads=[b_hTd], writes=[b_hT[i]])
                t.dma("sync", hT[i][:, 16:32, :], src[:, 16:32, :], reads=[b_hTd], writes=[b_hT[i]])
                return hT[i], b_hT[i]

            def proj_fm(wt, bw, j, ht, bh, a):
                for kt in range(32):
                    t.op("tensor", nc.tensor.matmul, acc[a][:], lhsT=wt[:, kt, j * 128:(j + 1) * 128],
                         rhs=ht[:, kt, :], start=(kt == 0), stop=(kt == 31),
                         reads=[bw, bh], writes=[b_acc[a]], inc=(kt == 31))

            def act_blocks():
                lst = []
                for bi, base in ((0, 12), (1, 20), (2, 28)):
                    if (bi == 0 and not ENABLE_A) or (bi == 1 and not ENABLE_S):
                        continue
                    for k in range(4):
                        lst.append(("silu", szd[bi], b_szd[bi], base + k, k))
                for bi, base in ((0, 32), (1, 40), (2, 48)):
                    if (bi == 0 and not ENABLE_A) or (bi == 1 and not ENABLE_S):
                        continue
                    for k in range(8):
                        lst.append(("sigm", sgd[bi], b_sgd[bi], base + k, k))
                return lst

            b_kTd, b_qTd, b_Vd, b_uTd = Buf("kTd"), Buf("qTd"), Buf("Vd"), Buf("uTd")
            OT0 = NCT - NTT
            items = []
            for kind, dst, bdst, cb, k in act_blocks():
                for tt in range(NTT):
                    items.append((cb, OT0 + tt, ("act", kind, dst, bdst, k, tt)))
            for hd in range(4):
                for tt in range(NTT):
                    items.append((24 + hd, OT0 + tt, ("qc", hd, tt)))
            if ENABLE_A:
                for is_k in (True, False):
                    for cbl in range(4):
                        for tt in (range(NCT) if is_k else range(OT0, NCT)):
                            items.append(((4 if is_k else 0) + cbl, tt, ("qk", is_k, cbl, tt)))
                for cbl in range(4):
                    for tt in range(NCT):
                        items.append((8 + cbl, tt, ("v", cbl, tt)))
            if ENABLE_S:
                for cbl in range(4):
                    for tt in range(NCT):
                        items.append((16 + cbl, tt, ("u", cbl, tt)))

            if DBG:
                seen_k = set()
                cnt_k = {}
                keep = []
                for it in items:
                    kk = (it[2][0], it[2][1])
                    cnt_k[kk] = cnt_k.get(kk, 0) + 1
                    if cnt_k[kk] <= DBG:
                        keep.append(it)
                items = keep

            def nexta():
                a = octr[0] % 4
                octr[0] += 1
                return a

            def do_item(spec, wt, bw, ht, bh, tt):
                kind = spec[0]
                if kind == "act":
                    _, fn, dst, bdst, k, tq = spec
                    for j in range(4):
                        a = nexta()
                        o = a % 2
                        proj_fm(wt, bw, j, ht, bh, a)
                        t.op("scalar", nc.scalar.activation, out=ob32[o][:], in_=acc[a][:],
                             func=AF.Silu if fn == "silu" else AF.Sigmoid, reads=[b_acc[a]], writes=[b_ob32[o]])
                        t.dma("sync", dst[k * 4 + j, :, tq * TT:(tq + 1) * TT], ob32[o][:],
                              reads=[b_ob32[o]], writes=[bdst])
                elif kind == "qc":
                    _, hd, tq = spec
                    o = nexta() % 2
                    for j in range(4):
                        proj_fm(wt, bw, j, ht, bh, j)
                        t.op("scalar", nc.scalar.activation, out=sq[:, j, :], in_=acc[j][:], func=AF.Square,
                             reads=[b_acc[j]], writes=[b_sq])
                    for j in range(4):
                        t.op("tensor", nc.tensor.matmul, ssp[:], lhsT=onesb[:], rhs=sq[:, j, :],
                             start=(j == 0), stop=(j == 3), reads=[b_sq, b_const], writes=[b_ssp], inc=(j == 3))
                    t.op("scalar", nc.scalar.activation, out=rstd[:], in_=ssp[:], func=AF.Sqrt,
                         bias=epst[:, 0:1], scale=1.0 / 512, reads=[b_ssp, b_const], writes=[b_rstd])
                    t.op("vector", nc.vector.reciprocal, rstd[:], rstd[:], reads=[b_rstd], writes=[b_rstd])
                    for j in range(4):
                        t.op("vector", nc.vector.scalar_tensor_tensor, out=ob16[o][:, j, :], in0=acc[j][:],
                             scalar=qgct[:, j:j + 1], in1=rstd[:], op0=ALU.mult, op1=ALU.mult,
                             reads=[b_acc[j], b_g, b_rstd], writes=[b_ob16[o]])
                    t.dma("sync", qcTd[hd * 4:(hd + 1) * 4, :, tq * TT:(tq + 1) * TT].rearrange("j p t -> p j t"),
                          ob16[o][:], reads=[b_ob16[o]], writes=[b_qcTd])
                elif kind == "qk":
                    _, is_k, cbl, _tt = spec
                    banks = [nexta() for _ in range(4)]

                    def post(j):
                        a = banks[j]
                        o = a % 2
                        head = cbl * 4 + j
                        t.op("scalar", nc.scalar.activation, out=sq[:, j, :], in_=acc[a][:], func=AF.Square,
                             reads=[b_acc[a]], writes=[b_sqj[j]])
                        t.op("tensor", nc.tensor.matmul, ssp2[j % 2][:], lhsT=onesb[:], rhs=sq[:, j, :],
                             start=True, stop=True, reads=[b_sqj[j], b_const], writes=[b_ssp2[j % 2]])
                        t.op("scalar", nc.scalar.activation, out=rstd2[j % 2][:], in_=ssp2[j % 2][:], func=AF.Sqrt,
                             bias=epst[:, 0:1], scale=1.0 / 128, reads=[b_ssp2[j % 2], b_const], writes=[b_rstd2[j % 2]])
                        t.op("vector", nc.vector.reciprocal, rstd2[j % 2][:], rstd2[j % 2][:],
                             reads=[b_rstd2[j % 2]], writes=[b_rstd2[j % 2]])
                        t.op("vector", nc.vector.scalar_tensor_tensor, out=obq[a][:], in0=acc[a][:],
                             scalar=(kgat if is_k else qgat)[:, 0:1], in1=rstd2[j % 2][:], op0=ALU.mult, op1=ALU.mult,
                             reads=[b_acc[a], b_g, b_rstd2[j % 2]], writes=[b_obq[a]])
                        if is_k:
                            t.op("vector", nc.vector.tensor_reduce, out=kmT[:, head, tt * 2:(tt + 1) * 2],
                                 in_=obq[a][:].rearrange("p (b k) -> p b k", b=2), axis=AX.X, op=ALU.add,
                                 reads=[b_obq[a]], writes=[b_kmT])
                            t.dma("sync", kTd[head, :, tt * TT:(tt + 1) * TT], obq[a][:],
                                  reads=[b_obq[a]], writes=[b_kTd])
                        else:
                            tq = tt - OT0
                            t.dma("sync", qTd[head, :, tq * TT:(tq + 1) * TT], obq[a][:],
                                  reads=[b_obq[a]], writes=[b_qTd])
                    for step in range(5):
                        if step < 4:
                            proj_fm(wt, bw, step, ht, bh, banks[step])
                        if step >= 1:
                            post(step - 1)
                elif kind == "v":
                    _, cbl, _tt = spec
                    for s_ in range(4):
                        a = nexta()
                        for kt in range(32):
                            t.op("tensor", nc.tensor.matmul, acc[a][:], lhsT=ht[:, kt, s_ * 128:(s_ + 1) * 128],
                                 rhs=wt[:, kt, :], start=(kt == 0), stop=(kt == 31),
                                 reads=[bw, bh], writes=[b_acc[a]], inc=(kt == 31))
                        t.op("scalar", nc.scalar.copy, obq[a][:], acc[a][:], reads=[b_acc[a]], writes=[b_obq[a]])
                        r0 = tt * TT + s_ * 128
                        t.dma("sync", Vd[r0:r0 + 128, cbl * 512:(cbl + 1) * 512], obq[a][:],
                              reads=[b_obq[a]], writes=[b_Vd])
                elif kind == "u":
                    _, cbl, _tt = spec
                    for j in range(4):
                        a = nexta()
                        proj_fm(wt, bw, j, ht, bh, a)
                        t.op("scalar", nc.scalar.copy, obq[a][:], acc[a][:], reads=[b_acc[a]], writes=[b_obq[a]])
                        t.dma("sync", uTd[cbl * 4 + j, :, tt * TT:(tt + 1) * TT], obq[a][:],
                              reads=[b_obq[a]], writes=[b_uTd])

            obq = [sb(f"p1obq{i}", [128, TT], BF16, p1) for i in range(4)]
            b_obq = [Buf() for _ in range(4)]
            b_sqj = [Buf() for _ in range(4)]
            ssp2 = [ssp, ps("p1ssp2", [128, 512], F32, p1)]
            b_ssp2 = [b_ssp, Buf()]
            rstd2 = [rstd, sb("p1rstd2", [128, TT], F32, p1)]
            b_rstd2 = [b_rstd, Buf()]
            cur_cb = None
            nxt = load_hT(items[0][1])
            for idx, (cb, tt, spec) in enumerate(items):
                ht, bh = nxt
                if cb != cur_cb:
                    wt, bw = load_w(w_in, 32, cb * 512)
                    cur_cb = cb
                if idx + 1 < len(items):
                    nxt = load_hT(items[idx + 1][1])
                do_item(spec, wt, bw, ht, bh, tt)
            t.barrier()

        b_yTd = [Buf() for _ in range(3)]
        if ENABLE_A:
          with ExitStack() as pa:
            NB = CTX // 256
            QB0 = OFF // 256
            KT_ = [sb(f"paKT{i}", [128, CTX], BF16, pa) for i in range(2)]
            Vh = [sb(f"paVh{i}", [128, CTX // 128, 128], BF16, pa) for i in range(2)]
            QT = [sb(f"paQT{i}", [128, OWN], BF16, pa) for i in range(2)]
            b_KT, b_Vh, b_QT = [Buf(), Buf()], [Buf(), Buf()], [Buf(), Buf()]
            maskT = sb("pamaskT", [32, OWN], BF16, pa)
            Sel = sb("paSel", [32, 32, 128], BF16, pa)
            kbias = sb("pakbias", [128, 16, 64], F32, pa)
            Ftab = sb("paFtab", [128, 16, 256], F32, pa)
            Dtab = sb("paDtab", [128, 2, 256], F32, pa)
            cbias = sb("pacbias", [128, 8, 32], F32, pa)
            vbt = sb("pavbt", [128, 32], F32, pa)
            kmb = sb("pakmb", [128, 16, 32], BF16, pa)
            iot = sb("paiot", [128, 512], mybir.dt.int32, pa)
            iof = sb("paiof", [128, 512], F32, pa)
            tmpf = sb("patmpf", [128, 512], F32, pa)
            b_tab = Buf("tab")
            b_maskT = Buf("maskT")
            t.dma("sync", vbt[:], vbB[:, :], writes=[b_tab])
            t.op("gpsimd", nc.gpsimd.iota, iot[:, 0:64], pattern=[[-128, 64]], base=0, channel_multiplier=1,
                 reads=[b_tab], writes=[b_tab])
            t.op("vector", nc.vector.tensor_copy, iof[:, 0:64], iot[:, 0:64], reads=[b_tab], writes=[b_tab])
            for h in range(16):
                t.op("vector", nc.vector.tensor_scalar, out=kbias[:, h, :], in0=iof[:, 0:64], scalar1=float(SLOPES[h]),
                     scalar2=None, op0=ALU.mult, reads=[b_tab], writes=[b_tab])
            t.op("gpsimd", nc.gpsimd.iota, iot[:, 0:256], pattern=[[1, 256]], base=0, channel_multiplier=0,
                 reads=[b_tab], writes=[b_tab])
            t.op("vector", nc.vector.tensor_copy, iof[:, 0:256], iot[:, 0:256], reads=[b_tab], writes=[b_tab])
            for h in range(16):
                t.op("scalar", nc.scalar.activation, out=Ftab[:, h, :], in_=iof[:, 0:256], func=AF.Exp,
                     scale=-float(SLOPES[h]), reads=[b_tab], writes=[b_tab])
            t.op("gpsimd", nc.gpsimd.iota, iot[:, 0:512].rearrange("p (a b) -> p a b", a=2),
                 pattern=[[-128, 2], [1, 256]], base=0, channel_multiplier=-1, reads=[b_tab], writes=[b_tab])
            Dflat = Dtab[:].rearrange("p a b -> p (a b)")
            t.op("vector", nc.vector.tensor_copy, Dflat, iot[:, 0:512], reads=[b_tab], writes=[b_tab])
            t.op("vector", nc.vector.tensor_scalar, out=tmpf[:], in0=Dflat, scalar1=0.0, scalar2=1.0e6,
                 op0=ALU.is_lt, op1=ALU.mult, reads=[b_tab], writes=[b_tab])
            t.op("vector", nc.vector.tensor_tensor, out=Dflat, in0=Dflat, in1=tmpf[:], op=ALU.add,
                 reads=[b_tab], writes=[b_tab])
            t.op("vector", nc.vector.tensor_copy, Sel[:], identf[0:32, 0:32].unsqueeze(2).broadcast_to([32, 32, 128]),
                 reads=[b_const, b_tab], writes=[b_tab])
            t.op("vector", nc.vector.memset, cbias[:], -1.0e30, reads=[b_tab], writes=[b_tab])
            for qbl in range(8):
                t.op("vector", nc.vector.tensor_copy, cbias[:, qbl, 0:QB0 + qbl], vbt[:, 0:QB0 + qbl],
                     reads=[b_tab], writes=[b_tab])
            t.op("vector", nc.vector.tensor_copy, kmb[:], kmT[:], reads=[b_kmT, b_tab], writes=[b_tab])

            scpB2 = [ps(f"pascpB{i}", [128, 512], F32, pa) for i in range(2)]
            accB = [ps(f"paaccB{i}", [128, 512], F32, pa) for i in range(2)]
            dgB = [ps(f"padgB{i}", [128, 512], F32, pa) for i in range(2)]
            gps = ps("pagps", [128, 512], F32, pa)
            tps = ps("patps", [128, 1024], BF16, pa)
            scp = [scpB2[0][:, 0:256], scpB2[1][:, 0:256]]
            b_scp = [Buf(), Buf()]
            b_oacc, b_dacc, b_odg, b_ddg = [Buf(), Buf()], [Buf(), Buf()], [Buf(), Buf()], [Buf(), Buf()]
            b_gps, b_tps = Buf(), Buf()
            Gs = sb("paGs", [128, 32], F32, pa)
            m8 = sb("pam8", [128, 8], F32, pa)
            thr = sb("pathr", [128, 1], F32, pa)
            mf = sb("pamf", [128, 32], F32, pa)
            mb16 = sb("pamb16", [128, 32], BF16, pa)
            b_gs = Buf("gs")
            pT = [sb(f"papT{i}", [128, 256], BF16, pa) for i in range(2)]
            b_pT = [Buf(), Buf()]
            s32 = sb("pas32", [128, 256], F32, pa)
            b_s32 = Buf()
            num = sb("panum", [128, 256], F32, pa)
            den = sb("paden", [128, 256], F32, pa)
            b_nd = Buf()
            sza = [sb(f"pasza{i}", [128, 256], F32, pa) for i in range(2)]
            b_sza = [Buf(), Buf()]
            yo = [sb(f"payo{i}", [128, 256], BF16, pa) for i in range(2)]
            b_yo = [Buf(), Buf()]

            def load_head(h):
                hi = h % 2
                t.dma("sync", KT_[hi][:, 0:CTX // 2], kTd[h, :, 0:CTX // 2], reads=[b_kTd], writes=[b_KT[hi]])
                t.dma("sync", KT_[hi][:, CTX // 2:CTX], kTd[h, :, CTX // 2:CTX], reads=[b_kTd], writes=[b_KT[hi]])
                vsrc = Vd[:, h * 128:(h + 1) * 128].rearrange("(kt p) d -> p kt d", p=128)
                for q4 in range(4):
                    t.dma("sync", Vh[hi][:, q4 * 16:(q4 + 1) * 16, :], vsrc[:, q4 * 16:(q4 + 1) * 16, :],
                          reads=[b_Vd], writes=[b_Vh[hi]])
                t.dma("sync", QT[hi][:], qTd[h, :, :], reads=[b_qTd], writes=[b_QT[hi]])

            pc_ = 0
            load_head(0)
            NH = DBG if DBG else 16
            for h in range(NH):
                hi = h % 2
                for qt in range(OWN // 128):
                    qsl = slice(qt * 128, (qt + 1) * 128)
                    t.op("tensor", nc.tensor.matmul, gps[:, 0:32], lhsT=QT[hi][:, qsl], rhs=kmb[:, h, :],
                         start=True, stop=True, reads=[b_QT[hi], b_tab], writes=[b_gps])
                    t.op("vector", nc.vector.tensor_tensor, out=Gs[:], in0=gps[:, 0:32], in1=cbias[:, qt // 2, :],
                         op=ALU.add, reads=[b_gps, b_tab], writes=[b_gs])
                    t.op("vector", nc.vector.max, out=m8[:], in_=Gs[:], reads=[b_gs], writes=[b_gs])
                    t.op("vector", nc.vector.tensor_scalar, out=thr[:], in0=m8[:, 2:3], scalar1=-1.0e29, scalar2=None,
                         op0=ALU.max, reads=[b_gs], writes=[b_gs])
                    t.op("vector", nc.vector.tensor_scalar, out=mf[:], in0=Gs[:], scalar1=thr[:, 0:1], scalar2=None,
                         op0=ALU.is_ge, reads=[b_gs], writes=[b_gs])
                    t.op("vector", nc.vector.tensor_scalar, out=mb16[:], in0=mf[:], scalar1=-1.0, scalar2=30000.0,
                         op0=ALU.add, op1=ALU.mult, reads=[b_gs], writes=[b_gs])
                    t.op("tensor", nc.tensor.transpose, tps[0:32, 0:128], mb16[:], ident[:],
                         reads=[b_gs, b_const], writes=[b_tps])
                    t.op("vector", nc.vector.tensor_copy, maskT[:, qsl], tps[0:32, 0:128],
                         reads=[b_tps], writes=[b_maskT])
                if h + 1 < NH:
                    load_head(h + 1)
                jobs = []
                for qbl in range(DBG + 1 if DBG else 8):
                    qb = QB0 + qbl
                    for ktile in range(2 * qb):
                        jobs.append((qbl, "past", ktile, ktile == 0, ktile == 2 * qb - 1))
                    for kt2 in range(2):
                        jobs.append((qbl, "diag", 2 * qb + kt2, kt2 == 0, kt2 == 1))

                def stage1(job):
                    nonlocal pc_
                    qbl, kind, ktile, first, last = job
                    qb = QB0 + qbl
                    qsl = slice(qbl * 256, qbl * 256 + 256)
                    pi = pc_ % 2
                    pc_ += 1
                    if kind == "past":
                        if first:
                            oi = qbl % 2
                            t.dma("sync", sza[oi][:], szd[0][h, :, qsl], reads=[b_szd[0]], writes=[b_sza[oi]])
                        n = ktile // 2
                        t.op("tensor", nc.tensor.matmul, scp[pi], lhsT=KT_[hi][:, ktile * 128:(ktile + 1) * 128],
                             rhs=QT[hi][:, qsl], start=True, stop=False,
                             reads=[b_KT[hi], b_QT[hi]], writes=[b_scp[pi]], inc=False)
                        t.op("tensor", nc.tensor.matmul, scp[pi], lhsT=Sel[:, n, :], rhs=maskT[:, qsl],
                             start=False, stop=True, reads=[b_tab, b_maskT], writes=[b_scp[pi]])
                        m = (2 * qb) - ktile
                        t.op("scalar", nc.scalar.activation, out=pT[pi][:], in_=scp[pi], func=AF.Exp,
                             bias=kbias[:, h, m:m + 1], scale=1.0, reads=[b_scp[pi], b_tab], writes=[b_pT[pi]])
                    else:
                        kt2 = ktile - 2 * qb
                        t.op("tensor", nc.tensor.matmul, scp[pi], lhsT=KT_[hi][:, ktile * 128:(ktile + 1) * 128],
                             rhs=QT[hi][:, qsl], start=True, stop=True,
                             reads=[b_KT[hi], b_QT[hi]], writes=[b_scp[pi]])
                        t.op("vector", nc.vector.scalar_tensor_tensor, out=s32[:], in0=Dtab[:, kt2, :],
                             scalar=-float(SLOPES[h]), in1=scp[pi], op0=ALU.mult, op1=ALU.add,
                             reads=[b_tab, b_scp[pi]], writes=[b_s32])
                        t.op("scalar", nc.scalar.activation, out=pT[pi][:], in_=s32[:], func=AF.Exp,
                             reads=[b_s32], writes=[b_pT[pi]])
                    return pi

                def stage2(job, pi):
                    qbl, kind, ktile, first, last = job
                    s_ = qbl % 2
                    qsl = slice(qbl * 256, qbl * 256 + 256)
                    if kind == "past":
                        o_t, d_t, bo, bd = accB[s_][:, 0:256], dgB[s_][:, 0:256], b_oacc[s_], b_dacc[s_]
                    else:
                        o_t, d_t, bo, bd = accB[s_][:, 256:512], dgB[s_][:, 256:512], b_odg[s_], b_ddg[s_]
                    t.op("tensor", nc.tensor.matmul, o_t, lhsT=Vh[hi][:, ktile, :], rhs=pT[pi][:],
                         start=first, stop=last, reads=[b_Vh[hi], b_pT[pi]], writes=[bo], inc=last)
                    t.op("tensor", nc.tensor.matmul, d_t, lhsT=onesb[:], rhs=pT[pi][:],
                         start=first, stop=last, reads=[b_const, b_pT[pi]], writes=[bd], inc=last)
                    if kind == "diag" and last:
                        oi = qbl % 2
                        oa, da, og, dg = accB[s_][:, 0:256], dgB[s_][:, 0:256], accB[s_][:, 256:512], dgB[s_][:, 256:512]
                        V_ = lambda *a, r=(), w=(), **k: t.op("vector", nc.vector.tensor_tensor, *a, reads=r, writes=w, **k)
                        V_(out=num[:], in0=oa, in1=Ftab[:, h, :], op=ALU.mult,
                           r=[b_oacc[s_], b_odg[s_], b_dacc[s_], b_ddg[s_], b_tab], w=[b_nd])
                        V_(out=num[:], in0=og, in1=num[:], op=ALU.add, r=[b_odg[s_], b_nd], w=[b_nd])
                        V_(out=den[:], in0=da, in1=Ftab[:, h, :], op=ALU.mult, r=[b_dacc[s_], b_tab], w=[b_nd])
                        V_(out=den[:], in0=dg, in1=den[:], op=ALU.add, r=[b_ddg[s_], b_nd], w=[b_nd])
                        t.op("vector", nc.vector.reciprocal, den[:], den[:], reads=[b_nd], writes=[b_nd])
                        V_(out=num[:], in0=num[:], in1=den[:], op=ALU.mult, r=[b_nd], w=[b_nd])
                        V_(out=yo[oi][:], in0=num[:], in1=sza[oi][:], op=ALU.mult, r=[b_nd, b_sza[oi]], w=[b_yo[oi]])
                        t.dma("sync", yTd[0][h, :, qsl], yo[oi][:], reads=[b_yo[oi]], writes=[b_yTd[0]])

                prev = None
                for job in jobs + [None]:
                    cur = None
                    if job is not None:
                        cur = (job, stage1(job))
                    if prev is not None:
                        stage2(*prev)
                    prev = cur
            t.barrier()

        if ENABLE_S:
          with ExitStack() as pS:
            TWO_PI = 2.0 * math.pi
            V = lambda fn, *a, r=(), w=(), **k: t.op("vector", fn, *a, reads=r, writes=w, **k)
            A_ = lambda *a, r=(), w=(), **k: t.op("scalar", nc.scalar.activation, *a, reads=r, writes=w, **k)
            tt_, ts_, stt_ = nc.vector.tensor_tensor, nc.vector.tensor_scalar, nc.vector.scalar_tensor_tensor

            def sincos(arg, cos_o, sin_o, tmp, tmpi, b):
                for dst, shift in ((sin_o, 0.0), (cos_o, math.pi / 2)):
                    V(ts_, out=tmp, in0=arg, scalar1=shift, scalar2=1.0 / TWO_PI, op0=ALU.add, op1=ALU.mult, r=[b], w=[b])
                    V(nc.vector.tensor_copy, tmpi, tmp, r=[b], w=[b])
                    V(nc.vector.tensor_copy, tmp, tmpi, r=[b], w=[b])
                    V(stt_, out=tmp, in0=tmp, scalar=-TWO_PI, in1=arg, op0=ALU.mult, op1=ALU.add, r=[b], w=[b])
                    if shift:
                        V(ts_, out=tmp, in0=tmp, scalar1=shift, scalar2=None, op0=ALU.add, r=[b], w=[b])
                    V(ts_, out=dst, in0=tmp, scalar1=math.pi, scalar2=TWO_PI, op0=ALU.is_gt, op1=ALU.mult, r=[b], w=[b])
                    V(tt_, out=tmp, in0=tmp, in1=dst, op=ALU.subtract, r=[b], w=[b])
                    V(ts_, out=dst, in0=tmp, scalar1=-math.pi, scalar2=TWO_PI, op0=ALU.is_lt, op1=ALU.mult, r=[b], w=[b])
                    V(tt_, out=tmp, in0=tmp, in1=dst, op=ALU.add, r=[b], w=[b])
                    V(ts_, out=tmp, in0=tmp, scalar1=-3.14159, scalar2=3.14159, op0=ALU.max, op1=ALU.min, r=[b], w=[b])
                    A_(out=dst, in_=tmp, func=AF.Sin, r=[b], w=[b])

            rcol = sb("srcol", [128, 64], F32, pS)
            thcol = sb("sthcol", [128, 64], F32, pS)
            dcol = sb("sdcol", [128, 16], F32, pS)
            bglu = sb("sbglu", [128, 16], F32, pS)
            pbc = pS.enter_context(ExitStack())
            BbT = [sb(f"sBbT{i}", [128, 64 * 128], BF16, pbc) for i in range(2)]
            Cb = [sb(f"sCb{i}", [128, 64 * 128], BF16, pbc) for i in range(2)]
            b_par = Buf("spar")
            with ExitStack() as pp:
                CW = 2048
                nm = ["A", "I", "L", "T1", "T2", "T3", "T4", "T5", "T6", "Br", "Bi"]
                T = {n: sb("sp" + n, [128, CW], F32, pp) for n in nm}
                Ti = sb("spTi", [128, CW], mybir.dt.int32, pp)
                for c in range(8192 // CW):
                    cs = slice(c * CW, (c + 1) * CW)
                    for n, src in (("A", s_arep), ("I", s_irep), ("L", s_lrep), ("Br", s_bre), ("Bi", s_bim)):
                        t.dma("sync", T[n][:], src[:, cs], writes=[b_par])
                    g = lambda n: T[n][:]
                    A_(out=g("L"), in_=g("L"), func=AF.Exp, r=[b_par], w=[b_par])
                    V(tt_, out=g("T1"), in0=g("L"), in1=g("A"), op=ALU.mult, r=[b_par], w=[b_par])
                    A_(out=g("T1"), in_=g("T1"), func=AF.Exp, r=[b_par], w=[b_par])
                    V(tt_, out=g("T2"), in0=g("L"), in1=g("I"), op=ALU.mult, r=[b_par], w=[b_par])
                    sincos(g("T2"), g("T3"), g("T4"), g("T5"), Ti[:], b_par)
                    V(tt_, out=g("T3"), in0=g("T1"), in1=g("T3"), op=ALU.mult, r=[b_par], w=[b_par])
                    V(tt_, out=g("T4"), in0=g("T1"), in1=g("T4"), op=ALU.mult, r=[b_par], w=[b_par])
                    V(tt_, out=g("T5"), in0=g("A"), in1=g("A"), op=ALU.mult, r=[b_par], w=[b_par])
                    V(tt_, out=g("T6"), in0=g("I"), in1=g("I"), op=ALU.mult, r=[b_par], w=[b_par])
                    V(tt_, out=g("T5"), in0=g("T5"), in1=g("T6"), op=ALU.add, r=[b_par], w=[b_par])
                    V(nc.vector.reciprocal, g("T5"), g("T5"), r=[b_par], w=[b_par])
                    V(ts_, out=g("T3"), in0=g("T3"), scalar1=-1.0, scalar2=None, op0=ALU.add, r=[b_par], w=[b_par])
                    V(tt_, out=g("T6"), in0=g("T3"), in1=g("A"), op=ALU.mult, r=[b_par], w=[b_par])
                    V(tt_, out=g("T2"), in0=g("T4"), in1=g("I"), op=ALU.mult, r=[b_par], w=[b_par])
                    V(tt_, out=g("T6"), in0=g("T6"), in1=g("T2"), op=ALU.add, r=[b_par], w=[b_par])
                    V(tt_, out=g("T6"), in0=g("T6"), in1=g("T5"), op=ALU.mult, r=[b_par], w=[b_par])
                    V(tt_, out=g("T2"), in0=g("T4"), in1=g("A"), op=ALU.mult, r=[b_par], w=[b_par])
                    V(tt_, out=g("T1"), in0=g("T3"), in1=g("I"), op=ALU.mult, r=[b_par], w=[b_par])
                    V(tt_, out=g("T2"), in0=g("T2"), in1=g("T1"), op=ALU.subtract, r=[b_par], w=[b_par])
                    V(tt_, out=g("T2"), in0=g("T2"), in1=g("T5"), op=ALU.mult, r=[b_par], w=[b_par])
                    V(tt_, out=g("T1"), in0=g("Br"), in1=g("T6"), op=ALU.mult, r=[b_par], w=[b_par])
                    V(tt_, out=g("T3"), in0=g("Bi"), in1=g("T2"), op=ALU.mult, r=[b_par], w=[b_par])
                    V(tt_, out=BbT[0][:, cs], in0=g("T1"), in1=g("T3"), op=ALU.subtract, r=[b_par], w=[b_par])
                    V(tt_, out=g("T1"), in0=g("Br"), in1=g("T2"), op=ALU.mult, r=[b_par], w=[b_par])
                    V(tt_, out=g("T3"), in0=g("Bi"), in1=g("T6"), op=ALU.mult, r=[b_par], w=[b_par])
                    V(tt_, out=BbT[1][:, cs], in0=g("T1"), in1=g("T3"), op=ALU.add, r=[b_par], w=[b_par])
                    t.dma("sync", T["A"][:], s_cre[:, cs], writes=[b_par])
                    t.dma("sync", T["I"][:], s_cim[:, cs], writes=[b_par])
                    V(nc.vector.tensor_copy, Cb[0][:, cs], g("A"), r=[b_par], w=[b_par])
                    V(ts_, out=Cb[1][:, cs], in0=g("I"), scalar1=-1.0, scalar2=None, op0=ALU.mult, r=[b_par], w=[b_par])
                t.dma("sync", T["A"][:, 0:64], s_acol[:, :], writes=[b_par])
                t.dma("sync", T["I"][:, 0:64], s_icol[:, :], writes=[b_par])
                t.dma("sync", T["L"][:, 0:64], s_lcol[:, :], writes=[b_par])
                t.dma("sync", dcol[:], s_dcol[:, :], writes=[b_par])
                t.dma("sync", bglu[:], s_bglu[:, :], writes=[b_par])
                A_(out=T["L"][:, 0:64], in_=T["L"][:, 0:64], func=AF.Exp, r=[b_par], w=[b_par])
                V(tt_, out=T["T1"][:, 0:64], in0=T["L"][:, 0:64], in1=T["A"][:, 0:64], op=ALU.mult, r=[b_par], w=[b_par])
                A_(out=rcol[:], in_=T["T1"][:, 0:64], func=AF.Exp, r=[b_par], w=[b_par])
                V(tt_, out=thcol[:], in0=T["L"][:, 0:64], in1=T["I"][:, 0:64], op=ALU.mult, r=[b_par], w=[b_par])
                t.barrier()

            b_y1T = Buf("y1T")
            b_y1d = Buf("y1d")
            b_y1bd = Buf("y1bd")
            with ExitStack() as pm:
                SW = 512
                NSEG = CTX // SW
                SEG0 = OFF // SW
                UT = [sb(f"sUT{i}", [128, CTX], BF16, pm) for i in range(2)]
                b_UT = [Buf(), Buf()]
                iot = sb("siot", [128, SW + 1], mybir.dt.int32, pm)
                tauf = sb("stauf", [128, SW + 1], F32, pm)
                arg = sb("sarg", [128, SW + 1], F32, pm)
                tmp = sb("stmp", [128, SW + 1], F32, pm)
                tmpi = sb("stmpi", [128, SW + 1], mybir.dt.int32, pm)
                cosT = sb("scosT", [128, SW + 1], F32, pm)
                sinT = sb("ssinT", [128, SW + 1], F32, pm)
                Rb = sb("sRb", [128, SW], F32, pm)
                onesf = sb("sonesf", [128, SW], F32, pm)
                b_tb = Buf("stab")
                t1 = [sb(f"st1_{i}", [128, SW], F32, pm) for i in range(2)]
                t2 = [sb(f"st2_{i}", [128, SW], F32, pm) for i in range(2)]
                bre = [sb(f"sbre{i}", [128, SW], F32, pm) for i in range(2)]
                bim = [sb(f"sbim{i}", [128, SW], F32, pm) for i in range(2)]
                wre = [sb(f"swre{i}", [128, SW], F32, pm) for i in range(2)]
                wim = [sb(f"swim{i}", [128, SW], F32, pm) for i in range(2)]
                ini = [sb(f"sini{i}", [128, 4], F32, pm) for i in range(2)]
                xre = [sb(f"sxre{i}", [128, SW], BF16, pm) for i in range(2)]
                xim = [sb(f"sxim{i}", [128, SW], BF16, pm) for i in range(2)]
                b_seg = [Buf(), Buf()]
                b_seg2 = [Buf(), Buf()]
                b_bre = [Buf(), Buf()]
                b_bim = [Buf(), Buf()]
                t3 = [sb(f"st3_{i}", [128, SW], F32, pm) for i in range(2)]
                t4 = [sb(f"st4_{i}", [128, SW], F32, pm) for i in range(2)]
                t5 = sb("st5", [128, SW], F32, pm)
                t6 = sb("st6", [128, SW], F32, pm)
                b_t56 = Buf()
                if S_USE_POOL:
                    P = lambda r=(), w=(), **k: t.op("gpsimd", nc.gpsimd.tensor_tensor, reads=r, writes=w, **k)
                else:
                    P = lambda r=(), w=(), **k: t.op("vector", nc.vector.tensor_tensor, reads=r, writes=w, **k)
                b_w_ = [Buf(), Buf()]
                b_ini = [Buf(), Buf()]
                b_x = [Buf(), Buf()]
                bups = [[ps(f"sbups{i}{j}", [128, 512], F32, pm) for j in range(2)] for i in range(2)]
                b_bups = [Buf(), Buf()]
                yacc = [ps(f"syacc{i}", [128, 512], F32, pm) for i in range(4)]
                b_yacc = [Buf() for _ in range(4)]
                y32 = sb("sy32", [128, SW], F32, pm)
                g1 = sb("sg1", [128, SW], F32, pm)
                y16 = sb("sy16", [128, SW], BF16, pm)
                b_y32 = Buf()
                b_y16 = Buf()
                t.op("gpsimd", nc.gpsimd.iota, iot[:], pattern=[[1, SW + 1]], base=0, channel_multiplier=0, writes=[b_tb])
                V(nc.vector.tensor_copy, tauf[:], iot[:], r=[b_tb], w=[b_tb])
                V(nc.vector.memset, onesf[:], 1.0, r=[b_tb], w=[b_tb])
                sc = 0
                def load_UT(c8):
                    u_ = c8 % 2
                    t.dma("sync", UT[u_][:, 0:CTX // 2], uTd[c8, :, 0:CTX // 2], reads=[b_uTd], writes=[b_UT[u_]])
                    t.dma("sync", UT[u_][:, CTX // 2:CTX], uTd[c8, :, CTX // 2:CTX], reads=[b_uTd], writes=[b_UT[u_]])
                load_UT(0)
                NC8 = DBG if DBG else 16
                for ct8 in range(NC8):
                    ui = ct8 % 2
                    if ct8 + 1 < NC8:
                        load_UT(ct8 + 1)
                    for p4 in range(4):
                        pr = ct8 * 4 + p4
                        psl = slice(pr * 128, (pr + 1) * 128)
                        V(ts_, out=arg[:], in0=tauf[:], scalar1=thcol[:, pr:pr + 1], scalar2=None, op0=ALU.mult,
                          r=[b_tb, b_par], w=[b_tb])
                        sincos(arg[:], cosT[:], sinT[:], tmp[:], tmpi[:], b_tb)
                        V(ts_, out=Rb[:], in0=onesf[:], scalar1=rcol[:, pr:pr + 1], scalar2=None, op0=ALU.mult,
                          r=[b_tb, b_par], w=[b_tb])
                        for seg in range(NSEG):
                            i = sc % 2
                            sc += 1
                            tsl = slice(seg * SW, (seg + 1) * SW)
                            for ri in range(2):
                                t.op("tensor", nc.tensor.matmul, bups[i][ri][:], lhsT=BbT[ri][:, psl], rhs=UT[ui][:, tsl],
                                     start=True, stop=True, reads=[b_par, b_UT[ui]], writes=[b_bups[i]], inc=(ri == 1))
                            c_, s_ = cosT[:, 0:SW], sinT[:, 0:SW]
                            V(tt_, out=t1[i][:], in0=bups[i][0][:], in1=c_, op=ALU.mult, r=[b_bups[i], b_tb], w=[b_seg[i]])
                            V(tt_, out=t2[i][:], in0=bups[i][1][:], in1=s_, op=ALU.mult, r=[b_bups[i], b_tb], w=[b_seg[i]])
                            P(out=bre[i][:], in0=t1[i][:], in1=t2[i][:], op=ALU.add, r=[b_seg[i]], w=[b_bre[i]])
                            V(tt_, out=t3[i][:], in0=bups[i][1][:], in1=c_, op=ALU.mult, r=[b_bups[i], b_tb], w=[b_seg2[i]])
                            V(tt_, out=t4[i][:], in0=bups[i][0][:], in1=s_, op=ALU.mult, r=[b_bups[i], b_tb], w=[b_seg2[i]])
                            P(out=bim[i][:], in0=t3[i][:], in1=t4[i][:], op=ALU.subtract, r=[b_seg2[i]], w=[b_bim[i]])
                            if seg == 0:
                                i_re, i_im = 0.0, 0.0
                                rd = [b_tb]
                            else:
                                i_re, i_im = ini[i][:, 0:1], ini[i][:, 1:2]
                                rd = [b_tb, b_ini[i]]
                            V(nc.vector.tensor_tensor_scan, out=wre[i][:], data0=Rb[:], data1=bre[i][:], initial=i_re,
                              op0=ALU.mult, op1=ALU.add, r=rd + [b_bre[i]], w=[b_w_[i]])
                            V(nc.vector.tensor_tensor_scan, out=wim[i][:], data0=Rb[:], data1=bim[i][:], initial=i_im,
                              op0=ALU.mult, op1=ALU.add, r=rd + [b_bim[i]], w=[b_w_[i]])
                            if seg < NSEG - 1:
                                j = 1 - i
                                Ec, Es = cosT[:, SW:SW + 1], sinT[:, SW:SW + 1]
                                lr, li = wre[i][:, SW - 1:SW], wim[i][:, SW - 1:SW]
                                V(ts_, out=ini[j][:, 2:3], in0=li, scalar1=Es, scalar2=None, op0=ALU.mult,
                                  r=[b_w_[i], b_tb], w=[b_ini[j]])
                                V(stt_, out=ini[j][:, 0:1], in0=lr, scalar=Ec, in1=ini[j][:, 2:3], op0=ALU.mult, op1=ALU.subtract,
                                  r=[b_w_[i], b_tb, b_ini[j]], w=[b_ini[j]])
                                V(ts_, out=ini[j][:, 3:4], in0=li, scalar1=Ec, scalar2=None, op0=ALU.mult,
                                  r=[b_w_[i], b_tb, b_ini[j]], w=[b_ini[j]])
                                V(stt_, out=ini[j][:, 1:2], in0=lr, scalar=Es, in1=ini[j][:, 3:4], op0=ALU.mult, op1=ALU.add,
                                  r=[b_w_[i], b_tb, b_ini[j]], w=[b_ini[j]])
                            if seg >= SEG0:
                                so = seg - SEG0
                                P(out=t5[:], in0=wre[i][:], in1=c_, op=ALU.mult, r=[b_w_[i], b_tb], w=[b_t56])
                                P(out=t6[:], in0=wim[i][:], in1=s_, op=ALU.mult, r=[b_w_[i], b_tb], w=[b_t56])
                                P(out=xre[i][:], in0=t5[:], in1=t6[:], op=ALU.subtract, r=[b_t56], w=[b_x[i]])
                                P(out=t5[:], in0=wre[i][:], in1=s_, op=ALU.mult, r=[b_w_[i], b_tb], w=[b_t56])
                                P(out=t6[:], in0=wim[i][:], in1=c_, op=ALU.mult, r=[b_w_[i], b_tb], w=[b_t56])
                                P(out=xim[i][:], in0=t5[:], in1=t6[:], op=ALU.add, r=[b_t56], w=[b_x[i]])
                                t.op("tensor", nc.tensor.matmul, yacc[so][:], lhsT=Cb[0][:, psl], rhs=xre[i][:],
                                     start=(p4 == 0), stop=False, reads=[b_par, b_x[i]], writes=[b_yacc[so]], inc=False)
                                t.op("tensor", nc.tensor.matmul, yacc[so][:], lhsT=Cb[1][:, psl], rhs=xim[i][:],
                                     start=False, stop=(p4 == 3), reads=[b_par, b_x[i]], writes=[b_yacc[so]], inc=True)
                    for so in range(NSEG - SEG0):
                        tsl = slice(OFF + so * SW, OFF + (so + 1) * SW)
                        osl = slice(so * SW, (so + 1) * SW)
                        V(stt_, out=y32[:], in0=UT[ui][:, tsl], scalar=dcol[:, ct8:ct8 + 1], in1=yacc[so][:],
                          op0=ALU.mult, op1=ALU.add, r=[b_UT[ui], b_par, b_yacc[so]], w=[b_y32])
                        V(tt_, out=g1[:], in0=y32[:], in1=y32[:], op=ALU.mult, r=[b_y32], w=[b_y32])
                        V(ts_, out=g1[:], in0=g1[:], scalar1=0.044715, scalar2=1.0, op0=ALU.mult, op1=ALU.add, r=[b_y32], w=[b_y32])
                        V(tt_, out=g1[:], in0=g1[:], in1=y32[:], op=ALU.mult, r=[b_y32], w=[b_y32])
                        A_(out=g1[:], in_=g1[:], func=AF.Sigmoid, scale=1.5957691216057308, r=[b_y32], w=[b_y32])
                        V(tt_, out=y32[:], in0=y32[:], in1=g1[:], op=ALU.mult, r=[b_y32], w=[b_y32])
                        V(nc.vector.tensor_copy, y16[:], y32[:], r=[b_y32], w=[b_y16])
                        t.dma("sync", y1bd[ct8, :, osl], y16[:], reads=[b_y16], writes=[b_y1bd])
                        t.dma("sync", y1d[ct8, :, osl], y32[:], reads=[b_y32], writes=[b_y1d])
                t.barrier()
            pbc.close()
            with ExitStack() as pg:
                mk_w(pg)
                y1T = sb("sy1T", [128, 16, OWN], BF16, pg)
                for q4 in range(4):
                    t.dma("sync", y1T[:, q4 * 4:(q4 + 1) * 4, :], y1bd[q4 * 4:(q4 + 1) * 4, :, :].rearrange("c p t -> p c t"),
                          reads=[b_y1bd], writes=[b_y1T])
                gacc = [ps(f"sgacc{i}", [128, 512], F32, pg) for i in range(2)]
                b_gacc = [Buf(), Buf()]
                sgl = [sb(f"ssgl{i}", [128, 512], F32, pg) for i in range(2)]
                y1f = [sb(f"sy1f{i}", [128, 512], F32, pg) for i in range(2)]
                szs = [sb(f"sszs{i}", [128, 512], F32, pg) for i in range(2)]
                yo = [sb(f"ssyo{i}", [128, 512], BF16, pg) for i in range(2)]
                b_sgl, b_y1f, b_szs, b_yo = [Buf(), Buf()], [Buf(), Buf()], [Buf(), Buf()], [Buf(), Buf()]
                gc = 0
                for cb in range(1 if DBG else 4):
                    wt, bw = load_w(w_glu, 16, cb * 512)
                    for j in range(1 if DBG else 4):
                        f = cb * 4 + j
                        for tt in range(1 if DBG else NTT):
                            i = gc % 2
                            gc += 1
                            tsl = slice(tt * TT, (tt + 1) * TT)
                            t.dma("sync", y1f[i][:], y1d[f, :, tsl], reads=[b_y1d], writes=[b_y1f[i]])
                            t.dma("sync", szs[i][:], szd[1][f, :, tsl], reads=[b_szd[1]], writes=[b_szs[i]])
                            for ct in range(16):
                                t.op("tensor", nc.tensor.matmul, gacc[i][:], lhsT=wt[:, ct, j * 128:(j + 1) * 128],
                                     rhs=y1T[:, ct, tsl], start=(ct == 0), stop=(ct == 15),
                                     reads=[bw, b_y1T], writes=[b_gacc[i]], inc=(ct == 15))
                            A_(out=sgl[i][:], in_=gacc[i][:], func=AF.Sigmoid, bias=bglu[:, f:f + 1], scale=1.0,
                               r=[b_gacc[i], b_par], w=[b_sgl[i]])
                            V(tt_, out=sgl[i][:], in0=sgl[i][:], in1=y1f[i][:], op=ALU.mult, r=[b_sgl[i], b_y1f[i]], w=[b_sgl[i]])
                            V(tt_, out=yo[i][:], in0=sgl[i][:], in1=szs[i][:], op=ALU.mult, r=[b_sgl[i], b_szs[i]], w=[b_yo[i]])
                            t.dma("sync", yTd[1][f, :, tsl], yo[i][:], reads=[b_yo[i]], writes=[b_yTd[1]])
                t.barrier()

        with ExitStack() as pc2:
            qc = [sb(f"c2qc{i}", [128, 4, TT], BF16, pc2) for i in range(2)]
            szt = [sb(f"c2sz{i}", [128, 4, TT], F32, pc2) for i in range(2)]
            b_qc = [Buf(), Buf()]
            b_szt = [Buf(), Buf()]
            scp = [ps(f"c2scp{i}", [128, 512], F32, pc2) for i in range(2)]
            denp = ps("c2denp", [128, 512], F32, pc2)
            op_ = [ps(f"c2op{i}", [128, 512], F32, pc2) for i in range(4)]
            b_scp = [Buf(), Buf()]
            b_denp = Buf()
            b_op = [Buf() for _ in range(4)]
            pT = sb("c2pT", [128, 2, TT], BF16, pc2)
            rden = sb("c2rden", [128, TT], F32, pc2)
            otmp = sb("c2otmp", [128, TT], F32, pc2)
            yo = [sb(f"c2yo{i}", [128, 4, TT], BF16, pc2) for i in range(2)]
            b_pT, b_rden, b_otmp = Buf(), Buf(), Buf()
            b_yo = [Buf(), Buf()]
            it = 0
            for tt in range(1 if DBG else NTT):
                for hd in range(1 if DBG else 4):
                    i = it % 2
                    it += 1
                    tsl = slice(tt * TT, (tt + 1) * TT)
                    t.dma("sync", qc[i][:], qcTd[hd * 4:(hd + 1) * 4, :, tsl].rearrange("j p t -> p j t"),
                          reads=[b_qcTd], writes=[b_qc[i]])
                    t.dma("sync", szt[i][:], szd[2][hd * 4:(hd + 1) * 4, :, tsl].rearrange("j p t -> p j t"),
                          reads=[b_szd[2]], writes=[b_szt[i]])
                    for mt in range(2):
                        for j in range(4):
                            t.op("tensor", nc.tensor.matmul, scp[mt][:],
                                 lhsT=KcT[:, hd * 4 + j, mt * 128:(mt + 1) * 128], rhs=qc[i][:, j, :],
                                 start=(j == 0), stop=(j == 3), reads=[b_KcT, b_qc[i]], writes=[b_scp[mt]],
                                 inc=(j == 3))
                        t.op("scalar", nc.scalar.activation, out=pT[:, mt, :], in_=scp[mt][:], func=AF.Exp,
                             reads=[b_scp[mt]], writes=[b_pT])
                    for mt in range(2):
                        t.op("tensor", nc.tensor.matmul, denp[:], lhsT=onesb[:], rhs=pT[:, mt, :],
                             start=(mt == 0), stop=(mt == 1), reads=[b_pT, b_const], writes=[b_denp], inc=(mt == 1))
                    t.op("vector", nc.vector.reciprocal, rden[:], denp[:], reads=[b_denp], writes=[b_rden])
                    for j in range(4):
                        for mt in range(2):
                            t.op("tensor", nc.tensor.matmul, op_[j][:],
                                 lhsT=Vc[:, mt, hd * 512 + j * 128: hd * 512 + (j + 1) * 128], rhs=pT[:, mt, :],
                                 start=(mt == 0), stop=(mt == 1), reads=[b_Vc, b_pT], writes=[b_op[j]], inc=(mt == 1))
                        t.op("vector", nc.vector.tensor_tensor, out=otmp[:], in0=op_[j][:], in1=rden[:], op=ALU.mult,
                             reads=[b_op[j], b_rden], writes=[b_otmp])
                        t.op("vector", nc.vector.tensor_tensor, out=yo[i][:, j, :], in0=otmp[:], in1=szt[i][:, j, :],
                             op=ALU.mult, reads=[b_otmp, b_szt[i]], writes=[b_yo[i]])
                    t.dma("sync", yTd[2][hd * 4:(hd + 1) * 4, :, tsl].rearrange("j p t -> p j t"), yo[i][:],
                          reads=[b_yo[i]], writes=[b_yTd[2]])
            t.barrier()

        b_out = Buf("out")
        with ExitStack() as pf:
            mk_w(pf)
            yT = [sb(f"pfyT{i}", [128, 16, FT], BF16, pf) for i in range(2)]
            b_yT = [Buf(), Buf()]
            sg = [sb(f"pfsg{i}", [128, 4, FT], F32, pf) for i in range(2)]
            b_sg = [Buf(), Buf()]
            mrg32 = sb("pfmrg32", [128, 32, FT], F32, pf) if (ENABLE_A or ENABLE_S) else None
            mrgT = sb("pfmrgT", [128, 32, FT], BF16, pf)
            b_m32 = [Buf() for _ in range(32)]
            b_mT = Buf()
            acc = [ps(f"pfacc{i}", [128, 512], F32, pf) for i in range(4)]
            b_acc = [Buf() for _ in range(4)]
            xr = [sb(f"pfxr{i}", [128, 512], F32, pf) for i in range(2)]
            b_xr = [Buf(), Buf()]
            oo = [sb(f"pfoo{i}", [128, 512], F32, pf) for i in range(2)]
            b_oo = [Buf(), Buf()]
            brs = [b for b in range(3) if (b == 0 and ENABLE_A) or (b == 1 and ENABLE_S) or b == 2]
            ctr = 0
            yc = 0
            for tt in range(1 if DBG else OWN // FT):
                tsl = slice(tt * FT, (tt + 1) * FT)
                for bi, b in enumerate(brs):
                    yi = yc % 2
                    yc += 1
                    t.dma("sync", yT[yi][:], yTd[b][:, :, tsl].rearrange("c p t -> p c t"),
                          reads=[b_yTd[b]], writes=[b_yT[yi]])
                    for fb in range(8):
                        wt, bw = load_w(w_br[b], 16, fb * 512)
                        si = (yc * 8 + fb) % 2
                        t.dma("sync", sg[si][:], sgd[b][fb * 4:(fb + 1) * 4, :, tsl].rearrange("j p t -> p j t"),
                              reads=[b_sgd[b]], writes=[b_sg[si]])
                        for j in range(4):
                            a = ctr % 4
                            ctr += 1
                            f = fb * 4 + j
                            for ct in range(16):
                                t.op("tensor", nc.tensor.matmul, acc[a][:, 0:FT], lhsT=wt[:, ct, j * 128:(j + 1) * 128],
                                     rhs=yT[yi][:, ct, :], start=(ct == 0), stop=(ct == 15),
                                     reads=[bw, b_yT[yi]], writes=[b_acc[a]], inc=(ct == 15))
                            last = (bi == len(brs) - 1)
                            if bi == 0:
                                t.op("vector", nc.vector.tensor_tensor,
                                     out=(mrgT[:, f, :] if last else mrg32[:, f, :]),
                                     in0=acc[a][:, 0:FT], in1=sg[si][:, j, :], op=ALU.mult,
                                     reads=[b_acc[a], b_sg[si]], writes=[b_mT if last else b_m32[f]])
                            else:
                                t.op("vector", nc.vector.tensor_tensor, out=sg[si][:, j, :], in0=acc[a][:, 0:FT],
                                     in1=sg[si][:, j, :], op=ALU.mult,
                                     reads=[b_acc[a], b_sg[si]], writes=[b_sg[si]])
                                t.op("gpsimd", nc.gpsimd.tensor_tensor,
                                     out=(mrgT[:, f, :] if last else mrg32[:, f, :]),
                                     in0=sg[si][:, j, :], in1=mrg32[:, f, :], op=ALU.add,
                                     reads=[b_sg[si], b_m32[f]], writes=[b_mT if last else b_m32[f]])
                for cb in range(1 if DBG else 8):
                    wt, bw = load_w(w_out, 32, cb * 512)
                    for s in range(FT // 128):
                        a = ctr % 4
                        o = ctr % 2
                        ctr += 1
                        r0 = tt * FT + s * 128
                        t.dma("sync", xr[o][:], x[r0:r0 + 128, cb * 512:(cb + 1) * 512], writes=[b_xr[o]])
                        for ft in range(32):
                            t.op("tensor", nc.tensor.matmul, acc[a][:], lhsT=mrgT[:, ft, s * 128:(s + 1) * 128],
                                 rhs=wt[:, ft, :], start=(ft == 0), stop=(ft == 31),
                                 reads=[bw, b_mT], writes=[b_acc[a]], inc=(ft == 31))
                        t.op("vector", nc.vector.tensor_tensor, out=oo[o][:], in0=acc[a][:], in1=xr[o][:], op=ALU.add,
                             reads=[b_acc[a], b_xr[o]], writes=[b_oo[o]])
                        t.dma("sync", out[r0:r0 + 128, cb * 512:(cb + 1) * 512], oo[o][:],
                              reads=[b_oo[o]], writes=[b_out])
            t.wait_all("sync", [b_out])
            t.barrier()

        block = st.enter_context(nc.Block())
        t.replay(block)
    return nc


_NC = None


def _in_maps(inputs):
    f32 = lambda a: np.ascontiguousarray(np.asarray(a, dtype=np.float32))
    x = f32(inputs["x"])
    mem = f32(inputs["mem"])
    B, S, _ = x.shape
    xs = x.reshape(B * S // OWN, OWN, D)
    shared = {
        "w_in": f32(inputs["w_in"]),
        "w_mem_kv": f32(inputs["w_mem_kv"]),
        "w_br_a": f32(inputs["w_br_a"]),
        "w_br_s": f32(inputs["w_br_s"]),
        "w_br_c": f32(inputs["w_br_c"]),
        "w_out": f32(inputs["w_out"]),
        "gnB": f32(np.broadcast_to(f32(inputs["g_norm"])[None, :], (128, D))),
        "gmB": f32(np.broadcast_to(f32(inputs["g_mem"])[None, :], (128, D))),
        "qgc": f32(f32(inputs["q_gain_c"]).reshape(4, 128).T),
        "kgc": f32(f32(inputs["k_gain_c"]).reshape(4, 128).T),
    }
    shared["qga"] = f32(f32(inputs["q_gain_a"]).reshape(128, 1))
    shared["kga"] = f32(f32(inputs["k_gain_a"]).reshape(128, 1))

    a_re = f32(inputs["ssm_a_re"]); a_im = f32(inputs["ssm_a_im"]); ldt = f32(inputs["ssm_log_dt"])
    row = lambda v: f32(np.broadcast_to(v.reshape(1, 8192), (128, 8192)))
    shared["s_arep"] = row(a_re); shared["s_irep"] = row(a_im)
    shared["s_lrep"] = row(np.repeat(ldt, 64))
    col = lambda v: f32(v.reshape(64, 128).T)
    shared["s_acol"] = col(a_re); shared["s_icol"] = col(a_im); shared["s_lcol"] = col(np.repeat(ldt, 64))
    b_re = f32(inputs["ssm_b_re"]); b_im = f32(inputs["ssm_b_im"])
    c_re = f32(inputs["ssm_c_re"]); c_im = f32(inputs["ssm_c_im"])
    def arrB(b):
        o = np.zeros((128, 64, 128), np.float32)
        for pr in range(64):
            for gi in range(2):
                g = 2 * pr + gi
                k0 = (g % 8) * 16
                o[k0:k0 + 16, pr, gi * 64:(gi + 1) * 64] = b[g].T
        return f32(o.reshape(128, 8192))
    def arrC(cm):
        o = np.zeros((128, 64, 128), np.float32)
        for pr in range(64):
            for gi in range(2):
                g = 2 * pr + gi
                k0 = (g % 8) * 16
                o[gi * 64:(gi + 1) * 64, pr, k0:k0 + 16] = cm[g].T
        return f32(o.reshape(128, 8192))
    shared["s_bre"] = arrB(b_re); shared["s_bim"] = arrB(b_im)
    shared["s_cre"] = arrC(c_re); shared["s_cim"] = arrC(c_im)
    shared["s_dcol"] = f32(f32(inputs["ssm_d"]).reshape(16, 128).T)
    shared["s_bglu"] = f32(f32(inputs["b_glu"]).reshape(16, 128).T)
    shared["w_glu"] = f32(inputs["w_glu"])
    in_maps = []
    nper = S // OWN
    for c in range(8):
        b, pos = divmod(c, nper)
        m = dict(shared)
        m["x"] = np.ascontiguousarray(xs[c])
        m["mem"] = np.ascontiguousarray(mem[b])
        xp = np.zeros((OFF, D), np.float32)
        if pos > 0:
            xp[OFF - pos * OWN:] = x[b, :pos * OWN]
        m["xprev"] = xp
        vb = np.zeros((128, 32), np.float32)
        vb[:, :(OFF - pos * OWN) // 256] = -1.0e30
        m["vbB"] = vb
        in_maps.append(m)
    return in_maps, (B, S)


def kernel(**inputs):
    global _NC
    in_maps, (B, S) = _in_maps(inputs)
    if _NC is None:
        _NC = build_nc()
    res = run_bass_kernel_spmd(_NC, in_maps, core_ids=list(range(8)))
    o = np.stack([np.asarray(r["out"], dtype=np.float32) for r in res.results], axis=0)
    return o.reshape(B, S, D)
```

```python
import math
import os
import numpy as np
from contextlib import ExitStack
import concourse.bass as bass
import concourse.mybir as mybir
from concourse.bass_utils import run_bass_kernel_spmd

F32 = mybir.dt.float32
BF16 = mybir.dt.bfloat16
AF = mybir.ActivationFunctionType
ALU = mybir.AluOpType
AX = mybir.AxisListType

D = 4096
W = 2048
NIN = 28672
OWN = 2048
TT = 512
NTT = OWN // TT
EPS = 1e-6
MEM = 256

DBG = int(os.environ.get("K_DBG", "0"))
ENGS = ("tensor", "vector", "scalar", "gpsimd", "sync")

ENABLE_A = True
ENABLE_S = True
S_USE_POOL = False
CTX = 8192
OFF = CTX - OWN
NCT = CTX // TT
FT = 512
SLOPES = [2.0 ** (-8.0 * (i + 1) / 16) for i in range(16)]


class Buf:
    __slots__ = ("name", "w", "r", "dsem", "dcount")

    def __init__(self, name=""):
        self.name = name
        self.w = None
        self.r = {}
        self.dsem = None
        self.dcount = 0


class Trk:
    def __init__(self, nc, stack):
        self.nc = nc
        self.stack = stack
        self.ops = {e: [] for e in ENGS}
        self.cnt = {e: 0 for e in ENGS}
        self.sem = {e: stack.enter_context(nc.semaphore(f"s_{e}")) for e in ENGS}
        self.seen = {e: {} for e in ENGS}
        self.dsems = []

    def _dsem(self, buf):
        if buf.dsem is None:
            buf.dsem = self.stack.enter_context(self.nc.semaphore(f"d{len(self.dsems)}"))
            self.dsems.append(buf)
        return buf.dsem

    def _need(self, eng, dep, waits):
        if dep is None:
            return
        kind, key, count = dep
        k = (kind, id(key) if kind == 'd' else key)
        if self.seen[eng].get(k, 0) >= count:
            return
        self.seen[eng][k] = count
        waits.append((self.sem[key] if kind == 'e' else key, count))

    def _deps(self, eng, reads, writes):
        waits = []
        for b in reads:
            self._need(eng, b.w, waits)
        for b in writes:
            self._need(eng, b.w, waits)
            for d in b.r.values():
                self._need(eng, d, waits)
        return waits

    def op(self, eng, fn, *args, reads=(), writes=(), inc=True, **kw):
        waits = self._deps(eng, reads, writes)
        if inc:
            self.cnt[eng] += 1
            c = self.cnt[eng]
        else:
            c = self.cnt[eng] + 1
        me = ('e', eng, c)
        for b in reads:
            b.r[eng] = me
        if inc:
            for b in writes:
                b.w = me
                b.r = {}
        self.ops[eng].append((fn, args, kw, waits, (self.sem[eng], 1) if inc else None))

    def dma(self, eng, out, in_, reads=(), writes=()):
        waits = self._deps(eng, reads, writes)
        anchor = writes[0] if writes else reads[0]
        sem = self._dsem(anchor)
        anchor.dcount += 16
        me = ('d', sem, anchor.dcount)
        for b in reads:
            b.r[('d', id(sem))] = me
        for b in writes:
            b.w = me
            b.r = {}
        fn = getattr(self.nc, eng).dma_start
        self.ops[eng].append((fn, (), dict(out=out, in_=in_), waits, (sem, 16)))

    def wait_all(self, eng, bufs):
        waits = []
        for b in bufs:
            self._need(eng, b.w, waits)
            for d in b.r.values():
                self._need(eng, d, waits)
        self.ops[eng].append((None, (), {}, waits, None))

    def barrier(self):
        snap = [('e', e, self.cnt[e]) for e in ENGS if self.cnt[e] > 0]
        snap += [('d', b.dsem, b.dcount) for b in self.dsems if b.dcount > 0]
        for e in ENGS:
            waits = []
            for d in snap:
                if d[0] == 'e' and d[1] == e:
                    continue
                self._need(e, d, waits)
            self.ops[e].append((None, (), {}, waits, None))

    def replay(self, block):
        for e in ENGS:
            ops = self.ops[e]
            if not ops:
                continue

            def body(engine, ops=ops):
                for fn, args, kw, waits, inc in ops:
                    for s, v in waits:
                        engine.wait_ge(s, v)
                    if fn is not None:
                        ins = fn(*args, **kw)
                        if inc is not None:
                            ins.then_inc(inc[0], inc[1])
            getattr(block, e)(body)


DRAM_NAMES = []


def build_nc():
    nc = bass.Bass("TRN2", target_bir_lowering=False)

    def din(name, shape, dt=F32):
        DRAM_NAMES.append(name)
        return nc.dram_tensor(name, list(shape), dt, kind="ExternalInput").ap()

    def dscr(name, shape, dt):
        DRAM_NAMES.append(name)
        return nc.dram_tensor(name, list(shape), dt, kind="Internal").ap()

    x = din("x", [OWN, D])
    mem = din("mem", [MEM, D])
    w_in = din("w_in", [D, NIN])
    w_kv = din("w_mem_kv", [D, 2 * W])
    w_br = [din(n, [W, D]) for n in ("w_br_a", "w_br_s", "w_br_c")]
    w_out = din("w_out", [D, D])
    gnB = din("gnB", [128, D])
    gmB = din("gmB", [128, D])
    qgc = din("qgc", [128, 4])
    kgc = din("kgc", [128, 4])
    xprev = din("xprev", [OFF, D])
    vbB = din("vbB", [128, 32])
    qga = din("qga", [128, 1])
    kga = din("kga", [128, 1])
    s_arep = din("s_arep", [128, 8192]); s_irep = din("s_irep", [128, 8192]); s_lrep = din("s_lrep", [128, 8192])
    s_acol = din("s_acol", [128, 64]); s_icol = din("s_icol", [128, 64]); s_lcol = din("s_lcol", [128, 64])
    s_bre = din("s_bre", [128, 8192]); s_bim = din("s_bim", [128, 8192])
    s_cre = din("s_cre", [128, 8192]); s_cim = din("s_cim", [128, 8192])
    s_dcol = din("s_dcol", [128, 16]); s_bglu = din("s_bglu", [128, 16])
    w_glu = din("w_glu", [W, W])
    out = nc.dram_tensor("out", [OWN, D], F32, kind="ExternalOutput").ap()

    hTd = dscr("hTd", [32, 128, CTX], BF16)
    kTd = dscr("kTd", [16, 128, CTX], BF16)
    qTd = dscr("qTd", [16, 128, OWN], BF16)
    Vd = dscr("Vd", [CTX, W], BF16)
    uTd = dscr("uTd", [16, 128, CTX], BF16)
    y1d = dscr("y1d", [16, 128, OWN], F32)
    y1bd = dscr("y1bd", [16, 128, OWN], BF16)
    qcTd = dscr("qcTd", [16, 128, OWN], BF16)
    szd = [dscr(f"szd{i}", [16, 128, OWN], F32) for i in range(3)]
    sgd = [dscr(f"sgd{i}", [32, 128, OWN], F32) for i in range(3)]
    yTd = [dscr(f"yTd{i}", [16, 128, OWN], BF16) for i in range(3)]

    with ExitStack() as st:
        t = Trk(nc, st)
        sb = lambda name, shape, dt, stk: stk.enter_context(nc.sbuf_tensor(name, list(shape), dt))
        ps = lambda name, shape, dt, stk: stk.enter_context(nc.psum_tensor(name, list(shape), dt))

        identf = sb("identf", [128, 128], F32, st)
        ident = sb("ident", [128, 128], BF16, st)
        onesb = sb("onesb", [128, 128], BF16, st)
        epst = sb("epst", [128, 1], F32, st)
        qgct = sb("qgct", [128, 4], F32, st)
        kgct = sb("kgct", [128, 4], F32, st)
        KcT = sb("KcT", [128, 16, MEM], BF16, st)
        Vc = sb("Vc", [128, 2, W], BF16, st)
        qgat = sb("qgat", [128, 1], F32, st)
        kgat = sb("kgat", [128, 1], F32, st)
        kmT = sb("kmT", [128, 16, 32], F32, st)
        b_kmT = Buf("kmT")
        b_const = Buf("const")
        b_KcT = Buf("KcT")
        b_Vc = Buf("Vc")
        t.op("gpsimd", nc.gpsimd.memset, identf[:], 0.0, writes=[b_const])
        t.op("gpsimd", nc.gpsimd.affine_select, out=identf[:], in_=identf[:], pattern=[[-1, 128]],
             compare_op=ALU.not_equal, fill=1.0, base=0, channel_multiplier=1,
             reads=[b_const], writes=[b_const])
        t.op("vector", nc.vector.tensor_copy, ident[:], identf[:], reads=[b_const], writes=[b_const])
        t.op("vector", nc.vector.memset, onesb[:], 1.0, reads=[b_const], writes=[b_const])
        t.op("vector", nc.vector.memset, epst[:], EPS, reads=[b_const], writes=[b_const])
        if DBG:
            t.op("vector", nc.vector.memset, kmT[:], 0.0, writes=[b_kmT])
        b_g = Buf("gains")
        t.dma("sync", qgct[:], qgc[:, :], writes=[b_g])
        t.dma("sync", kgct[:], kgc[:, :], writes=[b_g])
        t.dma("sync", qgat[:], qga[:, :], writes=[b_g])
        t.dma("sync", kgat[:], kga[:, :], writes=[b_g])
        t.op("vector", nc.vector.tensor_scalar, out=qgat[:], in0=qgat[:], scalar1=float(128 ** -0.5),
             scalar2=None, op0=ALU.mult, reads=[b_g], writes=[b_g])
        t.op("vector", nc.vector.tensor_scalar, out=qgct[:], in0=qgct[:], scalar1=float(512 ** -0.5),
             scalar2=None, op0=ALU.mult, reads=[b_g], writes=[b_g])

        wbuf = [None, None]
        b_w = [Buf(f"w{i}") for i in range(2)]
        wctr = [0]
        wgen = [0]

        def mk_w(stk):
            wgen[0] += 1
            for i in range(2):
                wbuf[i] = sb(f"wbuf{wgen[0]}_{i}", [128, 32, 512], BF16, stk)

        def load_w(src, nkt, c0, ncols=512):
            i = wctr[0] % 2
            wctr[0] += 1
            v = src.rearrange("(kt p) c -> p kt c", p=128)
            half = nkt // 2
            t.dma("gpsimd", wbuf[i][:, 0:half, 0:ncols], v[:, 0:half, c0:c0 + ncols], writes=[b_w[i]])
            t.dma("gpsimd", wbuf[i][:, half:nkt, 0:ncols], v[:, half:nkt, c0:c0 + ncols], writes=[b_w[i]])
            return wbuf[i], b_w[i]

        b_hTd = Buf("hTd")
        with ExitStack() as p0:
            hT = [sb(f"p0hT{i}", [128, 32, TT], BF16, p0) for i in range(2)]
            b_hT = [Buf(), Buf()]
            gB = sb("p0gB", [128, D], F32, p0)
            xt = [sb(f"p0xt{i}", [128, D], F32, p0) for i in range(2)]
            hb = sb("p0hb", [128, D], BF16, p0)
            stat = sb("p0stat", [128, 4], F32, p0)
            tp = [ps(f"p0tp{i}", [128, 1024], BF16, p0) for i in range(4)]
            b_gB, b_hb, b_junk, b_stat = Buf(), Buf(), Buf(), Buf()
            b_xt = [Buf(), Buf()]
            b_tp = [Buf() for _ in range(4)]
            t.dma("sync", gB[:], gnB[:, :], writes=[b_gB])
            for i in range(CTX // 128):
                tt, s = divmod(i, 4)
                xi, bx = xt[i % 2], b_xt[i % 2]
                src = xprev[i * 128:(i + 1) * 128, :] if i < OFF // 128 else x[i * 128 - OFF:(i + 1) * 128 - OFF, :]
                t.dma("sync", xi[:, 0:D // 2], src[:, 0:D // 2], writes=[bx])
                t.dma("sync", xi[:, D // 2:D], src[:, D // 2:D], writes=[bx])
                t.op("scalar", nc.scalar.activation, out=hb[:], in_=xi[:], func=AF.Square,
                     accum_out=stat[:, 0:1], reads=[bx], writes=[b_hb, b_stat])
                t.op("scalar", nc.scalar.activation, out=stat[:, 1:2], in_=stat[:, 0:1], func=AF.Sqrt,
                     bias=epst[:, 0:1], scale=1.0 / D, reads=[b_stat, b_const], writes=[b_stat])
                t.op("vector", nc.vector.reciprocal, stat[:, 2:3], stat[:, 1:2], reads=[b_stat], writes=[b_stat])
                t.op("vector", nc.vector.scalar_tensor_tensor, out=hb[:], in0=xi[:], scalar=stat[:, 2:3],
                     in1=gB[:], op0=ALU.mult, op1=ALU.mult, reads=[bx, b_stat, b_gB], writes=[b_hb])
                for q in range(4):
                    for k in range(8):
                        kt = q * 8 + k
                        t.op("tensor", nc.tensor.transpose, tp[q][:, k * 128:(k + 1) * 128],
                             hb[:, kt * 128:(kt + 1) * 128], ident[:],
                             reads=[b_hb, b_const], writes=[b_tp[q]], inc=(k == 7))
                    dst = hT[tt % 2][:, q * 8:(q + 1) * 8, s * 128:(s + 1) * 128]
                    srcp = tp[q][:].rearrange("p (k c) -> p k c", k=8)
                    if q % 2 == 0:
                        t.op("vector", nc.vector.tensor_copy, dst, srcp, reads=[b_tp[q]], writes=[b_hT[tt % 2]])
                    else:
                        t.op("scalar", nc.scalar.copy, dst, srcp, reads=[b_tp[q]], writes=[b_hT[tt % 2]])
                if s == 3:
                    t.dma("sync", hTd[:, :, tt * TT:(tt + 1) * TT].rearrange("kt p t -> p kt t"),
                          hT[tt % 2][:], reads=[b_hT[tt % 2]], writes=[b_hTd])
            t.barrier()

        with ExitStack() as pc:
            mT = sb("pcmT", [128, 32, MEM], BF16, pc)
            b_mT = Buf("mT")
            pca = pc.enter_context(ExitStack())
            gB = sb("pcgB", [128, D], F32, pca)
            xt = [sb(f"pcxt{i}", [128, D], F32, pca) for i in range(2)]
            hb = sb("pchb", [128, D], BF16, pca)
            junk = sb("pcjunk", [128, D], BF16, pca)
            stat = sb("pcstat", [128, 4], F32, pca)
            tp = [ps(f"pctp{i}", [128, 1024], BF16, pca) for i in range(4)]
            b_gB, b_hb, b_junk, b_stat = Buf(), Buf(), Buf(), Buf()
            b_xt = [Buf(), Buf()]
            b_tp = [Buf() for _ in range(4)]
            t.dma("sync", gB[:], gmB[:, :], writes=[b_gB])
            for i in range(MEM // 128):
                xi, bx = xt[i % 2], b_xt[i % 2]
                src = mem[i * 128:(i + 1) * 128, :]
                t.dma("sync", xi[:], src, writes=[bx])
                t.op("scalar", nc.scalar.activation, out=junk[:], in_=xi[:], func=AF.Square,
                     accum_out=stat[:, 0:1], reads=[bx], writes=[b_junk, b_stat])
                t.op("scalar", nc.scalar.activation, out=stat[:, 1:2], in_=stat[:, 0:1], func=AF.Sqrt,
                     bias=epst[:, 0:1], scale=1.0 / D, reads=[b_stat, b_const], writes=[b_stat])
                t.op("vector", nc.vector.reciprocal, stat[:, 2:3], stat[:, 1:2], reads=[b_stat], writes=[b_stat])
                t.op("vector", nc.vector.scalar_tensor_tensor, out=hb[:], in0=xi[:], scalar=stat[:, 2:3],
                     in1=gB[:], op0=ALU.mult, op1=ALU.mult, reads=[bx, b_stat, b_gB], writes=[b_hb])
                for q in range(4):
                    for k in range(8):
                        kt = q * 8 + k
                        t.op("tensor", nc.tensor.transpose, tp[q][:, k * 128:(k + 1) * 128],
                             hb[:, kt * 128:(kt + 1) * 128], ident[:],
                             reads=[b_hb, b_const], writes=[b_tp[q]], inc=(k == 7))
                    dst = mT[:, q * 8:(q + 1) * 8, i * 128:(i + 1) * 128]
                    srcp = tp[q][:].rearrange("p (k c) -> p k c", k=8)
                    t.op("vector", nc.vector.tensor_copy, dst, srcp, reads=[b_tp[q]], writes=[b_mT])
            t.barrier()
            pca.close()
            mk_w(pc)
            kps = [ps(f"pckps{i}", [128, 512], F32, pc) for i in range(4)]
            ssp = ps("pcssp", [128, 512], F32, pc)
            vps = [ps(f"pcvps{i}", [128, 512], F32, pc) for i in range(2)]
            sq = sb("pcsq", [128, 4, MEM], BF16, pc)
            rstd = sb("pcrstd", [128, MEM], F32, pc)
            b_kps = [Buf() for _ in range(4)]
            b_ssp, b_sq, b_rstd = Buf(), Buf(), Buf()
            b_vps = [Buf(), Buf()]
            for hd in range(4):
                wt, bw = load_w(w_kv, 32, hd * 512)
                for j in range(4):
                    for kt in range(32):
                        t.op("tensor", nc.tensor.matmul, kps[j][:, 0:MEM], lhsT=wt[:, kt, j * 128:(j + 1) * 128],
                             rhs=mT[:, kt, :], start=(kt == 0), stop=(kt == 31),
                             reads=[bw, b_mT], writes=[b_kps[j]], inc=(kt == 31))
                    t.op("scalar", nc.scalar.activation, out=sq[:, j, :], in_=kps[j][:, 0:MEM], func=AF.Square,
                         reads=[b_kps[j]], writes=[b_sq])
                for j in range(4):
                    t.op("tensor", nc.tensor.matmul, ssp[:, 0:MEM], lhsT=onesb[:], rhs=sq[:, j, :],
                         start=(j == 0), stop=(j == 3), reads=[b_sq, b_const], writes=[b_ssp], inc=(j == 3))
                t.op("scalar", nc.scalar.activation, out=rstd[:], in_=ssp[:, 0:MEM], func=AF.Sqrt,
                     bias=epst[:, 0:1], scale=1.0 / 512, reads=[b_ssp, b_const], writes=[b_rstd])
                t.op("vector", nc.vector.reciprocal, rstd[:], rstd[:], reads=[b_rstd], writes=[b_rstd])
                for j in range(4):
                    t.op("vector", nc.vector.scalar_tensor_tensor, out=KcT[:, hd * 4 + j, :], in0=kps[j][:, 0:MEM],
                         scalar=kgct[:, j:j + 1], in1=rstd[:], op0=ALU.mult, op1=ALU.mult,
                         reads=[b_kps[j], b_g, b_rstd], writes=[b_KcT])
            for cb in range(4):
                wt, bw = load_w(w_kv, 32, W + cb * 512)
                for mt in range(2):
                    for kt in range(32):
                        t.op("tensor", nc.tensor.matmul, vps[mt][:], lhsT=mT[:, kt, mt * 128:(mt + 1) * 128],
                             rhs=wt[:, kt, :], start=(kt == 0), stop=(kt == 31),
                             reads=[bw, b_mT], writes=[b_vps[mt]], inc=(kt == 31))
                    t.op("vector", nc.vector.tensor_copy, Vc[:, mt, cb * 512:(cb + 1) * 512], vps[mt][:],
                         reads=[b_vps[mt]], writes=[b_Vc])
            t.barrier()

        b_qcTd = Buf("qcTd")
        b_szd = [Buf() for _ in range(3)]
        b_sgd = [Buf() for _ in range(3)]
        with ExitStack() as p1:
            mk_w(p1)
            hT = [sb(f"p1hT{i}", [128, 32, TT], BF16, p1) for i in range(2)]
            b_hT = [Buf(), Buf()]
            acc = [ps(f"p1acc{i}", [128, 512], F32, p1) for i in range(4)]
            b_acc = [Buf() for _ in range(4)]
            ssp = ps("p1ssp", [128, 512], F32, p1)
            b_ssp = Buf()
            sq = sb("p1sq", [128, 4, TT], BF16, p1)
            rstd = sb("p1rstd", [128, TT], F32, p1)
            b_sq, b_rstd = Buf(), Buf()
            ob32 = [sb(f"p1ob32_{i}", [128, TT], F32, p1) for i in range(2)]
            ob16 = [sb(f"p1ob16_{i}", [128, 4, TT], BF16, p1) for i in range(2)]
            b_ob32 = [Buf(), Buf()]
            b_ob16 = [Buf(), Buf()]
            hctr = [0]
            octr = [0]

            def load_hT(tt):
                i = hctr[0] % 2
                hctr[0] += 1
                src = hTd[:, :, tt * TT:(tt + 1) * TT].rearrange("kt p t -> p kt t")
                t.dma("sync", hT[i][:, 0:16, :], src[:, 0:16, :], reads=[b_hTd], writes=[b_hT[i]])
                t.dma("sync", hT[i][:, 16:32, :], src[:, 16:32, :], reads=[b_hTd], writes=[b_hT[i]])
                return hT[i], b_hT[i]

            def proj_fm(wt, bw, j, ht, bh, a):
                for kt in range(32):
                    t.op("tensor", nc.tensor.matmul, acc[a][:], lhsT=wt[:, kt, j * 128:(j + 1) * 128],
                         rhs=ht[:, kt, :], start=(kt == 0), stop=(kt == 31),
                         reads=[bw, bh], writes=[b_acc[a]], inc=(kt == 31))

            def act_blocks():
                lst = []
                for bi, base in ((0, 12), (1, 20), (2, 28)):
                    if (bi == 0 and not ENABLE_A) or (bi == 1 and not ENABLE_S):
                        continue
                    for k in range(4):
                        lst.append(("silu", szd[bi], b_szd[bi], base + k, k))
                for bi, base in ((0, 32), (1, 40), (2, 48)):
                    if (bi == 0 and not ENABLE_A) or (bi == 1 and not ENABLE_S):
                        continue
                    for k in range(8):
                        lst.append(("sigm", sgd[bi], b_sgd[bi], base + k, k))
                return lst

            b_kTd, b_qTd, b_Vd, b_uTd = Buf("kTd"), Buf("qTd"), Buf("Vd"), Buf("uTd")
            OT0 = NCT - NTT
            items = []
            for kind, dst, bdst, cb, k in act_blocks():
                for tt in range(NTT):
                    items.append((cb, OT0 + tt, ("act", kind, dst, bdst, k, tt)))
            for hd in range(4):
                for tt in range(NTT):
                    items.append((24 + hd, OT0 + tt, ("qc", hd, tt)))
            if ENABLE_A:
                for is_k in (True, False):
                    for cbl in range(4):
                        for tt in (range(NCT) if is_k else range(OT0, NCT)):
                            items.append(((4 if is_k else 0) + cbl, tt, ("qk", is_k, cbl, tt)))
                for cbl in range(4):
                    for tt in range(NCT):
                        items.append((8 + cbl, tt, ("v", cbl, tt)))
            if ENABLE_S:
                for cbl in range(4):
                    for tt in range(NCT):
                        items.append((16 + cbl, tt, ("u", cbl, tt)))

            if DBG:
                seen_k = set()
                cnt_k = {}
                keep = []
                for it in items:
                    kk = (it[2][0], it[2][1])
                    cnt_k[kk] = cnt_k.get(kk, 0) + 1
                    if cnt_k[kk] <= DBG:
                        keep.append(it)
                items = keep

            def nexta():
                a = octr[0] % 4
                octr[0] += 1
                return a

            def do_item(spec, wt, bw, ht, bh, tt):
                kind = spec[0]
                if kind == "act":
                    _, fn, dst, bdst, k, tq = spec
                    for j in range(4):
                        a = nexta()
                        o = a % 2
                        proj_fm(wt, bw, j, ht, bh, a)
                        t.op("scalar", nc.scalar.activation, out=ob32[o][:], in_=acc[a][:],
                             func=AF.Silu if fn == "silu" else AF.Sigmoid, reads=[b_acc[a]], writes=[b_ob32[o]])
                        t.dma("sync", dst[k * 4 + j, :, tq * TT:(tq + 1) * TT], ob32[o][:],
                              reads=[b_ob32[o]], writes=[bdst])
                elif kind == "qc":
                    _, hd, tq = spec
                    o = nexta() % 2
                    for j in range(4):
                        proj_fm(wt, bw, j, ht, bh, j)
                        t.op("scalar", nc.scalar.activation, out=sq[:, j, :], in_=acc[j][:], func=AF.Square,
                             reads=[b_acc[j]], writes=[b_sq])
                    for j in range(4):
                        t.op("tensor", nc.tensor.matmul, ssp[:], lhsT=onesb[:], rhs=sq[:, j, :],
                             start=(j == 0), stop=(j == 3), reads=[b_sq, b_const], writes=[b_ssp], inc=(j == 3))
                    t.op("scalar", nc.scalar.activation, out=rstd[:], in_=ssp[:], func=AF.Sqrt,
                         bias=epst[:, 0:1], scale=1.0 / 512, reads=[b_ssp, b_const], writes=[b_rstd])
                    t.op("vector", nc.vector.reciprocal, rstd[:], rstd[:], reads=[b_rstd], writes=[b_rstd])
                    for j in range(4):
                        t.op("vector", nc.vector.scalar_tensor_tensor, out=ob16[o][:, j, :], in0=acc[j][:],
                             scalar=qgct[:, j:j + 1], in1=rstd[:], op0=ALU.mult, op1=ALU.mult,
                             reads=[b_acc[j], b_g, b_rstd], writes=[b_ob16[o]])
                    t.dma("sync", qcTd[hd * 4:(hd + 1) * 4, :, tq * TT:(tq + 1) * TT].rearrange("j p t -> p j t"),
                          ob16[o][:], reads=[b_ob16[o]], writes=[b_qcTd])
                elif kind == "qk":
                    _, is_k, cbl, _tt = spec
                    banks = [nexta() for _ in range(4)]

                    def post(j):
                        a = banks[j]
                        o = a % 2
                        head = cbl * 4 + j
                        t.op("scalar", nc.scalar.activation, out=sq[:, j, :], in_=acc[a][:], func=AF.Square,
                             reads=[b_acc[a]], writes=[b_sqj[j]])
                        t.op("tensor", nc.tensor.matmul, ssp2[j % 2][:], lhsT=onesb[:], rhs=sq[:, j, :],
                             start=True, stop=True, reads=[b_sqj[j], b_const], writes=[b_ssp2[j % 2]])
                        t.op("scalar", nc.scalar.activation, out=rstd2[j % 2][:], in_=ssp2[j % 2][:], func=AF.Sqrt,
                             bias=epst[:, 0:1], scale=1.0 / 128, reads=[b_ssp2[j % 2], b_const], writes=[b_rstd2[j % 2]])
                        t.op("vector", nc.vector.reciprocal, rstd2[j % 2][:], rstd2[j % 2][:],
                             reads=[b_rstd2[j % 2]], writes=[b_rstd2[j % 2]])
                        t.op("vector", nc.vector.scalar_tensor_tensor, out=obq[a][:], in0=acc[a][:],
                             scalar=(kgat if is_k else qgat)[:, 0:1], in1=rstd2[j % 2][:], op0=ALU.mult, op1=ALU.mult,
                             reads=[b_acc[a], b_g, b_rstd2[j % 2]], writes=[b_obq[a]])
                        if is_k:
                            t.op("vector", nc.vector.tensor_reduce, out=kmT[:, head, tt * 2:(tt + 1) * 2],
                                 in_=obq[a][:].rearrange("p (b k) -> p b k", b=2), axis=AX.X, op=ALU.add,
                                 reads=[b_obq[a]], writes=[b_kmT])
                            t.dma("sync", kTd[head, :, tt * TT:(tt + 1) * TT], obq[a][:],
                                  reads=[b_obq[a]], writes=[b_kTd])
                        else:
                            tq = tt - OT0
                            t.dma("sync", qTd[head, :, tq * TT:(tq + 1) * TT], obq[a][:],
                                  reads=[b_obq[a]], writes=[b_qTd])
                    for step in range(5):
                        if step < 4:
                            proj_fm(wt, bw, step, ht, bh, banks[step])
                        if step >= 1:
                            post(step - 1)
                elif kind == "v":
                    _, cbl, _tt = spec
                    for s_ in range(4):
                        a = nexta()
                        for kt in range(32):
                            t.op("tensor", nc.tensor.matmul, acc[a][:], lhsT=ht[:, kt, s_ * 128:(s_ + 1) * 128],
                                 rhs=wt[:, kt, :], start=(kt == 0), stop=(kt == 31),
                                 reads=[bw, bh], writes=[b_acc[a]], inc=(kt == 31))
                        t.op("scalar", nc.scalar.copy, obq[a][:], acc[a][:], reads=[b_acc[a]], writes=[b_obq[a]])
                        r0 = tt * TT + s_ * 128
                        t.dma("sync", Vd[r0:r0 + 128, cbl * 512:(cbl + 1) * 512], obq[a][:],
                              reads=[b_obq[a]], writes=[b_Vd])
                elif kind == "u":
                    _, cbl, _tt = spec
                    for j in range(4):
                        a = nexta()
                        proj_fm(wt, bw, j, ht, bh, a)
                        t.op("scalar", nc.scalar.copy, obq[a][:], acc[a][:], reads=[b_acc[a]], writes=[b_obq[a]])
                        t.dma("sync", uTd[cbl * 4 + j, :, tt * TT:(tt + 1) * TT], obq[a][:],
                              reads=[b_obq[a]], writes=[b_uTd])

            obq = [sb(f"p1obq{i}", [128, TT], BF16, p1) for i in range(4)]
            b_obq = [Buf() for _ in range(4)]
            b_sqj = [Buf() for _ in range(4)]
            ssp2 = [ssp, ps("p1ssp2", [128, 512], F32, p1)]
            b_ssp2 = [b_ssp, Buf()]
            rstd2 = [rstd, sb("p1rstd2", [128, TT], F32, p1)]
            b_rstd2 = [b_rstd, Buf()]
            cur_cb = None
            nxt = load_hT(items[0][1])
            for idx, (cb, tt, spec) in enumerate(items):
                ht, bh = nxt
                if cb != cur_cb:
                    wt, bw = load_w(w_in, 32, cb * 512)
                    cur_cb = cb
                if idx + 1 < len(items):
                    nxt = load_hT(items[idx + 1][1])
                do_item(spec, wt, bw, ht, bh, tt)
            t.barrier()

        b_yTd = [Buf() for _ in range(3)]
        if ENABLE_A:
          with ExitStack() as pa:
            NB = CTX // 256
            QB0 = OFF // 256
            KT_ = [sb(f"paKT{i}", [128, CTX], BF16, pa) for i in range(2)]
            Vh = [sb(f"paVh{i}", [128, CTX // 128, 128], BF16, pa) for i in range(2)]
            QT = [sb(f"paQT{i}", [128, OWN], BF16, pa) for i in range(2)]
            b_KT, b_Vh, b_QT = [Buf(), Buf()], [Buf(), Buf()], [Buf(), Buf()]
            maskT = sb("pamaskT", [32, OWN], BF16, pa)
            Sel = sb("paSel", [32, 32, 128], BF16, pa)
            kbias = sb("pakbias", [128, 16, 64], F32, pa)
            Ftab = sb("paFtab", [128, 16, 256], F32, pa)
            Dtab = sb("paDtab", [128, 2, 256], F32, pa)
            cbias = sb("pacbias", [128, 8, 32], F32, pa)
            vbt = sb("pavbt", [128, 32], F32, pa)
            kmb = sb("pakmb", [128, 16, 32], BF16, pa)
            iot = sb("paiot", [128, 512], mybir.dt.int32, pa)
            iof = sb("paiof", [128, 512], F32, pa)
            tmpf = sb("patmpf", [128, 512], F32, pa)
            b_tab = Buf("tab")
            b_maskT = Buf("maskT")
            t.dma("sync", vbt[:], vbB[:, :], writes=[b_tab])
            t.op("gpsimd", nc.gpsimd.iota, iot[:, 0:64], pattern=[[-128, 64]], base=0, channel_multiplier=1,
                 reads=[b_tab], writes=[b_tab])
            t.op("vector", nc.vector.tensor_copy, iof[:, 0:64], iot[:, 0:64], reads=[b_tab], writes=[b_tab])
            for h in range(16):
                t.op("vector", nc.vector.tensor_scalar, out=kbias[:, h, :], in0=iof[:, 0:64], scalar1=float(SLOPES[h]),
                     scalar2=None, op0=ALU.mult, reads=[b_tab], writes=[b_tab])
            t.op("gpsimd", nc.gpsimd.iota, iot[:, 0:256], pattern=[[1, 256]], base=0, channel_multiplier=0,
                 reads=[b_tab], writes=[b_tab])
            t.op("vector", nc.vector.tensor_copy, iof[:, 0:256], iot[:, 0:256], reads=[b_tab], writes=[b_tab])
            for h in range(16):
                t.op("scalar", nc.scalar.activation, out=Ftab[:, h, :], in_=iof[:, 0:256], func=AF.Exp,
                     scale=-float(SLOPES[h]), reads=[b_tab], writes=[b_tab])
            t.op("gpsimd", nc.gpsimd.iota, iot[:, 0:512].rearrange("p (a b) -> p a b", a=2),
                 pattern=[[-128, 2], [1, 256]], base=0, channel_multiplier=-1, reads=[b_tab], writes=[b_tab])
            Dflat = Dtab[:].rearrange("p a b -> p (a b)")
            t.op("vector", nc.vector.tensor_copy, Dflat, iot[:, 0:512], reads=[b_tab], writes=[b_tab])
            t.op("vector", nc.vector.tensor_scalar, out=tmpf[:], in0=Dflat, scalar1=0.0, scalar2=1.0e6,
                 op0=ALU.is_lt, op1=ALU.mult, reads=[b_tab], writes=[b_tab])
            t.op("vector", nc.vector.tensor_tensor, out=Dflat, in0=Dflat, in1=tmpf[:], op=ALU.add,
                 reads=[b_tab], writes=[b_tab])
            t.op("vector", nc.vector.tensor_copy, Sel[:], identf[0:32, 0:32].unsqueeze(2).broadcast_to([32, 32, 128]),
                 reads=[b_const, b_tab], writes=[b_tab])
            t.op("vector", nc.vector.memset, cbias[:], -1.0e30, reads=[b_tab], writes=[b_tab])
            for qbl in range(8):
                t.op("vector", nc.vector.tensor_copy, cbias[:, qbl, 0:QB0 + qbl], vbt[:, 0:QB0 + qbl],
                     reads=[b_tab], writes=[b_tab])
            t.op("vector", nc.vector.tensor_copy, kmb[:], kmT[:], reads=[b_kmT, b_tab], writes=[b_tab])

            scpB2 = [ps(f"pascpB{i}", [128, 512], F32, pa) for i in range(2)]
            accB = [ps(f"paaccB{i}", [128, 512], F32, pa) for i in range(2)]
            dgB = [ps(f"padgB{i}", [128, 512], F32, pa) for i in range(2)]
            gps = ps("pagps", [128, 512], F32, pa)
            tps = ps("patps", [128, 1024], BF16, pa)
            scp = [scpB2[0][:, 0:256], scpB2[1][:, 0:256]]
            b_scp = [Buf(), Buf()]
            b_oacc, b_dacc, b_odg, b_ddg = [Buf(), Buf()], [Buf(), Buf()], [Buf(), Buf()], [Buf(), Buf()]
            b_gps, b_tps = Buf(), Buf()
            Gs = sb("paGs", [128, 32], F32, pa)
            m8 = sb("pam8", [128, 8], F32, pa)
            thr = sb("pathr", [128, 1], F32, pa)
            mf = sb("pamf", [128, 32], F32, pa)
            mb16 = sb("pamb16", [128, 32], BF16, pa)
            b_gs = Buf("gs")
            pT = [sb(f"papT{i}", [128, 256], BF16, pa) for i in range(2)]
            b_pT = [Buf(), Buf()]
            s32 = sb("pas32", [128, 256], F32, pa)
            b_s32 = Buf()
            num = sb("panum", [128, 256], F32, pa)
            den = sb("paden", [128, 256], F32, pa)
            b_nd = Buf()
            sza = [sb(f"pasza{i}", [128, 256], F32, pa) for i in range(2)]
            b_sza = [Buf(), Buf()]
            yo = [sb(f"payo{i}", [128, 256], BF16, pa) for i in range(2)]
            b_yo = [Buf(), Buf()]
            dsum = [[sb(f"padsum{i}{j}", [128, 256], F32, pa) for j in range(2)] for i in range(2)]
            b_dsum = [[Buf(), Buf()], [Buf(), Buf()]]
            onesf32 = sb("paonesf32", [128, 128], F32, pa)
            t.op("vector", nc.vector.memset, onesf32[:], 1.0, reads=[b_tab], writes=[b_tab])

            def load_head(h):
                hi = h % 2
                t.dma("sync", KT_[hi][:, 0:CTX // 2], kTd[h, :, 0:CTX // 2], reads=[b_kTd], writes=[b_KT[hi]])
                t.dma("sync", KT_[hi][:, CTX // 2:CTX], kTd[h, :, CTX // 2:CTX], reads=[b_kTd], writes=[b_KT[hi]])
                vsrc = Vd[:, h * 128:(h + 1) * 128].rearrange("(kt p) d -> p kt d", p=128)
                for q4 in range(4):
                    t.dma("sync", Vh[hi][:, q4 * 16:(q4 + 1) * 16, :], vsrc[:, q4 * 16:(q4 + 1) * 16, :],
                          reads=[b_Vd], writes=[b_Vh[hi]])
                t.dma("sync", QT[hi][:], qTd[h, :, :], reads=[b_qTd], writes=[b_QT[hi]])

            pc_ = 0
            load_head(0)
            NH = DBG if DBG else 16
            for h in range(NH):
                hi = h % 2
                for qt in range(OWN // 128):
                    qsl = slice(qt * 128, (qt + 1) * 128)
                    t.op("tensor", nc.tensor.matmul, gps[:, 0:32], lhsT=QT[hi][:, qsl], rhs=kmb[:, h, :],
                         start=True, stop=True, reads=[b_QT[hi], b_tab], writes=[b_gps])
                    t.op("vector", nc.vector.tensor_tensor, out=Gs[:], in0=gps[:, 0:32], in1=cbias[:, qt // 2, :],
                         op=ALU.add, reads=[b_gps, b_tab], writes=[b_gs])
                    t.op("vector", nc.vector.max, out=m8[:], in_=Gs[:], reads=[b_gs], writes=[b_gs])
                    t.op("vector", nc.vector.tensor_scalar, out=thr[:], in0=m8[:, 2:3], scalar1=-1.0e29, scalar2=None,
                         op0=ALU.max, reads=[b_gs], writes=[b_gs])
                    t.op("vector", nc.vector.tensor_scalar, out=mf[:], in0=Gs[:], scalar1=thr[:, 0:1], scalar2=None,
                         op0=ALU.is_ge, reads=[b_gs], writes=[b_gs])
                    t.op("vector", nc.vector.tensor_scalar, out=mb16[:], in0=mf[:], scalar1=-1.0, scalar2=30000.0,
                         op0=ALU.add, op1=ALU.mult, reads=[b_gs], writes=[b_gs])
                    t.op("tensor", nc.tensor.transpose, tps[0:32, 0:128], mb16[:], ident[:],
                         reads=[b_gs, b_const], writes=[b_tps])
                    t.op("vector", nc.vector.tensor_copy, maskT[:, qsl], tps[0:32, 0:128],
                         reads=[b_tps], writes=[b_maskT])
                if h + 1 < NH:
                    load_head(h + 1)
                jobs = []
                for qbl in range(DBG + 1 if DBG else 8):
                    qb = QB0 + qbl
                    for ktile in range(2 * qb):
                        jobs.append((qbl, "past", ktile, ktile == 0, ktile == 2 * qb - 1))
                    for kt2 in range(2):
                        jobs.append((qbl, "diag", 2 * qb + kt2, kt2 == 0, kt2 == 1))

                def stage1(job):
                    nonlocal pc_
                    qbl, kind, ktile, first, last = job
                    qb = QB0 + qbl
                    qsl = slice(qbl * 256, qbl * 256 + 256)
                    pi = pc_ % 2
                    pc_ += 1
                    if kind == "past":
                        if first:
                            oi = qbl % 2
                            t.dma("sync", sza[oi][:], szd[0][h, :, qsl], reads=[b_szd[0]], writes=[b_sza[oi]])
                        n = ktile // 2
                        t.op("tensor", nc.tensor.matmul, scp[pi], lhsT=KT_[hi][:, ktile * 128:(ktile + 1) * 128],
                             rhs=QT[hi][:, qsl], start=True, stop=False,
                             reads=[b_KT[hi], b_QT[hi]], writes=[b_scp[pi]], inc=False)
                        t.op("tensor", nc.tensor.matmul, scp[pi], lhsT=Sel[:, n, :], rhs=maskT[:, qsl],
                             start=False, stop=True, reads=[b_tab, b_maskT], writes=[b_scp[pi]])
                        m = (2 * qb) - ktile
                        t.op("scalar", nc.scalar.activation, out=pT[pi][:], in_=scp[pi], func=AF.Exp,
                             bias=kbias[:, h, m:m + 1], scale=1.0, reads=[b_scp[pi], b_tab], writes=[b_pT[pi]])
                    else:
                        kt2 = ktile - 2 * qb
                        t.op("tensor", nc.tensor.matmul, scp[pi], lhsT=KT_[hi][:, ktile * 128:(ktile + 1) * 128],
                             rhs=QT[hi][:, qsl], start=True, stop=True,
                             reads=[b_KT[hi], b_QT[hi]], writes=[b_scp[pi]])
                        t.op("vector", nc.vector.scalar_tensor_tensor, out=s32[:], in0=Dtab[:, kt2, :],
                             scalar=-float(SLOPES[h]), in1=scp[pi], op0=ALU.mult, op1=ALU.add,
                             reads=[b_tab, b_scp[pi]], writes=[b_s32])
                        t.op("scalar", nc.scalar.activation, out=pT[pi][:], in_=s32[:], func=AF.Exp,
                             reads=[b_s32], writes=[b_pT[pi]])
                    return pi

                def stage2(job, pi):
                    qbl, kind, ktile, first, last = job
                    s_ = qbl % 2
                    qsl = slice(qbl * 256, qbl * 256 + 256)
                    if kind == "past":
                        o_t, d_t, bo, bd = accB[s_][:, 0:256], dgB[s_][:, 0:256], b_oacc[s_], b_dacc[s_]
                    else:
                        o_t, d_t, bo, bd = accB[s_][:, 256:512], dgB[s_][:, 256:512], b_odg[s_], b_ddg[s_]
                    t.op("tensor", nc.tensor.matmul, o_t, lhsT=Vh[hi][:, ktile, :], rhs=pT[pi][:],
                         start=first, stop=last, reads=[b_Vh[hi], b_pT[pi]], writes=[bo], inc=last)
                    di = 0 if kind == "past" else 1
                    if first:
                        t.op("vector", nc.vector.tensor_copy, dsum[s_][di][:], pT[pi][:],
                             reads=[b_pT[pi]], writes=[b_dsum[s_][di]])
                    else:
                        t.op("vector", nc.vector.tensor_tensor, out=dsum[s_][di][:], in0=dsum[s_][di][:], in1=pT[pi][:],
                             op=ALU.add, reads=[b_pT[pi], b_dsum[s_][di]], writes=[b_dsum[s_][di]])
                    if last:
                        t.op("tensor", nc.tensor.matmul, d_t, lhsT=onesf32[:], rhs=dsum[s_][di][:],
                             start=True, stop=True, reads=[b_tab, b_dsum[s_][di]], writes=[bd])
                    if kind == "diag" and last:
                        oi = qbl % 2
                        oa, da, og, dg = accB[s_][:, 0:256], dgB[s_][:, 0:256], accB[s_][:, 256:512], dgB[s_][:, 256:512]
                        V_ = lambda *a, r=(), w=(), **k: t.op("vector", nc.vector.tensor_tensor, *a, reads=r, writes=w, **k)
                        V_(out=num[:], in0=oa, in1=Ftab[:, h, :], op=ALU.mult,
                           r=[b_oacc[s_], b_odg[s_], b_dacc[s_], b_ddg[s_], b_tab], w=[b_nd])
                        V_(out=num[:], in0=og, in1=num[:], op=ALU.add, r=[b_odg[s_], b_nd], w=[b_nd])
                        V_(out=den[:], in0=da, in1=Ftab[:, h, :], op=ALU.mult, r=[b_dacc[s_], b_tab], w=[b_nd])
                        V_(out=den[:], in0=dg, in1=den[:], op=ALU.add, r=[b_ddg[s_], b_nd], w=[b_nd])
                        t.op("vector", nc.vector.reciprocal, den[:], den[:], reads=[b_nd], writes=[b_nd])
                        V_(out=num[:], in0=num[:], in1=den[:], op=ALU.mult, r=[b_nd], w=[b_nd])
                        V_(out=yo[oi][:], in0=num[:], in1=sza[oi][:], op=ALU.mult, r=[b_nd, b_sza[oi]], w=[b_yo[oi]])
                        t.dma("sync", yTd[0][h, :, qsl], yo[oi][:], reads=[b_yo[oi]], writes=[b_yTd[0]])

                prev = None
                for job in jobs + [None]:
                    cur = None
                    if job is not None:
                        cur = (job, stage1(job))
                    if prev is not None:
                        stage2(*prev)
                    prev = cur
            t.barrier()

        if ENABLE_S:
          with ExitStack() as pS:
            TWO_PI = 2.0 * math.pi
            V = lambda fn, *a, r=(), w=(), **k: t.op("vector", fn, *a, reads=r, writes=w, **k)
            A_ = lambda *a, r=(), w=(), **k: t.op("scalar", nc.scalar.activation, *a, reads=r, writes=w, **k)
            tt_, ts_, stt_ = nc.vector.tensor_tensor, nc.vector.tensor_scalar, nc.vector.scalar_tensor_tensor

            def sincos(arg, cos_o, sin_o, tmp, tmpi, b):
                for dst, shift in ((sin_o, 0.0), (cos_o, math.pi / 2)):
                    V(ts_, out=tmp, in0=arg, scalar1=shift, scalar2=1.0 / TWO_PI, op0=ALU.add, op1=ALU.mult, r=[b], w=[b])
                    V(nc.vector.tensor_copy, tmpi, tmp, r=[b], w=[b])
                    V(nc.vector.tensor_copy, tmp, tmpi, r=[b], w=[b])
                    V(stt_, out=tmp, in0=tmp, scalar=-TWO_PI, in1=arg, op0=ALU.mult, op1=ALU.add, r=[b], w=[b])
                    if shift:
                        V(ts_, out=tmp, in0=tmp, scalar1=shift, scalar2=None, op0=ALU.add, r=[b], w=[b])
                    V(ts_, out=dst, in0=tmp, scalar1=math.pi, scalar2=TWO_PI, op0=ALU.is_gt, op1=ALU.mult, r=[b], w=[b])
                    V(tt_, out=tmp, in0=tmp, in1=dst, op=ALU.subtract, r=[b], w=[b])
                    V(ts_, out=dst, in0=tmp, scalar1=-math.pi, scalar2=TWO_PI, op0=ALU.is_lt, op1=ALU.mult, r=[b], w=[b])
                    V(tt_, out=tmp, in0=tmp, in1=dst, op=ALU.add, r=[b], w=[b])
                    V(ts_, out=tmp, in0=tmp, scalar1=-3.14159, scalar2=3.14159, op0=ALU.max, op1=ALU.min, r=[b], w=[b])
                    A_(out=dst, in_=tmp, func=AF.Sin, r=[b], w=[b])

            rcol = sb("srcol", [128, 64], F32, pS)
            thcol = sb("sthcol", [128, 64], F32, pS)
            dcol = sb("sdcol", [128, 16], F32, pS)
            bglu = sb("sbglu", [128, 16], F32, pS)
            pbc = pS.enter_context(ExitStack())
            BbT = [sb(f"sBbT{i}", [128, 64 * 128], BF16, pbc) for i in range(2)]
            Cb = [sb(f"sCb{i}", [128, 64 * 128], BF16, pbc) for i in range(2)]
            b_par = Buf("spar")
            with ExitStack() as pp:
                CW = 2048
                nm = ["A", "I", "L", "T1", "T2", "T3", "T4", "T5", "T6", "Br", "Bi"]
                T = {n: sb("sp" + n, [128, CW], F32, pp) for n in nm}
                Ti = sb("spTi", [128, CW], mybir.dt.int32, pp)
                for c in range(8192 // CW):
                    cs = slice(c * CW, (c + 1) * CW)
                    for n, src in (("A", s_arep), ("I", s_irep), ("L", s_lrep), ("Br", s_bre), ("Bi", s_bim)):
                        t.dma("sync", T[n][:], src[:, cs], writes=[b_par])
                    g = lambda n: T[n][:]
                    A_(out=g("L"), in_=g("L"), func=AF.Exp, r=[b_par], w=[b_par])
                    V(tt_, out=g("T1"), in0=g("L"), in1=g("A"), op=ALU.mult, r=[b_par], w=[b_par])
                    A_(out=g("T1"), in_=g("T1"), func=AF.Exp, r=[b_par], w=[b_par])
                    V(tt_, out=g("T2"), in0=g("L"), in1=g("I"), op=ALU.mult, r=[b_par], w=[b_par])
                    sincos(g("T2"), g("T3"), g("T4"), g("T5"), Ti[:], b_par)
                    V(tt_, out=g("T3"), in0=g("T1"), in1=g("T3"), op=ALU.mult, r=[b_par], w=[b_par])
                    V(tt_, out=g("T4"), in0=g("T1"), in1=g("T4"), op=ALU.mult, r=[b_par], w=[b_par])
                    V(tt_, out=g("T5"), in0=g("A"), in1=g("A"), op=ALU.mult, r=[b_par], w=[b_par])
                    V(tt_, out=g("T6"), in0=g("I"), in1=g("I"), op=ALU.mult, r=[b_par], w=[b_par])
                    V(tt_, out=g("T5"), in0=g("T5"), in1=g("T6"), op=ALU.add, r=[b_par], w=[b_par])
                    V(nc.vector.reciprocal, g("T5"), g("T5"), r=[b_par], w=[b_par])
                    V(ts_, out=g("T3"), in0=g("T3"), scalar1=-1.0, scalar2=None, op0=ALU.add, r=[b_par], w=[b_par])
                    V(tt_, out=g("T6"), in0=g("T3"), in1=g("A"), op=ALU.mult, r=[b_par], w=[b_par])
                    V(tt_, out=g("T2"), in0=g("T4"), in1=g("I"), op=ALU.mult, r=[b_par], w=[b_par])
                    V(tt_, out=g("T6"), in0=g("T6"), in1=g("T2"), op=ALU.add, r=[b_par], w=[b_par])
                    V(tt_, out=g("T6"), in0=g("T6"), in1=g("T5"), op=ALU.mult, r=[b_par], w=[b_par])
                    V(tt_, out=g("T2"), in0=g("T4"), in1=g("A"), op=ALU.mult, r=[b_par], w=[b_par])
                    V(tt_, out=g("T1"), in0=g("T3"), in1=g("I"), op=ALU.mult, r=[b_par], w=[b_par])
                    V(tt_, out=g("T2"), in0=g("T2"), in1=g("T1"), op=ALU.subtract, r=[b_par], w=[b_par])
                    V(tt_, out=g("T2"), in0=g("T2"), in1=g("T5"), op=ALU.mult, r=[b_par], w=[b_par])
                    V(tt_, out=g("T1"), in0=g("Br"), in1=g("T6"), op=ALU.mult, r=[b_par], w=[b_par])
                    V(tt_, out=g("T3"), in0=g("Bi"), in1=g("T2"), op=ALU.mult, r=[b_par], w=[b_par])
                    V(tt_, out=BbT[0][:, cs], in0=g("T1"), in1=g("T3"), op=ALU.subtract, r=[b_par], w=[b_par])
                    V(tt_, out=g("T1"), in0=g("Br"), in1=g("T2"), op=ALU.mult, r=[b_par], w=[b_par])
                    V(tt_, out=g("T3"), in0=g("Bi"), in1=g("T6"), op=ALU.mult, r=[b_par], w=[b_par])
                    V(tt_, out=BbT[1][:, cs], in0=g("T1"), in1=g("T3"), op=ALU.add, r=[b_par], w=[b_par])
                    t.dma("sync", T["A"][:], s_cre[:, cs], writes=[b_par])
                    t.dma("sync", T["I"][:], s_cim[:, cs], writes=[b_par])
                    V(nc.vector.tensor_copy, Cb[0][:, cs], g("A"), r=[b_par], w=[b_par])
                    V(ts_, out=Cb[1][:, cs], in0=g("I"), scalar1=-1.0, scalar2=None, op0=ALU.mult, r=[b_par], w=[b_par])
                t.dma("sync", T["A"][:, 0:64], s_acol[:, :], writes=[b_par])
                t.dma("sync", T["I"][:, 0:64], s_icol[:, :], writes=[b_par])
                t.dma("sync", T["L"][:, 0:64], s_lcol[:, :], writes=[b_par])
                t.dma("sync", dcol[:], s_dcol[:, :], writes=[b_par])
                t.dma("sync", bglu[:], s_bglu[:, :], writes=[b_par])
                A_(out=T["L"][:, 0:64], in_=T["L"][:, 0:64], func=AF.Exp, r=[b_par], w=[b_par])
                V(tt_, out=T["T1"][:, 0:64], in0=T["L"][:, 0:64], in1=T["A"][:, 0:64], op=ALU.mult, r=[b_par], w=[b_par])
                A_(out=rcol[:], in_=T["T1"][:, 0:64], func=AF.Exp, r=[b_par], w=[b_par])
                V(tt_, out=thcol[:], in0=T["L"][:, 0:64], in1=T["I"][:, 0:64], op=ALU.mult, r=[b_par], w=[b_par])
                t.barrier()

            b_y1T = Buf("y1T")
            b_y1d = Buf("y1d")
            b_y1bd = Buf("y1bd")
            with ExitStack() as pm:
                SW = 512
                NSEG = CTX // SW
                SEG0 = OFF // SW
                UT = [sb(f"sUT{i}", [128, CTX], BF16, pm) for i in range(2)]
                b_UT = [Buf(), Buf()]
                iot = sb("siot", [128, SW + 1], mybir.dt.int32, pm)
                tauf = sb("stauf", [128, SW + 1], F32, pm)
                arg = sb("sarg", [128, SW + 1], F32, pm)
                tmp = sb("stmp", [128, SW + 1], F32, pm)
                tmpi = sb("stmpi", [128, SW + 1], mybir.dt.int32, pm)
                cosT = sb("scosT", [128, SW + 1], F32, pm)
                sinT = sb("ssinT", [128, SW + 1], F32, pm)
                Rb = sb("sRb", [128, SW], F32, pm)
                onesf = sb("sonesf", [128, SW], F32, pm)
                b_tb = Buf("stab")
                t1 = [sb(f"st1_{i}", [128, SW], F32, pm) for i in range(2)]
                t2 = [sb(f"st2_{i}", [128, SW], F32, pm) for i in range(2)]
                bre = [sb(f"sbre{i}", [128, SW], F32, pm) for i in range(2)]
                bim = [sb(f"sbim{i}", [128, SW], F32, pm) for i in range(2)]
                wre = [sb(f"swre{i}", [128, SW], F32, pm) for i in range(2)]
                wim = [sb(f"swim{i}", [128, SW], F32, pm) for i in range(2)]
                ini = [sb(f"sini{i}", [128, 4], F32, pm) for i in range(2)]
                xre = [sb(f"sxre{i}", [128, SW], BF16, pm) for i in range(2)]
                xim = [sb(f"sxim{i}", [128, SW], BF16, pm) for i in range(2)]
                b_seg = [Buf(), Buf()]
                b_seg2 = [Buf(), Buf()]
                b_bre = [Buf(), Buf()]
                b_bim = [Buf(), Buf()]
                t3 = [sb(f"st3_{i}", [128, SW], F32, pm) for i in range(2)]
                t4 = [sb(f"st4_{i}", [128, SW], F32, pm) for i in range(2)]
                t5 = sb("st5", [128, SW], F32, pm)
                t6 = sb("st6", [128, SW], F32, pm)
                b_t56 = Buf()
                if S_USE_POOL:
                    P = lambda r=(), w=(), **k: t.op("gpsimd", nc.gpsimd.tensor_tensor, reads=r, writes=w, **k)
                else:
                    P = lambda r=(), w=(), **k: t.op("vector", nc.vector.tensor_tensor, reads=r, writes=w, **k)
                b_w_ = [Buf(), Buf()]
                b_ini = [Buf(), Buf()]
                b_x = [Buf(), Buf()]
                bups = [[ps(f"sbups{i}{j}", [128, 512], F32, pm) for j in range(2)] for i in range(2)]
                b_bups = [Buf(), Buf()]
                yacc = [ps(f"syacc{i}", [128, 512], F32, pm) for i in range(4)]
                b_yacc = [Buf() for _ in range(4)]
                y32 = sb("sy32", [128, SW], F32, pm)
                g1 = sb("sg1", [128, SW], F32, pm)
                y16 = sb("sy16", [128, SW], BF16, pm)
                b_y32 = Buf()
                b_y16 = Buf()
                t.op("gpsimd", nc.gpsimd.iota, iot[:], pattern=[[1, SW + 1]], base=0, channel_multiplier=0, writes=[b_tb])
                V(nc.vector.tensor_copy, tauf[:], iot[:], r=[b_tb], w=[b_tb])
                V(nc.vector.memset, onesf[:], 1.0, r=[b_tb], w=[b_tb])
                sc = 0
                def load_UT(c8):
                    u_ = c8 % 2
                    t.dma("sync", UT[u_][:, 0:CTX // 2], uTd[c8, :, 0:CTX // 2], reads=[b_uTd], writes=[b_UT[u_]])
                    t.dma("sync", UT[u_][:, CTX // 2:CTX], uTd[c8, :, CTX // 2:CTX], reads=[b_uTd], writes=[b_UT[u_]])
                load_UT(0)
                NC8 = DBG if DBG else 16
                for ct8 in range(NC8):
                    ui = ct8 % 2
                    if ct8 + 1 < NC8:
                        load_UT(ct8 + 1)
                    for p4 in range(4):
                        pr = ct8 * 4 + p4
                        psl = slice(pr * 128, (pr + 1) * 128)
                        V(ts_, out=arg[:], in0=tauf[:], scalar1=thcol[:, pr:pr + 1], scalar2=None, op0=ALU.mult,
                          r=[b_tb, b_par], w=[b_tb])
                        sincos(arg[:], cosT[:], sinT[:], tmp[:], tmpi[:], b_tb)
                        V(ts_, out=Rb[:], in0=onesf[:], scalar1=rcol[:, pr:pr + 1], scalar2=None, op0=ALU.mult,
                          r=[b_tb, b_par], w=[b_tb])
                        for seg in range(NSEG):
                            i = sc % 2
                            sc += 1
                            tsl = slice(seg * SW, (seg + 1) * SW)
                            for ri in range(2):
                                t.op("tensor", nc.tensor.matmul, bups[i][ri][:], lhsT=BbT[ri][:, psl], rhs=UT[ui][:, tsl],
                                     start=True, stop=True, reads=[b_par, b_UT[ui]], writes=[b_bups[i]], inc=(ri == 1))
                            c_, s_ = cosT[:, 0:SW], sinT[:, 0:SW]
                            V(tt_, out=t1[i][:], in0=bups[i][0][:], in1=c_, op=ALU.mult, r=[b_bups[i], b_tb], w=[b_seg[i]])
                            V(tt_, out=t2[i][:], in0=bups[i][1][:], in1=s_, op=ALU.mult, r=[b_bups[i], b_tb], w=[b_seg[i]])
                            P(out=bre[i][:], in0=t1[i][:], in1=t2[i][:], op=ALU.add, r=[b_seg[i]], w=[b_bre[i]])
                            V(tt_, out=t3[i][:], in0=bups[i][1][:], in1=c_, op=ALU.mult, r=[b_bups[i], b_tb], w=[b_seg2[i]])
                            V(tt_, out=t4[i][:], in0=bups[i][0][:], in1=s_, op=ALU.mult, r=[b_bups[i], b_tb], w=[b_seg2[i]])
                            P(out=bim[i][:], in0=t3[i][:], in1=t4[i][:], op=ALU.subtract, r=[b_seg2[i]], w=[b_bim[i]])
                            if seg == 0:
                                i_re, i_im = 0.0, 0.0
                                rd = [b_tb]
                            else:
                                i_re, i_im = ini[i][:, 0:1], ini[i][:, 1:2]
                                rd = [b_tb, b_ini[i]]
                            V(nc.vector.tensor_tensor_scan, out=wre[i][:], data0=Rb[:], data1=bre[i][:], initial=i_re,
                              op0=ALU.mult, op1=ALU.add, r=rd + [b_bre[i]], w=[b_w_[i]])
                            V(nc.vector.tensor_tensor_scan, out=wim[i][:], data0=Rb[:], data1=bim[i][:], initial=i_im,
                              op0=ALU.mult, op1=ALU.add, r=rd + [b_bim[i]], w=[b_w_[i]])
                            if seg < NSEG - 1:
                                j = 1 - i
                                Ec, Es = cosT[:, SW:SW + 1], sinT[:, SW:SW + 1]
                                lr, li = wre[i][:, SW - 1:SW], wim[i][:, SW - 1:SW]
                                V(ts_, out=ini[j][:, 2:3], in0=li, scalar1=Es, scalar2=None, op0=ALU.mult,
                                  r=[b_w_[i], b_tb], w=[b_ini[j]])
                                V(stt_, out=ini[j][:, 0:1], in0=lr, scalar=Ec, in1=ini[j][:, 2:3], op0=ALU.mult, op1=ALU.subtract,
                                  r=[b_w_[i], b_tb, b_ini[j]], w=[b_ini[j]])
                                V(ts_, out=ini[j][:, 3:4], in0=li, scalar1=Ec, scalar2=None, op0=ALU.mult,
                                  r=[b_w_[i], b_tb, b_ini[j]], w=[b_ini[j]])
                                V(stt_, out=ini[j][:, 1:2], in0=lr, scalar=Es, in1=ini[j][:, 3:4], op0=ALU.mult, op1=ALU.add,
                                  r=[b_w_[i], b_tb, b_ini[j]], w=[b_ini[j]])
                            if seg >= SEG0:
                                so = seg - SEG0
                                P(out=t5[:], in0=wre[i][:], in1=c_, op=ALU.mult, r=[b_w_[i], b_tb], w=[b_t56])
                                P(out=t6[:], in0=wim[i][:], in1=s_, op=ALU.mult, r=[b_w_[i], b_tb], w=[b_t56])
                                P(out=xre[i][:], in0=t5[:], in1=t6[:], op=ALU.subtract, r=[b_t56], w=[b_x[i]])
                                P(out=t5[:], in0=wre[i][:], in1=s_, op=ALU.mult, r=[b_w_[i], b_tb], w=[b_t56])
                                P(out=t6[:], in0=wim[i][:], in1=c_, op=ALU.mult, r=[b_w_[i], b_tb], w=[b_t56])
                                P(out=xim[i][:], in0=t5[:], in1=t6[:], op=ALU.add, r=[b_t56], w=[b_x[i]])
                                t.op("tensor", nc.tensor.matmul, yacc[so][:], lhsT=Cb[0][:, psl], rhs=xre[i][:],
                                     start=(p4 == 0), stop=False, reads=[b_par, b_x[i]], writes=[b_yacc[so]], inc=False)
                                t.op("tensor", nc.tensor.matmul, yacc[so][:], lhsT=Cb[1][:, psl], rhs=xim[i][:],
                                     start=False, stop=(p4 == 3), reads=[b_par, b_x[i]], writes=[b_yacc[so]], inc=True)
                    for so in range(NSEG - SEG0):
                        tsl = slice(OFF + so * SW, OFF + (so + 1) * SW)
                        osl = slice(so * SW, (so + 1) * SW)
                        V(stt_, out=y32[:], in0=UT[ui][:, tsl], scalar=dcol[:, ct8:ct8 + 1], in1=yacc[so][:],
                          op0=ALU.mult, op1=ALU.add, r=[b_UT[ui], b_par, b_yacc[so]], w=[b_y32])
                        V(tt_, out=g1[:], in0=y32[:], in1=y32[:], op=ALU.mult, r=[b_y32], w=[b_y32])
                        V(ts_, out=g1[:], in0=g1[:], scalar1=0.044715, scalar2=1.0, op0=ALU.mult, op1=ALU.add, r=[b_y32], w=[b_y32])
                        V(tt_, out=g1[:], in0=g1[:], in1=y32[:], op=ALU.mult, r=[b_y32], w=[b_y32])
                        A_(out=g1[:], in_=g1[:], func=AF.Sigmoid, scale=1.5957691216057308, r=[b_y32], w=[b_y32])
                        V(tt_, out=y32[:], in0=y32[:], in1=g1[:], op=ALU.mult, r=[b_y32], w=[b_y32])
                        V(nc.vector.tensor_copy, y16[:], y32[:], r=[b_y32], w=[b_y16])
                        t.dma("sync", y1bd[ct8, :, osl], y16[:], reads=[b_y16], writes=[b_y1bd])
                        t.dma("sync", y1d[ct8, :, osl], y32[:], reads=[b_y32], writes=[b_y1d])
                t.barrier()
            pbc.close()
            with ExitStack() as pg:
                mk_w(pg)
                y1T = sb("sy1T", [128, 16, OWN], BF16, pg)
                for q4 in range(4):
                    t.dma("sync", y1T[:, q4 * 4:(q4 + 1) * 4, :], y1bd[q4 * 4:(q4 + 1) * 4, :, :].rearrange("c p t -> p c t"),
                          reads=[b_y1bd], writes=[b_y1T])
                gacc = [ps(f"sgacc{i}", [128, 512], F32, pg) for i in range(2)]
                b_gacc = [Buf(), Buf()]
                sgl = [sb(f"ssgl{i}", [128, 512], F32, pg) for i in range(2)]
                y1f = [sb(f"sy1f{i}", [128, 512], F32, pg) for i in range(2)]
                szs = [sb(f"sszs{i}", [128, 512], F32, pg) for i in range(2)]
                yo = [sb(f"ssyo{i}", [128, 512], BF16, pg) for i in range(2)]
                b_sgl, b_y1f, b_szs, b_yo = [Buf(), Buf()], [Buf(), Buf()], [Buf(), Buf()], [Buf(), Buf()]
                gc = 0
                for cb in range(1 if DBG else 4):
                    wt, bw = load_w(w_glu, 16, cb * 512)
                    for j in range(1 if DBG else 4):
                        f = cb * 4 + j
                        for tt in range(1 if DBG else NTT):
                            i = gc % 2
                            gc += 1
                            tsl = slice(tt * TT, (tt + 1) * TT)
                            t.dma("sync", y1f[i][:], y1d[f, :, tsl], reads=[b_y1d], writes=[b_y1f[i]])
                            t.dma("sync", szs[i][:], szd[1][f, :, tsl], reads=[b_szd[1]], writes=[b_szs[i]])
                            for ct in range(16):
                                t.op("tensor", nc.tensor.matmul, gacc[i][:], lhsT=wt[:, ct, j * 128:(j + 1) * 128],
                                     rhs=y1T[:, ct, tsl], start=(ct == 0), stop=(ct == 15),
                                     reads=[bw, b_y1T], writes=[b_gacc[i]], inc=(ct == 15))
                            A_(out=sgl[i][:], in_=gacc[i][:], func=AF.Sigmoid, bias=bglu[:, f:f + 1], scale=1.0,
                               r=[b_gacc[i], b_par], w=[b_sgl[i]])
                            V(tt_, out=sgl[i][:], in0=sgl[i][:], in1=y1f[i][:], op=ALU.mult, r=[b_sgl[i], b_y1f[i]], w=[b_sgl[i]])
                            V(tt_, out=yo[i][:], in0=sgl[i][:], in1=szs[i][:], op=ALU.mult, r=[b_sgl[i], b_szs[i]], w=[b_yo[i]])
                            t.dma("sync", yTd[1][f, :, tsl], yo[i][:], reads=[b_yo[i]], writes=[b_yTd[1]])
                t.barrier()

        with ExitStack() as pc2:
            qc = [sb(f"c2qc{i}", [128, 4, TT], BF16, pc2) for i in range(2)]
            szt = [sb(f"c2sz{i}", [128, 4, TT], F32, pc2) for i in range(2)]
            b_qc = [Buf(), Buf()]
            b_szt = [Buf(), Buf()]
            scp = [ps(f"c2scp{i}", [128, 512], F32, pc2) for i in range(2)]
            denp = ps("c2denp", [128, 512], F32, pc2)
            op_ = [ps(f"c2op{i}", [128, 512], F32, pc2) for i in range(4)]
            b_scp = [Buf(), Buf()]
            b_denp = Buf()
            b_op = [Buf() for _ in range(4)]
            pT = sb("c2pT", [128, 2, TT], BF16, pc2)
            rden = sb("c2rden", [128, TT], F32, pc2)
            otmp = sb("c2otmp", [128, TT], F32, pc2)
            yo = [sb(f"c2yo{i}", [128, 4, TT], BF16, pc2) for i in range(2)]
            b_pT, b_rden, b_otmp = Buf(), Buf(), Buf()
            b_yo = [Buf(), Buf()]
            it = 0
            for tt in range(1 if DBG else NTT):
                for hd in range(1 if DBG else 4):
                    i = it % 2
                    it += 1
                    tsl = slice(tt * TT, (tt + 1) * TT)
                    t.dma("sync", qc[i][:], qcTd[hd * 4:(hd + 1) * 4, :, tsl].rearrange("j p t -> p j t"),
                          reads=[b_qcTd], writes=[b_qc[i]])
                    t.dma("sync", szt[i][:], szd[2][hd * 4:(hd + 1) * 4, :, tsl].rearrange("j p t -> p j t"),
                          reads=[b_szd[2]], writes=[b_szt[i]])
                    for mt in range(2):
                        for j in range(4):
                            t.op("tensor", nc.tensor.matmul, scp[mt][:],
                                 lhsT=KcT[:, hd * 4 + j, mt * 128:(mt + 1) * 128], rhs=qc[i][:, j, :],
                                 start=(j == 0), stop=(j == 3), reads=[b_KcT, b_qc[i]], writes=[b_scp[mt]],
                                 inc=(j == 3))
                        t.op("scalar", nc.scalar.activation, out=pT[:, mt, :], in_=scp[mt][:], func=AF.Exp,
                             reads=[b_scp[mt]], writes=[b_pT])
                    for mt in range(2):
                        t.op("tensor", nc.tensor.matmul, denp[:], lhsT=onesb[:], rhs=pT[:, mt, :],
                             start=(mt == 0), stop=(mt == 1), reads=[b_pT, b_const], writes=[b_denp], inc=(mt == 1))
                    t.op("vector", nc.vector.reciprocal, rden[:], denp[:], reads=[b_denp], writes=[b_rden])
                    for j in range(4):
                        for mt in range(2):
                            t.op("tensor", nc.tensor.matmul, op_[j][:],
                                 lhsT=Vc[:, mt, hd * 512 + j * 128: hd * 512 + (j + 1) * 128], rhs=pT[:, mt, :],
                                 start=(mt == 0), stop=(mt == 1), reads=[b_Vc, b_pT], writes=[b_op[j]], inc=(mt == 1))
                        t.op("vector", nc.vector.tensor_tensor, out=otmp[:], in0=op_[j][:], in1=rden[:], op=ALU.mult,
                             reads=[b_op[j], b_rden], writes=[b_otmp])
                        t.op("vector", nc.vector.tensor_tensor, out=yo[i][:, j, :], in0=otmp[:], in1=szt[i][:, j, :],
                             op=ALU.mult, reads=[b_otmp, b_szt[i]], writes=[b_yo[i]])
                    t.dma("sync", yTd[2][hd * 4:(hd + 1) * 4, :, tsl].rearrange("j p t -> p j t"), yo[i][:],
                          reads=[b_yo[i]], writes=[b_yTd[2]])
            t.barrier()

        b_out = Buf("out")
        with ExitStack() as pf:
            mk_w(pf)
            yT = [sb(f"pfyT{i}", [128, 16, FT], BF16, pf) for i in range(1)]
            b_yT = [Buf()]
            sg = [sb(f"pfsg{i}", [128, 4, FT], F32, pf) for i in range(1)]
            b_sg = [Buf()]
            mrg32 = sb("pfmrg32", [128, 32, FT], F32, pf) if (ENABLE_A or ENABLE_S) else None
            mrgT = sb("pfmrgT", [128, 32, FT], BF16, pf)
            b_m32 = [Buf() for _ in range(32)]
            b_mT = Buf()
            acc = [ps(f"pfacc{i}", [128, 512], F32, pf) for i in range(4)]
            b_acc = [Buf() for _ in range(4)]
            oo = [sb(f"pfoo{i}", [128, 512], F32, pf) for i in range(2)]
            b_oo = [Buf(), Buf()]
            brs = [b for b in range(3) if (b == 0 and ENABLE_A) or (b == 1 and ENABLE_S) or b == 2]
            ctr = 0
            yc = 0
            for tt in range(1 if DBG else OWN // FT):
                tsl = slice(tt * FT, (tt + 1) * FT)
                for bi, b in enumerate(brs):
                    yi = 0
                    yc += 1
                    t.dma("sync", yT[yi][:], yTd[b][:, :, tsl].rearrange("c p t -> p c t"),
                          reads=[b_yTd[b]], writes=[b_yT[yi]])
                    for fb in range(8):
                        wt, bw = load_w(w_br[b], 16, fb * 512)
                        si = 0
                        t.dma("sync", sg[si][:], sgd[b][fb * 4:(fb + 1) * 4, :, tsl].rearrange("j p t -> p j t"),
                              reads=[b_sgd[b]], writes=[b_sg[si]])
                        for j in range(4):
                            a = ctr % 4
                            ctr += 1
                            f = fb * 4 + j
                            for ct in range(16):
                                t.op("tensor", nc.tensor.matmul, acc[a][:, 0:FT], lhsT=wt[:, ct, j * 128:(j + 1) * 128],
                                     rhs=yT[yi][:, ct, :], start=(ct == 0), stop=(ct == 15),
                                     reads=[bw, b_yT[yi]], writes=[b_acc[a]], inc=(ct == 15))
                            last = (bi == len(brs) - 1)
                            if bi == 0:
                                t.op("vector", nc.vector.tensor_tensor,
                                     out=(mrgT[:, f, :] if last else mrg32[:, f, :]),
                                     in0=acc[a][:, 0:FT], in1=sg[si][:, j, :], op=ALU.mult,
                                     reads=[b_acc[a], b_sg[si]], writes=[b_mT if last else b_m32[f]])
                            else:
                                t.op("vector", nc.vector.tensor_tensor, out=sg[si][:, j, :], in0=acc[a][:, 0:FT],
                                     in1=sg[si][:, j, :], op=ALU.mult,
                                     reads=[b_acc[a], b_sg[si]], writes=[b_sg[si]])
                                t.op("vector", nc.vector.tensor_tensor,
                                     out=(mrgT[:, f, :] if last else mrg32[:, f, :]),
                                     in0=sg[si][:, j, :], in1=mrg32[:, f, :], op=ALU.add,
                                     reads=[b_sg[si], b_m32[f]], writes=[b_mT if last else b_m32[f]])
                for cb in range(1 if DBG else 8):
                    wt, bw = load_w(w_out, 32, cb * 512)
                    for s in range(FT // 128):
                        a = ctr % 4
                        o = ctr % 2
                        ctr += 1
                        r0 = tt * FT + s * 128
                        t.dma("sync", oo[o][:], x[r0:r0 + 128, cb * 512:(cb + 1) * 512], writes=[b_oo[o]])
                        for ft in range(32):
                            t.op("tensor", nc.tensor.matmul, acc[a][:], lhsT=mrgT[:, ft, s * 128:(s + 1) * 128],
                                 rhs=wt[:, ft, :], start=(ft == 0), stop=(ft == 31),
                                 reads=[bw, b_mT], writes=[b_acc[a]], inc=(ft == 31))
                        t.op("vector", nc.vector.tensor_tensor, out=oo[o][:], in0=acc[a][:], in1=oo[o][:], op=ALU.add,
                             reads=[b_acc[a], b_oo[o]], writes=[b_oo[o]])
                        t.dma("sync", out[r0:r0 + 128, cb * 512:(cb + 1) * 512], oo[o][:],
                              reads=[b_oo[o]], writes=[b_out])
            t.wait_all("sync", [b_out])
            t.barrier()

        block = st.enter_context(nc.Block())
        t.replay(block)
    return nc


_NC = None


def _in_maps(inputs):
    f32 = lambda a: np.ascontiguousarray(np.asarray(a, dtype=np.float32))
    x = f32(inputs["x"])
    mem = f32(inputs["mem"])
    B, S, _ = x.shape
    xs = x.reshape(B * S // OWN, OWN, D)
    shared = {
        "w_in": f32(inputs["w_in"]),
        "w_mem_kv": f32(inputs["w_mem_kv"]),
        "w_br_a": f32(inputs["w_br_a"]),
        "w_br_s": f32(inputs["w_br_s"]),
        "w_br_c": f32(inputs["w_br_c"]),
        "w_out": f32(inputs["w_out"]),
        "gnB": f32(np.broadcast_to(f32(inputs["g_norm"])[None, :], (128, D))),
        "gmB": f32(np.broadcast_to(f32(inputs["g_mem"])[None, :], (128, D))),
        "qgc": f32(f32(inputs["q_gain_c"]).reshape(4, 128).T),
        "kgc": f32(f32(inputs["k_gain_c"]).reshape(4, 128).T),
    }
    shared["qga"] = f32(f32(inputs["q_gain_a"]).reshape(128, 1))
    shared["kga"] = f32(f32(inputs["k_gain_a"]).reshape(128, 1))

    a_re = f32(inputs["ssm_a_re"]); a_im = f32(inputs["ssm_a_im"]); ldt = f32(inputs["ssm_log_dt"])
    row = lambda v: f32(np.broadcast_to(v.reshape(1, 8192), (128, 8192)))
    shared["s_arep"] = row(a_re); shared["s_irep"] = row(a_im)
    shared["s_lrep"] = row(np.repeat(ldt, 64))
    col = lambda v: f32(v.reshape(64, 128).T)
    shared["s_acol"] = col(a_re); shared["s_icol"] = col(a_im); shared["s_lcol"] = col(np.repeat(ldt, 64))
    b_re = f32(inputs["ssm_b_re"]); b_im = f32(inputs["ssm_b_im"])
    c_re = f32(inputs["ssm_c_re"]); c_im = f32(inputs["ssm_c_im"])
    def arrB(b):
        o = np.zeros((128, 64, 128), np.float32)
        for pr in range(64):
            for gi in range(2):
                g = 2 * pr + gi
                k0 = (g % 8) * 16
                o[k0:k0 + 16, pr, gi * 64:(gi + 1) * 64] = b[g].T
        return f32(o.reshape(128, 8192))
    def arrC(cm):
        o = np.zeros((128, 64, 128), np.float32)
        for pr in range(64):
            for gi in range(2):
                g = 2 * pr + gi
                k0 = (g % 8) * 16
                o[gi * 64:(gi + 1) * 64, pr, k0:k0 + 16] = cm[g].T
        return f32(o.reshape(128, 8192))
    shared["s_bre"] = arrB(b_re); shared["s_bim"] = arrB(b_im)
    shared["s_cre"] = arrC(c_re); shared["s_cim"] = arrC(c_im)
    shared["s_dcol"] = f32(f32(inputs["ssm_d"]).reshape(16, 128).T)
    shared["s_bglu"] = f32(f32(inputs["b_glu"]).reshape(16, 128).T)
    shared["w_glu"] = f32(inputs["w_glu"])
    in_maps = []
    nper = S // OWN
    for c in range(8):
        b, pos = divmod(c, nper)
        m = dict(shared)
        m["x"] = np.ascontiguousarray(xs[c])
        m["mem"] = np.ascontiguousarray(mem[b])
        xp = np.zeros((OFF, D), np.float32)
        if pos > 0:
            xp[OFF - pos * OWN:] = x[b, :pos * OWN]
        m["xprev"] = xp
        vb = np.zeros((128, 32), np.float32)
        vb[:, :(OFF - pos * OWN) // 256] = -1.0e30
        m["vbB"] = vb
        in_maps.append(m)
    return in_maps, (B, S)


def kernel(**inputs):
    global _NC
    in_maps, (B, S) = _in_maps(inputs)
    if _NC is None:
        _NC = build_nc()
    res = run_bass_kernel_spmd(_NC, in_maps, core_ids=list(range(8)))
    o = np.stack([np.asarray(r["out"], dtype=np.float32) for r in res.results], axis=0)
    return o.reshape(B, S, D)
```

```python
import math
import os
import numpy as np
from contextlib import ExitStack
import concourse.bass as bass
import concourse.mybir as mybir
from concourse.bass_utils import run_bass_kernel_spmd

F32 = mybir.dt.float32
BF16 = mybir.dt.bfloat16
AF = mybir.ActivationFunctionType
ALU = mybir.AluOpType
AX = mybir.AxisListType

D = 4096
W = 2048
NIN = 28672
OWN = 2048
TT = 512
NTT = OWN // TT
EPS = 1e-6
MEM = 256

DBG = int(os.environ.get("K_DBG", "0"))
ENGS = ("tensor", "vector", "scalar", "gpsimd", "sync")

ENABLE_A = True
ENABLE_S = True
S_USE_POOL = False
CTX = 8192
OFF = CTX - OWN
NCT = CTX // TT
FT = 512
SLOPES = [2.0 ** (-8.0 * (i + 1) / 16) for i in range(16)]


class Buf:
    __slots__ = ("name", "w", "r", "dsem", "dcount")

    def __init__(self, name=""):
        self.name = name
        self.w = None
        self.r = {}
        self.dsem = None
        self.dcount = 0


class Trk:
    def __init__(self, nc, stack):
        self.nc = nc
        self.stack = stack
        self.ops = {e: [] for e in ENGS}
        self.cnt = {e: 0 for e in ENGS}
        self.sem = {e: stack.enter_context(nc.semaphore(f"s_{e}")) for e in ENGS}
        self.seen = {e: {} for e in ENGS}
        self.dsems = []

    def _dsem(self, buf):
        if buf.dsem is None:
            buf.dsem = self.stack.enter_context(self.nc.semaphore(f"d{len(self.dsems)}"))
            self.dsems.append(buf)
        return buf.dsem

    def _need(self, eng, dep, waits):
        if dep is None:
            return
        kind, key, count = dep
        k = (kind, id(key) if kind == 'd' else key)
        if self.seen[eng].get(k, 0) >= count:
            return
        self.seen[eng][k] = count
        waits.append((self.sem[key] if kind == 'e' else key, count))

    def _deps(self, eng, reads, writes):
        waits = []
        for b in reads:
            self._need(eng, b.w, waits)
        for b in writes:
            self._need(eng, b.w, waits)
            for d in b.r.values():
                self._need(eng, d, waits)
        return waits

    def op(self, eng, fn, *args, reads=(), writes=(), inc=True, **kw):
        waits = self._deps(eng, reads, writes)
        if inc:
            self.cnt[eng] += 1
            c = self.cnt[eng]
        else:
            c = self.cnt[eng] + 1
        me = ('e', eng, c)
        for b in reads:
            b.r[eng] = me
        if inc:
            for b in writes:
                b.w = me
                b.r = {}
        self.ops[eng].append((fn, args, kw, waits, (self.sem[eng], 1) if inc else None))

    def dma(self, eng, out, in_, reads=(), writes=()):
        waits = self._deps(eng, reads, writes)
        anchor = writes[0] if writes else reads[0]
        sem = self._dsem(anchor)
        anchor.dcount += 16
        me = ('d', sem, anchor.dcount)
        for b in reads:
            b.r[('d', id(sem))] = me
        for b in writes:
            b.w = me
            b.r = {}
        fn = getattr(self.nc, eng).dma_start
        self.ops[eng].append((fn, (), dict(out=out, in_=in_), waits, (sem, 16)))

    def wait_all(self, eng, bufs):
        waits = []
        for b in bufs:
            self._need(eng, b.w, waits)
            for d in b.r.values():
                self._need(eng, d, waits)
        self.ops[eng].append((None, (), {}, waits, None))

    def barrier(self):
        snap = [('e', e, self.cnt[e]) for e in ENGS if self.cnt[e] > 0]
        snap += [('d', b.dsem, b.dcount) for b in self.dsems if b.dcount > 0]
        for e in ENGS:
            waits = []
            for d in snap:
                if d[0] == 'e' and d[1] == e:
                    continue
                self._need(e, d, waits)
            self.ops[e].append((None, (), {}, waits, None))

    def replay(self, block):
        for e in ENGS:
            ops = self.ops[e]
            if not ops:
                continue

            def body(engine, ops=ops):
                for fn, args, kw, waits, inc in ops:
                    for s, v in waits:
                        engine.wait_ge(s, v)
                    if fn is not None:
                        ins = fn(*args, **kw)
                        if inc is not None:
                            ins.then_inc(inc[0], inc[1])
            getattr(block, e)(body)


DRAM_NAMES = []


def build_nc():
    nc = bass.Bass("TRN2", target_bir_lowering=False)

    def din(name, shape, dt=F32):
        DRAM_NAMES.append(name)
        return nc.dram_tensor(name, list(shape), dt, kind="ExternalInput").ap()

    def dscr(name, shape, dt):
        DRAM_NAMES.append(name)
        return nc.dram_tensor(name, list(shape), dt, kind="Internal").ap()

    x = din("x", [OWN, D])
    mem = din("mem", [MEM, D])
    w_in = din("w_in", [D, NIN])
    w_kv = din("w_mem_kv", [D, 2 * W])
    w_br = [din(n, [W, D]) for n in ("w_br_a", "w_br_s", "w_br_c")]
    w_out = din("w_out", [D, D])
    gnB = din("gnB", [128, D])
    gmB = din("gmB", [128, D])
    qgc = din("qgc", [128, 4])
    kgc = din("kgc", [128, 4])
    xprev = din("xprev", [OFF, D])
    vbB = din("vbB", [128, 32])
    qga = din("qga", [128, 1])
    kga = din("kga", [128, 1])
    s_arep = din("s_arep", [128, 8192]); s_irep = din("s_irep", [128, 8192]); s_lrep = din("s_lrep", [128, 8192])
    s_acol = din("s_acol", [128, 64]); s_icol = din("s_icol", [128, 64]); s_lcol = din("s_lcol", [128, 64])
    s_bre = din("s_bre", [128, 8192]); s_bim = din("s_bim", [128, 8192])
    s_cre = din("s_cre", [128, 8192]); s_cim = din("s_cim", [128, 8192])
    s_dcol = din("s_dcol", [128, 16]); s_bglu = din("s_bglu", [128, 16])
    w_glu = din("w_glu", [W, W])
    out = nc.dram_tensor("out", [OWN, D], F32, kind="ExternalOutput").ap()

    hTd = dscr("hTd", [32, 128, CTX], BF16)
    kTd = dscr("kTd", [16, 128, CTX], BF16)
    qTd = dscr("qTd", [16, 128, OWN], BF16)
    Vd = dscr("Vd", [CTX, W], BF16)
    uTd = dscr("uTd", [16, 128, CTX], BF16)
    y1d = dscr("y1d", [16, 128, OWN], F32)
    y1bd = dscr("y1bd", [16, 128, OWN], BF16)
    qcTd = dscr("qcTd", [16, 128, OWN], BF16)
    szd = [dscr(f"szd{i}", [16, 128, OWN], F32) for i in range(3)]
    sgd = [dscr(f"sgd{i}", [32, 128, OWN], F32) for i in range(3)]
    yTd = [dscr(f"yTd{i}", [16, 128, OWN], BF16) for i in range(3)]

    with ExitStack() as st:
        t = Trk(nc, st)
        sb = lambda name, shape, dt, stk: stk.enter_context(nc.sbuf_tensor(name, list(shape), dt))
        ps = lambda name, shape, dt, stk: stk.enter_context(nc.psum_tensor(name, list(shape), dt))

        identf = sb("identf", [128, 128], F32, st)
        ident = sb("ident", [128, 128], BF16, st)
        onesb = sb("onesb", [128, 128], BF16, st)
        epst = sb("epst", [128, 1], F32, st)
        qgct = sb("qgct", [128, 4], F32, st)
        kgct = sb("kgct", [128, 4], F32, st)
        KcT = sb("KcT", [128, 16, MEM], BF16, st)
        Vc = sb("Vc", [128, 2, W], BF16, st)
        qgat = sb("qgat", [128, 1], F32, st)
        kgat = sb("kgat", [128, 1], F32, st)
        kmT = sb("kmT", [128, 16, 32], F32, st)
        b_kmT = Buf("kmT")
        b_const = Buf("const")
        b_KcT = Buf("KcT")
        b_Vc = Buf("Vc")
        t.op("gpsimd", nc.gpsimd.memset, identf[:], 0.0, writes=[b_const])
        t.op("gpsimd", nc.gpsimd.affine_select, out=identf[:], in_=identf[:], pattern=[[-1, 128]],
             compare_op=ALU.not_equal, fill=1.0, base=0, channel_multiplier=1,
             reads=[b_const], writes=[b_const])
        t.op("vector", nc.vector.tensor_copy, ident[:], identf[:], reads=[b_const], writes=[b_const])
        t.op("vector", nc.vector.memset, onesb[:], 1.0, reads=[b_const], writes=[b_const])
        t.op("vector", nc.vector.memset, epst[:], EPS, reads=[b_const], writes=[b_const])
        if DBG:
            t.op("vector", nc.vector.memset, kmT[:], 0.0, writes=[b_kmT])
        b_g = Buf("gains")
        t.dma("sync", qgct[:], qgc[:, :], writes=[b_g])
        t.dma("sync", kgct[:], kgc[:, :], writes=[b_g])
        t.dma("sync", qgat[:], qga[:, :], writes=[b_g])
        t.dma("sync", kgat[:], kga[:, :], writes=[b_g])
        t.op("vector", nc.vector.tensor_scalar, out=qgat[:], in0=qgat[:], scalar1=float(128 ** -0.5),
             scalar2=None, op0=ALU.mult, reads=[b_g], writes=[b_g])
        t.op("vector", nc.vector.tensor_scalar, out=qgct[:], in0=qgct[:], scalar1=float(512 ** -0.5),
             scalar2=None, op0=ALU.mult, reads=[b_g], writes=[b_g])

        wbuf = [None, None]
        b_w = [Buf(f"w{i}") for i in range(2)]
        wctr = [0]
        wgen = [0]

        def mk_w(stk):
            wgen[0] += 1
            for i in range(2):
                wbuf[i] = sb(f"wbuf{wgen[0]}_{i}", [128, 32, 512], BF16, stk)

        def load_w(src, nkt, c0, ncols=512):
            i = wctr[0] % 2
            wctr[0] += 1
            v = src.rearrange("(kt p) c -> p kt c", p=128)
            half = nkt // 2
            t.dma("gpsimd", wbuf[i][:, 0:half, 0:ncols], v[:, 0:half, c0:c0 + ncols], writes=[b_w[i]])
            t.dma("gpsimd", wbuf[i][:, half:nkt, 0:ncols], v[:, half:nkt, c0:c0 + ncols], writes=[b_w[i]])
            return wbuf[i], b_w[i]

        b_hTd = Buf("hTd")
        with ExitStack() as p0:
            hT = [sb(f"p0hT{i}", [128, 32, TT], BF16, p0) for i in range(2)]
            b_hT = [Buf(), Buf()]
            gB = sb("p0gB", [128, D], F32, p0)
            xt = [sb(f"p0xt{i}", [128, D], F32, p0) for i in range(2)]
            hb = sb("p0hb", [128, D], BF16, p0)
            stat = sb("p0stat", [128, 4], F32, p0)
            tp = [ps(f"p0tp{i}", [128, 1024], BF16, p0) for i in range(4)]
            b_gB, b_hb, b_junk, b_stat = Buf(), Buf(), Buf(), Buf()
            b_xt = [Buf(), Buf()]
            b_tp = [Buf() for _ in range(4)]
            t.dma("sync", gB[:], gnB[:, :], writes=[b_gB])
            for i in range(CTX // 128):
                tt, s = divmod(i, 4)
                xi, bx = xt[i % 2], b_xt[i % 2]
                src = xprev[i * 128:(i + 1) * 128, :] if i < OFF // 128 else x[i * 128 - OFF:(i + 1) * 128 - OFF, :]
                t.dma("sync", xi[:, 0:D // 2], src[:, 0:D // 2], writes=[bx])
                t.dma("sync", xi[:, D // 2:D], src[:, D // 2:D], writes=[bx])
                t.op("scalar", nc.scalar.activation, out=hb[:], in_=xi[:], func=AF.Square,
                     accum_out=stat[:, 0:1], reads=[bx], writes=[b_hb, b_stat])
                t.op("scalar", nc.scalar.activation, out=stat[:, 1:2], in_=stat[:, 0:1], func=AF.Sqrt,
                     bias=epst[:, 0:1], scale=1.0 / D, reads=[b_stat, b_const], writes=[b_stat])
                t.op("vector", nc.vector.reciprocal, stat[:, 2:3], stat[:, 1:2], reads=[b_stat], writes=[b_stat])
                t.op("vector", nc.vector.scalar_tensor_tensor, out=hb[:], in0=xi[:], scalar=stat[:, 2:3],
                     in1=gB[:], op0=ALU.mult, op1=ALU.mult, reads=[bx, b_stat, b_gB], writes=[b_hb])
                for q in range(4):
                    for k in range(8):
                        kt = q * 8 + k
                        t.op("tensor", nc.tensor.transpose, tp[q][:, k * 128:(k + 1) * 128],
                             hb[:, kt * 128:(kt + 1) * 128], ident[:],
                             reads=[b_hb, b_const], writes=[b_tp[q]], inc=(k == 7))
                    dst = hT[tt % 2][:, q * 8:(q + 1) * 8, s * 128:(s + 1) * 128]
                    srcp = tp[q][:].rearrange("p (k c) -> p k c", k=8)
                    if q % 2 == 0:
                        t.op("vector", nc.vector.tensor_copy, dst, srcp, reads=[b_tp[q]], writes=[b_hT[tt % 2]])
                    else:
                        t.op("scalar", nc.scalar.copy, dst, srcp, reads=[b_tp[q]], writes=[b_hT[tt % 2]])
                if s == 3:
                    t.dma("sync", hTd[:, :, tt * TT:(tt + 1) * TT].rearrange("kt p t -> p kt t"),
                          hT[tt % 2][:], reads=[b_hT[tt % 2]], writes=[b_hTd])
            t.barrier()

        with ExitStack() as pc:
            mT = sb("pcmT", [128, 32, MEM], BF16, pc)
            b_mT = Buf("mT")
            pca = pc.enter_context(ExitStack())
            gB = sb("pcgB", [128, D], F32, pca)
            xt = [sb(f"pcxt{i}", [128, D], F32, pca) for i in range(2)]
            hb = sb("pchb", [128, D], BF16, pca)
            junk = sb("pcjunk", [128, D], BF16, pca)
            stat = sb("pcstat", [128, 4], F32, pca)
            tp = [ps(f"pctp{i}", [128, 1024], BF16, pca) for i in range(4)]
            b_gB, b_hb, b_junk, b_stat = Buf(), Buf(), Buf(), Buf()
            b_xt = [Buf(), Buf()]
            b_tp = [Buf() for _ in range(4)]
            t.dma("sync", gB[:], gmB[:, :], writes=[b_gB])
            for i in range(MEM // 128):
                xi, bx = xt[i % 2], b_xt[i % 2]
                src = mem[i * 128:(i + 1) * 128, :]
                t.dma("sync", xi[:], src, writes=[bx])
                t.op("scalar", nc.scalar.activation, out=junk[:], in_=xi[:], func=AF.Square,
                     accum_out=stat[:, 0:1], reads=[bx], writes=[b_junk, b_stat])
                t.op("scalar", nc.scalar.activation, out=stat[:, 1:2], in_=stat[:, 0:1], func=AF.Sqrt,
                     bias=epst[:, 0:1], scale=1.0 / D, reads=[b_stat, b_const], writes=[b_stat])
                t.op("vector", nc.vector.reciprocal, stat[:, 2:3], stat[:, 1:2], reads=[b_stat], writes=[b_stat])
                t.op("vector", nc.vector.scalar_tensor_tensor, out=hb[:], in0=xi[:], scalar=stat[:, 2:3],
                     in1=gB[:], op0=ALU.mult, op1=ALU.mult, reads=[bx, b_stat, b_gB], writes=[b_hb])
                for q in range(4):
                    for k in range(8):
                        kt = q * 8 + k
                        t.op("tensor", nc.tensor.transpose, tp[q][:, k * 128:(k + 1) * 128],
                             hb[:, kt * 128:(kt + 1) * 128], ident[:],
                             reads=[b_hb, b_const], writes=[b_tp[q]], inc=(k == 7))
                    dst = mT[:, q * 8:(q + 1) * 8, i * 128:(i + 1) * 128]
                    srcp = tp[q][:].rearrange("p (k c) -> p k c", k=8)
                    t.op("vector", nc.vector.tensor_copy, dst, srcp, reads=[b_tp[q]], writes=[b_mT])
            t.barrier()
            pca.close()
            mk_w(pc)
            kps = [ps(f"pckps{i}", [128, 512], F32, pc) for i in range(4)]
            ssp = ps("pcssp", [128, 512], F32, pc)
            vps = [ps(f"pcvps{i}", [128, 512], F32, pc) for i in range(2)]
            sq = sb("pcsq", [128, 4, MEM], BF16, pc)
            rstd = sb("pcrstd", [128, MEM], F32, pc)
            b_kps = [Buf() for _ in range(4)]
            b_ssp, b_sq, b_rstd = Buf(), Buf(), Buf()
            b_vps = [Buf(), Buf()]
            for hd in range(4):
                wt, bw = load_w(w_kv, 32, hd * 512)
                for j in range(4):
                    for kt in range(32):
                        t.op("tensor", nc.tensor.matmul, kps[j][:, 0:MEM], lhsT=wt[:, kt, j * 128:(j + 1) * 128],
                             rhs=mT[:, kt, :], start=(kt == 0), stop=(kt == 31),
                             reads=[bw, b_mT], writes=[b_kps[j]], inc=(kt == 31))
                    t.op("scalar", nc.scalar.activation, out=sq[:, j, :], in_=kps[j][:, 0:MEM], func=AF.Square,
                         reads=[b_kps[j]], writes=[b_sq])
                for j in range(4):
                    t.op("tensor", nc.tensor.matmul, ssp[:, 0:MEM], lhsT=onesb[:], rhs=sq[:, j, :],
                         start=(j == 0), stop=(j == 3), reads=[b_sq, b_const], writes=[b_ssp], inc=(j == 3))
                t.op("scalar", nc.scalar.activation, out=rstd[:], in_=ssp[:, 0:MEM], func=AF.Sqrt,
                     bias=epst[:, 0:1], scale=1.0 / 512, reads=[b_ssp, b_const], writes=[b_rstd])
                t.op("vector", nc.vector.reciprocal, rstd[:], rstd[:], reads=[b_rstd], writes=[b_rstd])
                for j in range(4):
                    t.op("vector", nc.vector.scalar_tensor_tensor, out=KcT[:, hd * 4 + j, :], in0=kps[j][:, 0:MEM],
                         scalar=kgct[:, j:j + 1], in1=rstd[:], op0=ALU.mult, op1=ALU.mult,
                         reads=[b_kps[j], b_g, b_rstd], writes=[b_KcT])
            for cb in range(4):
                wt, bw = load_w(w_kv, 32, W + cb * 512)
                for mt in range(2):
                    for kt in range(32):
                        t.op("tensor", nc.tensor.matmul, vps[mt][:], lhsT=mT[:, kt, mt * 128:(mt + 1) * 128],
                             rhs=wt[:, kt, :], start=(kt == 0), stop=(kt == 31),
                             reads=[bw, b_mT], writes=[b_vps[mt]], inc=(kt == 31))
                    t.op("vector", nc.vector.tensor_copy, Vc[:, mt, cb * 512:(cb + 1) * 512], vps[mt][:],
                         reads=[b_vps[mt]], writes=[b_Vc])
            t.barrier()

        b_qcTd = Buf("qcTd")
        b_szd = [Buf() for _ in range(3)]
        b_sgd = [Buf() for _ in range(3)]
        with ExitStack() as p1:
            mk_w(p1)
            hT = [sb(f"p1hT{i}", [128, 32, TT], BF16, p1) for i in range(2)]
            b_hT = [Buf(), Buf()]
            acc = [ps(f"p1acc{i}", [128, 512], F32, p1) for i in range(4)]
            b_acc = [Buf() for _ in range(4)]
            ssp = ps("p1ssp", [128, 512], F32, p1)
            b_ssp = Buf()
            sq = sb("p1sq", [128, 4, TT], BF16, p1)
            rstd = sb("p1rstd", [128, TT], F32, p1)
            b_sq, b_rstd = Buf(), Buf()
            ob32 = [sb(f"p1ob32_{i}", [128, TT], F32, p1) for i in range(2)]
            ob16 = [sb(f"p1ob16_{i}", [128, 4, TT], BF16, p1) for i in range(2)]
            b_ob32 = [Buf(), Buf()]
            b_ob16 = [Buf(), Buf()]
            hctr = [0]
            octr = [0]

            def load_hT(tt):
                i = hctr[0] % 2
                hctr[0] += 1
                src = hTd[:, :, tt * TT:(tt + 1) * TT].rearrange("kt p t -> p kt t")
                t.dma("sync", hT[i][:, 0:16, :], src[:, 0:16, :], reads=[b_hTd], writes=[b_hT[i]])
                t.dma("sync", hT[i][:, 16:32, :], src[:, 16:32, :], reads=[b_hTd], writes=[b_hT[i]])
                return hT[i], b_hT[i]

            def proj_fm(wt, bw, j, ht, bh, a):
                for kt in range(32):
                    t.op("tensor", nc.tensor.matmul, acc[a][:], lhsT=wt[:, kt, j * 128:(j + 1) * 128],
                         rhs=ht[:, kt, :], start=(kt == 0), stop=(kt == 31),
                         reads=[bw, bh], writes=[b_acc[a]], inc=(kt == 31))

            def act_blocks():
                lst = []
                for bi, base in ((0, 12), (1, 20), (2, 28)):
                    if (bi == 0 and not ENABLE_A) or (bi == 1 and not ENABLE_S):
                        continue
                    for k in range(4):
                        lst.append(("silu", szd[bi], b_szd[bi], base + k, k))
                for bi, base in ((0, 32), (1, 40), (2, 48)):
                    if (bi == 0 and not ENABLE_A) or (bi == 1 and not ENABLE_S):
                        continue
                    for k in range(8):
                        lst.append(("sigm", sgd[bi], b_sgd[bi], base + k, k))
                return lst

            b_kTd, b_qTd, b_Vd, b_uTd = Buf("kTd"), Buf("qTd"), Buf("Vd"), Buf("uTd")
            OT0 = NCT - NTT
            items = []
            for kind, dst, bdst, cb, k in act_blocks():
                for tt in range(NTT):
                    items.append((cb, OT0 + tt, ("act", kind, dst, bdst, k, tt)))
            for hd in range(4):
                for tt in range(NTT):
                    items.append((24 + hd, OT0 + tt, ("qc", hd, tt)))
            if ENABLE_A:
                for is_k in (True, False):
                    for cbl in range(4):
                        for tt in (range(NCT) if is_k else range(OT0, NCT)):
                            items.append(((4 if is_k else 0) + cbl, tt, ("qk", is_k, cbl, tt)))
                for cbl in range(4):
                    for tt in range(NCT):
                        items.append((8 + cbl, tt, ("v", cbl, tt)))
            if ENABLE_S:
                for cbl in range(4):
                    for tt in range(NCT):
                        items.append((16 + cbl, tt, ("u", cbl, tt)))

            if DBG:
                seen_k = set()
                cnt_k = {}
                keep = []
                for it in items:
                    kk = (it[2][0], it[2][1])
                    cnt_k[kk] = cnt_k.get(kk, 0) + 1
                    if cnt_k[kk] <= DBG:
                        keep.append(it)
                items = keep

            def nexta():
                a = octr[0] % 4
                octr[0] += 1
                return a

            def do_item(spec, wt, bw, ht, bh, tt):
                kind = spec[0]
                if kind == "act":
                    _, fn, dst, bdst, k, tq = spec
                    for j in range(4):
                        a = nexta()
                        o = a % 2
                        proj_fm(wt, bw, j, ht, bh, a)
                        t.op("scalar", nc.scalar.activation, out=ob32[o][:], in_=acc[a][:],
                             func=AF.Silu if fn == "silu" else AF.Sigmoid, reads=[b_acc[a]], writes=[b_ob32[o]])
                        t.dma("sync", dst[k * 4 + j, :, tq * TT:(tq + 1) * TT], ob32[o][:],
                              reads=[b_ob32[o]], writes=[bdst])
                elif kind == "qc":
                    _, hd, tq = spec
                    o = nexta() % 2
                    for j in range(4):
                        proj_fm(wt, bw, j, ht, bh, j)
                        t.op("scalar", nc.scalar.activation, out=sq[:, j, :], in_=acc[j][:], func=AF.Square,
                             reads=[b_acc[j]], writes=[b_sq])
                    for j in range(4):
                        t.op("tensor", nc.tensor.matmul, ssp[:], lhsT=onesb[:], rhs=sq[:, j, :],
                             start=(j == 0), stop=(j == 3), reads=[b_sq, b_const], writes=[b_ssp], inc=(j == 3))
                    t.op("scalar", nc.scalar.activation, out=rstd[:], in_=ssp[:], func=AF.Sqrt,
                         bias=epst[:, 0:1], scale=1.0 / 512, reads=[b_ssp, b_const], writes=[b_rstd])
                    t.op("vector", nc.vector.reciprocal, rstd[:], rstd[:], reads=[b_rstd], writes=[b_rstd])
                    for j in range(4):
                        t.op("vector", nc.vector.scalar_tensor_tensor, out=ob16[o][:, j, :], in0=acc[j][:],
                             scalar=qgct[:, j:j + 1], in1=rstd[:], op0=ALU.mult, op1=ALU.mult,
                             reads=[b_acc[j], b_g, b_rstd], writes=[b_ob16[o]])
                    t.dma("sync", qcTd[hd * 4:(hd + 1) * 4, :, tq * TT:(tq + 1) * TT].rearrange("j p t -> p j t"),
                          ob16[o][:], reads=[b_ob16[o]], writes=[b_qcTd])
                elif kind == "qk":
                    _, is_k, cbl, _tt = spec
                    banks = [nexta() for _ in range(4)]

                    def post(j):
                        a = banks[j]
                        o = a % 2
                        head = cbl * 4 + j
                        t.op("scalar", nc.scalar.activation, out=sq[:, j, :], in_=acc[a][:], func=AF.Square,
                             reads=[b_acc[a]], writes=[b_sqj[j]])
                        t.op("tensor", nc.tensor.matmul, ssp2[j % 2][:], lhsT=onesb[:], rhs=sq[:, j, :],
                             start=True, stop=True, reads=[b_sqj[j], b_const], writes=[b_ssp2[j % 2]])
                        t.op("scalar", nc.scalar.activation, out=rstd2[j % 2][:], in_=ssp2[j % 2][:], func=AF.Sqrt,
                             bias=epst[:, 0:1], scale=1.0 / 128, reads=[b_ssp2[j % 2], b_const], writes=[b_rstd2[j % 2]])
                        t.op("vector", nc.vector.reciprocal, rstd2[j % 2][:], rstd2[j % 2][:],
                             reads=[b_rstd2[j % 2]], writes=[b_rstd2[j % 2]])
                        t.op("vector", nc.vector.scalar_tensor_tensor, out=obq[a][:], in0=acc[a][:],
                             scalar=(kgat if is_k else qgat)[:, 0:1], in1=rstd2[j % 2][:], op0=ALU.mult, op1=ALU.mult,
                             reads=[b_acc[a], b_g, b_rstd2[j % 2]], writes=[b_obq[a]])
                        if is_k:
                            t.op("vector", nc.vector.tensor_reduce, out=kmT[:, head, tt * 2:(tt + 1) * 2],
                                 in_=obq[a][:].rearrange("p (b k) -> p b k", b=2), axis=AX.X, op=ALU.add,
                                 reads=[b_obq[a]], writes=[b_kmT])
                            t.dma("sync", kTd[head, :, tt * TT:(tt + 1) * TT], obq[a][:],
                                  reads=[b_obq[a]], writes=[b_kTd])
                        else:
                            tq = tt - OT0
                            t.dma("sync", qTd[head, :, tq * TT:(tq + 1) * TT], obq[a][:],
                                  reads=[b_obq[a]], writes=[b_qTd])
                    for step in range(5):
                        if step < 4:
                            proj_fm(wt, bw, step, ht, bh, banks[step])
                        if step >= 1:
                            post(step - 1)
                elif kind == "v":
                    _, cbl, _tt = spec
                    for s_ in range(4):
                        a = nexta()
                        for kt in range(32):
                            t.op("tensor", nc.tensor.matmul, acc[a][:], lhsT=ht[:, kt, s_ * 128:(s_ + 1) * 128],
                                 rhs=wt[:, kt, :], start=(kt == 0), stop=(kt == 31),
                                 reads=[bw, bh], writes=[b_acc[a]], inc=(kt == 31))
                        t.op("scalar", nc.scalar.copy, obq[a][:], acc[a][:], reads=[b_acc[a]], writes=[b_obq[a]])
                        r0 = tt * TT + s_ * 128
                        t.dma("sync", Vd[r0:r0 + 128, cbl * 512:(cbl + 1) * 512], obq[a][:],
                              reads=[b_obq[a]], writes=[b_Vd])
                elif kind == "u":
                    _, cbl, _tt = spec
                    for j in range(4):
                        a = nexta()
                        proj_fm(wt, bw, j, ht, bh, a)
                        t.op("scalar", nc.scalar.copy, obq[a][:], acc[a][:], reads=[b_acc[a]], writes=[b_obq[a]])
                        t.dma("sync", uTd[cbl * 4 + j, :, tt * TT:(tt + 1) * TT], obq[a][:],
                              reads=[b_obq[a]], writes=[b_uTd])

            obq = [sb(f"p1obq{i}", [128, TT], BF16, p1) for i in range(4)]
            b_obq = [Buf() for _ in range(4)]
            b_sqj = [Buf() for _ in range(4)]
            ssp2 = [ssp, ps("p1ssp2", [128, 512], F32, p1)]
            b_ssp2 = [b_ssp, Buf()]
            rstd2 = [rstd, sb("p1rstd2", [128, TT], F32, p1)]
            b_rstd2 = [b_rstd, Buf()]
            cur_cb = None
            nxt = load_hT(items[0][1])
            for idx, (cb, tt, spec) in enumerate(items):
                ht, bh = nxt
                if cb != cur_cb:
                    wt, bw = load_w(w_in, 32, cb * 512)
                    cur_cb = cb
                if idx + 1 < len(items):
                    nxt = load_hT(items[idx + 1][1])
                do_item(spec, wt, bw, ht, bh, tt)
            t.barrier()

        b_yTd = [Buf() for _ in range(3)]
        if ENABLE_A:
          with ExitStack() as pa:
            NB = CTX // 256
            QB0 = OFF // 256
            KT_ = [sb(f"paKT{i}", [128, CTX], BF16, pa) for i in range(2)]
            Vh = [sb(f"paVh{i}", [128, CTX // 128, 128], BF16, pa) for i in range(2)]
            QT = [sb(f"paQT{i}", [128, OWN], BF16, pa) for i in range(2)]
            b_KT, b_Vh, b_QT = [Buf(), Buf()], [Buf(), Buf()], [Buf(), Buf()]
            maskT = sb("pamaskT", [32, OWN], BF16, pa)
            Sel = sb("paSel", [32, 32, 128], BF16, pa)
            kbias = sb("pakbias", [128, 16, 64], F32, pa)
            Ftab = sb("paFtab", [128, 16, 256], F32, pa)
            Dtab = sb("paDtab", [128, 2, 256], F32, pa)
            cbias = sb("pacbias", [128, 8, 32], F32, pa)
            vbt = sb("pavbt", [128, 32], F32, pa)
            kmb = sb("pakmb", [128, 16, 32], BF16, pa)
            iot = sb("paiot", [128, 512], mybir.dt.int32, pa)
            iof = sb("paiof", [128, 512], F32, pa)
            tmpf = sb("patmpf", [128, 512], F32, pa)
            b_tab = Buf("tab")
            b_maskT = Buf("maskT")
            t.dma("sync", vbt[:], vbB[:, :], writes=[b_tab])
            t.op("gpsimd", nc.gpsimd.iota, iot[:, 0:64], pattern=[[-128, 64]], base=0, channel_multiplier=1,
                 reads=[b_tab], writes=[b_tab])
            t.op("vector", nc.vector.tensor_copy, iof[:, 0:64], iot[:, 0:64], reads=[b_tab], writes=[b_tab])
            for h in range(16):
                t.op("vector", nc.vector.tensor_scalar, out=kbias[:, h, :], in0=iof[:, 0:64], scalar1=float(SLOPES[h]),
                     scalar2=None, op0=ALU.mult, reads=[b_tab], writes=[b_tab])
            t.op("gpsimd", nc.gpsimd.iota, iot[:, 0:256], pattern=[[1, 256]], base=0, channel_multiplier=0,
                 reads=[b_tab], writes=[b_tab])
            t.op("vector", nc.vector.tensor_copy, iof[:, 0:256], iot[:, 0:256], reads=[b_tab], writes=[b_tab])
            for h in range(16):
                t.op("scalar", nc.scalar.activation, out=Ftab[:, h, :], in_=iof[:, 0:256], func=AF.Exp,
                     scale=-float(SLOPES[h]), reads=[b_tab], writes=[b_tab])
            t.op("gpsimd", nc.gpsimd.iota, iot[:, 0:512].rearrange("p (a b) -> p a b", a=2),
                 pattern=[[-128, 2], [1, 256]], base=0, channel_multiplier=-1, reads=[b_tab], writes=[b_tab])
            Dflat = Dtab[:].rearrange("p a b -> p (a b)")
            t.op("vector", nc.vector.tensor_copy, Dflat, iot[:, 0:512], reads=[b_tab], writes=[b_tab])
            t.op("vector", nc.vector.tensor_scalar, out=tmpf[:], in0=Dflat, scalar1=0.0, scalar2=1.0e6,
                 op0=ALU.is_lt, op1=ALU.mult, reads=[b_tab], writes=[b_tab])
            t.op("vector", nc.vector.tensor_tensor, out=Dflat, in0=Dflat, in1=tmpf[:], op=ALU.add,
                 reads=[b_tab], writes=[b_tab])
            t.op("vector", nc.vector.tensor_copy, Sel[:], identf[0:32, 0:32].unsqueeze(2).broadcast_to([32, 32, 128]),
                 reads=[b_const, b_tab], writes=[b_tab])
            t.op("vector", nc.vector.memset, cbias[:], -1.0e30, reads=[b_tab], writes=[b_tab])
            for qbl in range(8):
                t.op("vector", nc.vector.tensor_copy, cbias[:, qbl, 0:QB0 + qbl], vbt[:, 0:QB0 + qbl],
                     reads=[b_tab], writes=[b_tab])
            t.op("vector", nc.vector.tensor_copy, kmb[:], kmT[:], reads=[b_kmT, b_tab], writes=[b_tab])

            scpB2 = [ps(f"pascpB{i}", [128, 512], F32, pa) for i in range(2)]
            accB = [ps(f"paaccB{i}", [128, 512], F32, pa) for i in range(2)]
            dgB = [ps(f"padgB{i}", [128, 512], F32, pa) for i in range(2)]
            gps = ps("pagps", [128, 512], F32, pa)
            tps = ps("patps", [128, 1024], BF16, pa)
            scp = [scpB2[0][:, 0:256], scpB2[1][:, 0:256]]
            b_scp = [Buf(), Buf()]
            b_oacc, b_dacc, b_odg, b_ddg = [Buf(), Buf()], [Buf(), Buf()], [Buf(), Buf()], [Buf(), Buf()]
            b_gps, b_tps = Buf(), Buf()
            Gs = sb("paGs", [128, 32], F32, pa)
            m8 = sb("pam8", [128, 8], F32, pa)
            thr = sb("pathr", [128, 1], F32, pa)
            mf = sb("pamf", [128, 32], F32, pa)
            mb16 = sb("pamb16", [128, 32], BF16, pa)
            b_gs = Buf("gs")
            pT = [sb(f"papT{i}", [128, 512], BF16, pa) for i in range(2)]
            b_pT = [Buf(), Buf()]
            s32 = sb("pas32", [128, 256], F32, pa)
            b_s32 = Buf()
            num = sb("panum", [128, 256], F32, pa)
            den = sb("paden", [128, 256], F32, pa)
            b_nd = Buf()
            sza = [sb(f"pasza{i}", [128, 256], F32, pa) for i in range(2)]
            b_sza = [Buf(), Buf()]
            yo = [sb(f"payo{i}", [128, 256], BF16, pa) for i in range(2)]
            b_yo = [Buf(), Buf()]
            dsum = [[sb(f"padsum{i}{j}", [128, 512], F32, pa) for j in range(2)] for i in range(2)]
            b_dsum = [[Buf(), Buf()], [Buf(), Buf()]]
            onesf32 = sb("paonesf32", [128, 128], F32, pa)
            t.op("vector", nc.vector.memset, onesf32[:], 1.0, reads=[b_tab], writes=[b_tab])

            def load_head(h):
                hi = h % 2
                t.dma("sync", KT_[hi][:, 0:CTX // 2], kTd[h, :, 0:CTX // 2], reads=[b_kTd], writes=[b_KT[hi]])
                t.dma("sync", KT_[hi][:, CTX // 2:CTX], kTd[h, :, CTX // 2:CTX], reads=[b_kTd], writes=[b_KT[hi]])
                vsrc = Vd[:, h * 128:(h + 1) * 128].rearrange("(kt p) d -> p kt d", p=128)
                for q4 in range(4):
                    t.dma("sync", Vh[hi][:, q4 * 16:(q4 + 1) * 16, :], vsrc[:, q4 * 16:(q4 + 1) * 16, :],
                          reads=[b_Vd], writes=[b_Vh[hi]])
                t.dma("sync", QT[hi][:], qTd[h, :, :], reads=[b_qTd], writes=[b_QT[hi]])

            pc_ = 0
            load_head(0)
            NH = DBG if DBG else 16
            for h in range(NH):
                hi = h % 2
                for qt in range(OWN // 128):
                    qsl = slice(qt * 128, (qt + 1) * 128)
                    t.op("tensor", nc.tensor.matmul, gps[:, 0:32], lhsT=QT[hi][:, qsl], rhs=kmb[:, h, :],
                         start=True, stop=True, reads=[b_QT[hi], b_tab], writes=[b_gps])
                    t.op("vector", nc.vector.tensor_tensor, out=Gs[:], in0=gps[:, 0:32], in1=cbias[:, qt // 2, :],
                         op=ALU.add, reads=[b_gps, b_tab], writes=[b_gs])
                    t.op("vector", nc.vector.max, out=m8[:], in_=Gs[:], reads=[b_gs], writes=[b_gs])
                    t.op("vector", nc.vector.tensor_scalar, out=thr[:], in0=m8[:, 2:3], scalar1=-1.0e29, scalar2=None,
                         op0=ALU.max, reads=[b_gs], writes=[b_gs])
                    t.op("vector", nc.vector.tensor_scalar, out=mf[:], in0=Gs[:], scalar1=thr[:, 0:1], scalar2=None,
                         op0=ALU.is_ge, reads=[b_gs], writes=[b_gs])
                    t.op("vector", nc.vector.tensor_scalar, out=mb16[:], in0=mf[:], scalar1=-1.0, scalar2=30000.0,
                         op0=ALU.add, op1=ALU.mult, reads=[b_gs], writes=[b_gs])
                    t.op("tensor", nc.tensor.transpose, tps[0:32, 0:128], mb16[:], ident[:],
                         reads=[b_gs, b_const], writes=[b_tps])
                    t.op("vector", nc.vector.tensor_copy, maskT[:, qsl], tps[0:32, 0:128],
                         reads=[b_tps], writes=[b_maskT])
                if h + 1 < NH:
                    load_head(h + 1)
                jobs = []
                for qbl in range(DBG + 1 if DBG else 8):
                    qb = QB0 + qbl
                    for n in range(qb):
                        jobs.append((qbl, "past", n, n == 0, n == qb - 1))
                    for kt2 in range(2):
                        jobs.append((qbl, "diag", 2 * qb + kt2, kt2 == 0, kt2 == 1))

                def stage1(job):
                    nonlocal pc_
                    qbl, kind, idx, first, last = job
                    qb = QB0 + qbl
                    qsl = slice(qbl * 256, qbl * 256 + 256)
                    pi = pc_ % 2
                    pc_ += 1
                    bank = scpB2[pi]
                    if kind == "past":
                        n = idx
                        if first:
                            oi = qbl % 2
                            t.dma("sync", sza[oi][:], szd[0][h, :, qsl], reads=[b_szd[0]], writes=[b_sza[oi]])
                        for k2 in range(2):
                            ktile = 2 * n + k2
                            t.op("tensor", nc.tensor.matmul, bank[:, k2 * 256:(k2 + 1) * 256],
                                 lhsT=KT_[hi][:, ktile * 128:(ktile + 1) * 128], rhs=QT[hi][:, qsl],
                                 start=(k2 == 0), stop=False, reads=[b_KT[hi], b_QT[hi]], writes=[b_scp[pi]], inc=False)
                        t.op("tensor", nc.tensor.matmul, bank[:].rearrange("p (a b) -> p a b", a=2), lhsT=Sel[:, n, :],
                             rhs=maskT[:, qsl].unsqueeze(1).broadcast_to([32, 2, 256]),
                             start=False, stop=True, reads=[b_tab, b_maskT], writes=[b_scp[pi]])
                        for k2 in range(2):
                            m = (2 * qb) - (2 * n + k2)
                            t.op("scalar", nc.scalar.activation, out=pT[pi][:, k2 * 256:(k2 + 1) * 256],
                                 in_=bank[:, k2 * 256:(k2 + 1) * 256], func=AF.Exp,
                                 bias=kbias[:, h, m:m + 1], scale=1.0, reads=[b_scp[pi], b_tab], writes=[b_pT[pi]])
                    else:
                        ktile = idx
                        kt2 = ktile - 2 * qb
                        t.op("tensor", nc.tensor.matmul, bank[:, 0:256], lhsT=KT_[hi][:, ktile * 128:(ktile + 1) * 128],
                             rhs=QT[hi][:, qsl], start=True, stop=True,
                             reads=[b_KT[hi], b_QT[hi]], writes=[b_scp[pi]])
                        t.op("vector", nc.vector.scalar_tensor_tensor, out=s32[:], in0=Dtab[:, kt2, :],
                             scalar=-float(SLOPES[h]), in1=bank[:, 0:256], op0=ALU.mult, op1=ALU.add,
                             reads=[b_tab, b_scp[pi]], writes=[b_s32])
                        t.op("scalar", nc.scalar.activation, out=pT[pi][:, 0:256], in_=s32[:], func=AF.Exp,
                             reads=[b_s32], writes=[b_pT[pi]])
                    return pi

                def stage2(job, pi):
                    qbl, kind, idx, first, last = job
                    s_ = qbl % 2
                    qsl = slice(qbl * 256, qbl * 256 + 256)
                    if kind == "past":
                        n = idx
                        o_t, d_t, bo, bd = accB[s_][:, 0:256], dgB[s_][:, 0:256], b_oacc[s_], b_dacc[s_]
                        for k2 in range(2):
                            lastmm = last and k2 == 1
                            t.op("tensor", nc.tensor.matmul, o_t, lhsT=Vh[hi][:, 2 * n + k2, :],
                                 rhs=pT[pi][:, k2 * 256:(k2 + 1) * 256], start=(first and k2 == 0), stop=lastmm,
                                 reads=[b_Vh[hi], b_pT[pi]], writes=[bo], inc=lastmm)
                        dsl = slice(0, 512)
                        di = 0
                    else:
                        ktile = idx
                        o_t, d_t, bo, bd = accB[s_][:, 256:512], dgB[s_][:, 256:512], b_odg[s_], b_ddg[s_]
                        t.op("tensor", nc.tensor.matmul, o_t, lhsT=Vh[hi][:, ktile, :], rhs=pT[pi][:, 0:256],
                             start=first, stop=last, reads=[b_Vh[hi], b_pT[pi]], writes=[bo], inc=last)
                        dsl = slice(0, 256)
                        di = 1
                    if first:
                        t.op("vector", nc.vector.tensor_copy, dsum[s_][di][:, dsl], pT[pi][:, dsl],
                             reads=[b_pT[pi]], writes=[b_dsum[s_][di]])
                    else:
                        t.op("vector", nc.vector.tensor_tensor, out=dsum[s_][di][:, dsl], in0=dsum[s_][di][:, dsl],
                             in1=pT[pi][:, dsl], op=ALU.add, reads=[b_pT[pi], b_dsum[s_][di]], writes=[b_dsum[s_][di]])
                    if last:
                        nh = 2 if kind == "past" else 1
                        for k2 in range(nh):
                            t.op("tensor", nc.tensor.matmul, d_t, lhsT=onesf32[:], rhs=dsum[s_][di][:, k2 * 256:(k2 + 1) * 256],
                                 start=(k2 == 0), stop=(k2 == nh - 1), reads=[b_tab, b_dsum[s_][di]], writes=[bd],
                                 inc=(k2 == nh - 1))
                    if kind == "diag" and last:
                        oi = qbl % 2
                        oa, da, og, dg = accB[s_][:, 0:256], dgB[s_][:, 0:256], accB[s_][:, 256:512], dgB[s_][:, 256:512]
                        V_ = lambda *a, r=(), w=(), **k: t.op("vector", nc.vector.tensor_tensor, *a, reads=r, writes=w, **k)
                        V_(out=num[:], in0=oa, in1=Ftab[:, h, :], op=ALU.mult,
                           r=[b_oacc[s_], b_odg[s_], b_dacc[s_], b_ddg[s_], b_tab], w=[b_nd])
                        V_(out=num[:], in0=og, in1=num[:], op=ALU.add, r=[b_odg[s_], b_nd], w=[b_nd])
                        V_(out=den[:], in0=da, in1=Ftab[:, h, :], op=ALU.mult, r=[b_dacc[s_], b_tab], w=[b_nd])
                        V_(out=den[:], in0=dg, in1=den[:], op=ALU.add, r=[b_ddg[s_], b_nd], w=[b_nd])
                        t.op("vector", nc.vector.reciprocal, den[:], den[:], reads=[b_nd], writes=[b_nd])
                        V_(out=num[:], in0=num[:], in1=den[:], op=ALU.mult, r=[b_nd], w=[b_nd])
                        V_(out=yo[oi][:], in0=num[:], in1=sza[oi][:], op=ALU.mult, r=[b_nd, b_sza[oi]], w=[b_yo[oi]])
                        t.dma("sync", yTd[0][h, :, qsl], yo[oi][:], reads=[b_yo[oi]], writes=[b_yTd[0]])

                prev = None
                for job in jobs + [None]:
                    cur = None
                    if job is not None:
                        cur = (job, stage1(job))
                    if prev is not None:
                        stage2(*prev)
                    prev = cur
            t.barrier()

        if ENABLE_S:
          with ExitStack() as pS:
            TWO_PI = 2.0 * math.pi
            V = lambda fn, *a, r=(), w=(), **k: t.op("vector", fn, *a, reads=r, writes=w, **k)
            A_ = lambda *a, r=(), w=(), **k: t.op("scalar", nc.scalar.activation, *a, reads=r, writes=w, **k)
            tt_, ts_, stt_ = nc.vector.tensor_tensor, nc.vector.tensor_scalar, nc.vector.scalar_tensor_tensor

            def sincos(arg, cos_o, sin_o, tmp, tmpi, b):
                for dst, shift in ((sin_o, 0.0), (cos_o, math.pi / 2)):
                    V(ts_, out=tmp, in0=arg, scalar1=shift, scalar2=1.0 / TWO_PI, op0=ALU.add, op1=ALU.mult, r=[b], w=[b])
                    V(nc.vector.tensor_copy, tmpi, tmp, r=[b], w=[b])
                    V(nc.vector.tensor_copy, tmp, tmpi, r=[b], w=[b])
                    V(stt_, out=tmp, in0=tmp, scalar=-TWO_PI, in1=arg, op0=ALU.mult, op1=ALU.add, r=[b], w=[b])
                    if shift:
                        V(ts_, out=tmp, in0=tmp, scalar1=shift, scalar2=None, op0=ALU.add, r=[b], w=[b])
                    V(ts_, out=dst, in0=tmp, scalar1=math.pi, scalar2=TWO_PI, op0=ALU.is_gt, op1=ALU.mult, r=[b], w=[b])
                    V(tt_, out=tmp, in0=tmp, in1=dst, op=ALU.subtract, r=[b], w=[b])
                    V(ts_, out=dst, in0=tmp, scalar1=-math.pi, scalar2=TWO_PI, op0=ALU.is_lt, op1=ALU.mult, r=[b], w=[b])
                    V(tt_, out=tmp, in0=tmp, in1=dst, op=ALU.add, r=[b], w=[b])
                    V(ts_, out=tmp, in0=tmp, scalar1=-3.14159, scalar2=3.14159, op0=ALU.max, op1=ALU.min, r=[b], w=[b])
                    A_(out=dst, in_=tmp, func=AF.Sin, r=[b], w=[b])

            rcol = sb("srcol", [128, 64], F32, pS)
            thcol = sb("sthcol", [128, 64], F32, pS)
            dcol = sb("sdcol", [128, 16], F32, pS)
            bglu = sb("sbglu", [128, 16], F32, pS)
            pbc = pS.enter_context(ExitStack())
            BbT = [sb(f"sBbT{i}", [128, 64 * 128], BF16, pbc) for i in range(2)]
            Cb = [sb(f"sCb{i}", [128, 64 * 128], BF16, pbc) for i in range(2)]
            b_par = Buf("spar")
            with ExitStack() as pp:
                CW = 2048
                nm = ["A", "I", "L", "T1", "T2", "T3", "T4", "T5", "T6", "Br", "Bi"]
                T = {n: sb("sp" + n, [128, CW], F32, pp) for n in nm}
                Ti = sb("spTi", [128, CW], mybir.dt.int32, pp)
                for c in range(8192 // CW):
                    cs = slice(c * CW, (c + 1) * CW)
                    for n, src in (("A", s_arep), ("I", s_irep), ("L", s_lrep), ("Br", s_bre), ("Bi", s_bim)):
                        t.dma("sync", T[n][:], src[:, cs], writes=[b_par])
                    g = lambda n: T[n][:]
                    A_(out=g("L"), in_=g("L"), func=AF.Exp, r=[b_par], w=[b_par])
                    V(tt_, out=g("T1"), in0=g("L"), in1=g("A"), op=ALU.mult, r=[b_par], w=[b_par])
                    A_(out=g("T1"), in_=g("T1"), func=AF.Exp, r=[b_par], w=[b_par])
                    V(tt_, out=g("T2"), in0=g("L"), in1=g("I"), op=ALU.mult, r=[b_par], w=[b_par])
                    sincos(g("T2"), g("T3"), g("T4"), g("T5"), Ti[:], b_par)
                    V(tt_, out=g("T3"), in0=g("T1"), in1=g("T3"), op=ALU.mult, r=[b_par], w=[b_par])
                    V(tt_, out=g("T4"), in0=g("T1"), in1=g("T4"), op=ALU.mult, r=[b_par], w=[b_par])
                    V(tt_, out=g("T5"), in0=g("A"), in1=g("A"), op=ALU.mult, r=[b_par], w=[b_par])
                    V(tt_, out=g("T6"), in0=g("I"), in1=g("I"), op=ALU.mult, r=[b_par], w=[b_par])
                    V(tt_, out=g("T5"), in0=g("T5"), in1=g("T6"), op=ALU.add, r=[b_par], w=[b_par])
                    V(nc.vector.reciprocal, g("T5"), g("T5"), r=[b_par], w=[b_par])
                    V(ts_, out=g("T3"), in0=g("T3"), scalar1=-1.0, scalar2=None, op0=ALU.add, r=[b_par], w=[b_par])
                    V(tt_, out=g("T6"), in0=g("T3"), in1=g("A"), op=ALU.mult, r=[b_par], w=[b_par])
                    V(tt_, out=g("T2"), in0=g("T4"), in1=g("I"), op=ALU.mult, r=[b_par], w=[b_par])
                    V(tt_, out=g("T6"), in0=g("T6"), in1=g("T2"), op=ALU.add, r=[b_par], w=[b_par])
                    V(tt_, out=g("T6"), in0=g("T6"), in1=g("T5"), op=ALU.mult, r=[b_par], w=[b_par])
                    V(tt_, out=g("T2"), in0=g("T4"), in1=g("A"), op=ALU.mult, r=[b_par], w=[b_par])
                    V(tt_, out=g("T1"), in0=g("T3"), in1=g("I"), op=ALU.mult, r=[b_par], w=[b_par])
                    V(tt_, out=g("T2"), in0=g("T2"), in1=g("T1"), op=ALU.subtract, r=[b_par], w=[b_par])
                    V(tt_, out=g("T2"), in0=g("T2"), in1=g("T5"), op=ALU.mult, r=[b_par], w=[b_par])
                    V(tt_, out=g("T1"), in0=g("Br"), in1=g("T6"), op=ALU.mult, r=[b_par], w=[b_par])
                    V(tt_, out=g("T3"), in0=g("Bi"), in1=g("T2"), op=ALU.mult, r=[b_par], w=[b_par])
                    V(tt_, out=BbT[0][:, cs], in0=g("T1"), in1=g("T3"), op=ALU.subtract, r=[b_par], w=[b_par])
                    V(tt_, out=g("T1"), in0=g("Br"), in1=g("T2"), op=ALU.mult, r=[b_par], w=[b_par])
                    V(tt_, out=g("T3"), in0=g("Bi"), in1=g("T6"), op=ALU.mult, r=[b_par], w=[b_par])
                    V(tt_, out=BbT[1][:, cs], in0=g("T1"), in1=g("T3"), op=ALU.add, r=[b_par], w=[b_par])
                    t.dma("sync", T["A"][:], s_cre[:, cs], writes=[b_par])
                    t.dma("sync", T["I"][:], s_cim[:, cs], writes=[b_par])
                    V(nc.vector.tensor_copy, Cb[0][:, cs], g("A"), r=[b_par], w=[b_par])
                    V(ts_, out=Cb[1][:, cs], in0=g("I"), scalar1=-1.0, scalar2=None, op0=ALU.mult, r=[b_par], w=[b_par])
                t.dma("sync", T["A"][:, 0:64], s_acol[:, :], writes=[b_par])
                t.dma("sync", T["I"][:, 0:64], s_icol[:, :], writes=[b_par])
                t.dma("sync", T["L"][:, 0:64], s_lcol[:, :], writes=[b_par])
                t.dma("sync", dcol[:], s_dcol[:, :], writes=[b_par])
                t.dma("sync", bglu[:], s_bglu[:, :], writes=[b_par])
                A_(out=T["L"][:, 0:64], in_=T["L"][:, 0:64], func=AF.Exp, r=[b_par], w=[b_par])
                V(tt_, out=T["T1"][:, 0:64], in0=T["L"][:, 0:64], in1=T["A"][:, 0:64], op=ALU.mult, r=[b_par], w=[b_par])
                A_(out=rcol[:], in_=T["T1"][:, 0:64], func=AF.Exp, r=[b_par], w=[b_par])
                V(tt_, out=thcol[:], in0=T["L"][:, 0:64], in1=T["I"][:, 0:64], op=ALU.mult, r=[b_par], w=[b_par])
                t.barrier()

            b_y1T = Buf("y1T")
            b_y1d = Buf("y1d")
            b_y1bd = Buf("y1bd")
            with ExitStack() as pm:
                SW = 512
                NSEG = CTX // SW
                SEG0 = OFF // SW
                UT = [sb(f"sUT{i}", [128, CTX], BF16, pm) for i in range(2)]
                b_UT = [Buf(), Buf()]
                iot = sb("siot", [128, SW + 1], mybir.dt.int32, pm)
                tauf = sb("stauf", [128, SW + 1], F32, pm)
                arg = sb("sarg", [128, SW + 1], F32, pm)
                tmp = sb("stmp", [128, SW + 1], F32, pm)
                tmpi = sb("stmpi", [128, SW + 1], mybir.dt.int32, pm)
                cosT = sb("scosT", [128, SW + 1], F32, pm)
                sinT = sb("ssinT", [128, SW + 1], F32, pm)
                Rb = sb("sRb", [128, SW], F32, pm)
                onesf = sb("sonesf", [128, SW], F32, pm)
                nEs = sb("snEs", [128, 1], F32, pm)
                b_tb = Buf("stab")
                t1 = [sb(f"st1_{i}", [128, SW], F32, pm) for i in range(2)]
                t2 = [sb(f"st2_{i}", [128, SW], F32, pm) for i in range(2)]
                bre = [sb(f"sbre{i}", [128, SW], F32, pm) for i in range(2)]
                bim = [sb(f"sbim{i}", [128, SW], F32, pm) for i in range(2)]
                wre = [sb(f"swre{i}", [128, SW], F32, pm) for i in range(2)]
                wim = [sb(f"swim{i}", [128, SW], F32, pm) for i in range(2)]
                ini = [sb(f"sini{i}", [128, 4], F32, pm) for i in range(2)]
                xre = [sb(f"sxre{i}", [128, SW], BF16, pm) for i in range(2)]
                xim = [sb(f"sxim{i}", [128, SW], BF16, pm) for i in range(2)]
                b_seg = [Buf(), Buf()]
                b_seg2 = [Buf(), Buf()]
                b_bre = [Buf(), Buf()]
                b_bim = [Buf(), Buf()]
                t3 = [sb(f"st3_{i}", [128, SW], F32, pm) for i in range(2)]
                t4 = [sb(f"st4_{i}", [128, SW], F32, pm) for i in range(2)]
                t5 = sb("st5", [128, SW], F32, pm)
                t6 = sb("st6", [128, SW], F32, pm)
                b_t56 = Buf()
                if S_USE_POOL:
                    P = lambda r=(), w=(), **k: t.op("gpsimd", nc.gpsimd.tensor_tensor, reads=r, writes=w, **k)
                else:
                    P = lambda r=(), w=(), **k: t.op("vector", nc.vector.tensor_tensor, reads=r, writes=w, **k)
                b_w_ = [Buf(), Buf()]
                b_ini = [Buf(), Buf()]
                b_x = [Buf(), Buf()]
                bups = [[ps(f"sbups{i}{j}", [128, 512], F32, pm) for j in range(2)] for i in range(2)]
                b_bups = [Buf(), Buf()]
                yacc = [ps(f"syacc{i}", [128, 512], F32, pm) for i in range(4)]
                b_yacc = [Buf() for _ in range(4)]
                y32 = sb("sy32", [128, SW], F32, pm)
                g1 = sb("sg1", [128, SW], F32, pm)
                y16 = sb("sy16", [128, SW], BF16, pm)
                b_y32 = Buf()
                b_y16 = Buf()
                t.op("gpsimd", nc.gpsimd.iota, iot[:], pattern=[[1, SW + 1]], base=0, channel_multiplier=0, writes=[b_tb])
                V(nc.vector.tensor_copy, tauf[:], iot[:], r=[b_tb], w=[b_tb])
                V(nc.vector.memset, onesf[:], 1.0, r=[b_tb], w=[b_tb])
                sc = 0
                def load_UT(c8):
                    u_ = c8 % 2
                    t.dma("sync", UT[u_][:, 0:CTX // 2], uTd[c8, :, 0:CTX // 2], reads=[b_uTd], writes=[b_UT[u_]])
                    t.dma("sync", UT[u_][:, CTX // 2:CTX], uTd[c8, :, CTX // 2:CTX], reads=[b_uTd], writes=[b_UT[u_]])
                load_UT(0)
                NC8 = DBG if DBG else 16
                for ct8 in range(NC8):
                    ui = ct8 % 2
                    if ct8 + 1 < NC8:
                        load_UT(ct8 + 1)
                    for p4 in range(4):
                        pr = ct8 * 4 + p4
                        psl = slice(pr * 128, (pr + 1) * 128)
                        V(ts_, out=arg[:], in0=tauf[:], scalar1=thcol[:, pr:pr + 1], scalar2=None, op0=ALU.mult,
                          r=[b_tb, b_par], w=[b_tb])
                        sincos(arg[:], cosT[:], sinT[:], tmp[:], tmpi[:], b_tb)
                        V(ts_, out=Rb[:], in0=onesf[:], scalar1=rcol[:, pr:pr + 1], scalar2=None, op0=ALU.mult,
                          r=[b_tb, b_par], w=[b_tb])
                        V(ts_, out=nEs[:], in0=sinT[:, SW:SW + 1], scalar1=-1.0, scalar2=None, op0=ALU.mult,
                          r=[b_tb], w=[b_tb])
                        for seg in range(NSEG):
                            i = sc % 2
                            sc += 1
                            tsl = slice(seg * SW, (seg + 1) * SW)
                            for ri in range(2):
                                t.op("tensor", nc.tensor.matmul, bups[i][ri][:], lhsT=BbT[ri][:, psl], rhs=UT[ui][:, tsl],
                                     start=True, stop=True, reads=[b_par, b_UT[ui]], writes=[b_bups[i]], inc=(ri == 1))
                            c_, s_ = cosT[:, 0:SW], sinT[:, 0:SW]
                            V(tt_, out=t1[i][:], in0=bups[i][0][:], in1=c_, op=ALU.mult, r=[b_bups[i], b_tb], w=[b_seg[i]])
                            V(tt_, out=t2[i][:], in0=bups[i][1][:], in1=s_, op=ALU.mult, r=[b_bups[i], b_tb], w=[b_seg[i]])
                            P(out=bre[i][:], in0=t1[i][:], in1=t2[i][:], op=ALU.add, r=[b_seg[i]], w=[b_bre[i]])
                            V(tt_, out=t3[i][:], in0=bups[i][1][:], in1=c_, op=ALU.mult, r=[b_bups[i], b_tb], w=[b_seg2[i]])
                            V(tt_, out=t4[i][:], in0=bups[i][0][:], in1=s_, op=ALU.mult, r=[b_bups[i], b_tb], w=[b_seg2[i]])
                            P(out=bim[i][:], in0=t3[i][:], in1=t4[i][:], op=ALU.subtract, r=[b_seg2[i]], w=[b_bim[i]])
                            if seg == 0:
                                i_re, i_im = 0.0, 0.0
                                rd = [b_tb]
                            else:
                                i_re, i_im = ini[i][:, 0:1], ini[i][:, 1:2]
                                rd = [b_tb, b_ini[i]]
                            V(nc.vector.tensor_tensor_scan, out=wre[i][:], data0=Rb[:], data1=bre[i][:], initial=i_re,
                              op0=ALU.mult, op1=ALU.add, r=rd + [b_bre[i]], w=[b_w_[i]])
                            V(nc.vector.tensor_tensor_scan, out=wim[i][:], data0=Rb[:], data1=bim[i][:], initial=i_im,
                              op0=ALU.mult, op1=ALU.add, r=rd + [b_bim[i]], w=[b_w_[i]])
                            if seg < NSEG - 1:
                                j = 1 - i
                                Ec, Es = cosT[:, SW:SW + 1], sinT[:, SW:SW + 1]
                                lr, li = wre[i][:, SW - 1:SW], wim[i][:, SW - 1:SW]
                                A_(out=ini[j][:, 2:3], in_=li, func=AF.Identity, scale=nEs[:, 0:1],
                                   r=[b_w_[i], b_tb], w=[b_ini[j]])
                                A_(out=ini[j][:, 0:1], in_=lr, func=AF.Identity, scale=Ec, bias=ini[j][:, 2:3],
                                   r=[b_w_[i], b_tb, b_ini[j]], w=[b_ini[j]])
                                A_(out=ini[j][:, 3:4], in_=li, func=AF.Identity, scale=Ec,
                                   r=[b_w_[i], b_tb, b_ini[j]], w=[b_ini[j]])
                                A_(out=ini[j][:, 1:2], in_=lr, func=AF.Identity, scale=Es, bias=ini[j][:, 3:4],
                                   r=[b_w_[i], b_tb, b_ini[j]], w=[b_ini[j]])
                            if seg >= SEG0:
                                so = seg - SEG0
                                P(out=t5[:], in0=wre[i][:], in1=c_, op=ALU.mult, r=[b_w_[i], b_tb], w=[b_t56])
                                P(out=t6[:], in0=wim[i][:], in1=s_, op=ALU.mult, r=[b_w_[i], b_tb], w=[b_t56])
                                P(out=xre[i][:], in0=t5[:], in1=t6[:], op=ALU.subtract, r=[b_t56], w=[b_x[i]])
                                P(out=t5[:], in0=wre[i][:], in1=s_, op=ALU.mult, r=[b_w_[i], b_tb], w=[b_t56])
                                P(out=t6[:], in0=wim[i][:], in1=c_, op=ALU.mult, r=[b_w_[i], b_tb], w=[b_t56])
                                P(out=xim[i][:], in0=t5[:], in1=t6[:], op=ALU.add, r=[b_t56], w=[b_x[i]])
                                t.op("tensor", nc.tensor.matmul, yacc[so][:], lhsT=Cb[0][:, psl], rhs=xre[i][:],
                                     start=(p4 == 0), stop=False, reads=[b_par, b_x[i]], writes=[b_yacc[so]], inc=False)
                                t.op("tensor", nc.tensor.matmul, yacc[so][:], lhsT=Cb[1][:, psl], rhs=xim[i][:],
                                     start=False, stop=(p4 == 3), reads=[b_par, b_x[i]], writes=[b_yacc[so]], inc=True)
                    for so in range(NSEG - SEG0):
                        tsl = slice(OFF + so * SW, OFF + (so + 1) * SW)
                        osl = slice(so * SW, (so + 1) * SW)
                        V(stt_, out=y32[:], in0=UT[ui][:, tsl], scalar=dcol[:, ct8:ct8 + 1], in1=yacc[so][:],
                          op0=ALU.mult, op1=ALU.add, r=[b_UT[ui], b_par, b_yacc[so]], w=[b_y32])
                        V(tt_, out=g1[:], in0=y32[:], in1=y32[:], op=ALU.mult, r=[b_y32], w=[b_y32])
                        V(ts_, out=g1[:], in0=g1[:], scalar1=0.044715, scalar2=1.0, op0=ALU.mult, op1=ALU.add, r=[b_y32], w=[b_y32])
                        V(tt_, out=g1[:], in0=g1[:], in1=y32[:], op=ALU.mult, r=[b_y32], w=[b_y32])
                        A_(out=g1[:], in_=g1[:], func=AF.Sigmoid, scale=1.5957691216057308, r=[b_y32], w=[b_y32])
                        V(tt_, out=y32[:], in0=y32[:], in1=g1[:], op=ALU.mult, r=[b_y32], w=[b_y32])
                        V(nc.vector.tensor_copy, y16[:], y32[:], r=[b_y32], w=[b_y16])
                        t.dma("sync", y1bd[ct8, :, osl], y16[:], reads=[b_y16], writes=[b_y1bd])
                        t.dma("sync", y1d[ct8, :, osl], y32[:], reads=[b_y32], writes=[b_y1d])
                t.barrier()
            pbc.close()
            with ExitStack() as pg:
                mk_w(pg)
                y1T = sb("sy1T", [128, 16, OWN], BF16, pg)
                for q4 in range(4):
                    t.dma("sync", y1T[:, q4 * 4:(q4 + 1) * 4, :], y1bd[q4 * 4:(q4 + 1) * 4, :, :].rearrange("c p t -> p c t"),
                          reads=[b_y1bd], writes=[b_y1T])
                gacc = [ps(f"sgacc{i}", [128, 512], F32, pg) for i in range(2)]
                b_gacc = [Buf(), Buf()]
                sgl = [sb(f"ssgl{i}", [128, 512], F32, pg) for i in range(2)]
                y1f = [sb(f"sy1f{i}", [128, 512], F32, pg) for i in range(2)]
                szs = [sb(f"sszs{i}", [128, 512], F32, pg) for i in range(2)]
                yo = [sb(f"ssyo{i}", [128, 512], BF16, pg) for i in range(2)]
                b_sgl, b_y1f, b_szs, b_yo = [Buf(), Buf()], [Buf(), Buf()], [Buf(), Buf()], [Buf(), Buf()]
                gc = 0
                for cb in range(1 if DBG else 4):
                    wt, bw = load_w(w_glu, 16, cb * 512)
                    for j in range(1 if DBG else 4):
                        f = cb * 4 + j
                        for tt in range(1 if DBG else NTT):
                            i = gc % 2
                            gc += 1
                            tsl = slice(tt * TT, (tt + 1) * TT)
                            t.dma("sync", y1f[i][:], y1d[f, :, tsl], reads=[b_y1d], writes=[b_y1f[i]])
                            t.dma("sync", szs[i][:], szd[1][f, :, tsl], reads=[b_szd[1]], writes=[b_szs[i]])
                            for ct in range(16):
                                t.op("tensor", nc.tensor.matmul, gacc[i][:], lhsT=wt[:, ct, j * 128:(j + 1) * 128],
                                     rhs=y1T[:, ct, tsl], start=(ct == 0), stop=(ct == 15),
                                     reads=[bw, b_y1T], writes=[b_gacc[i]], inc=(ct == 15))
                            A_(out=sgl[i][:], in_=gacc[i][:], func=AF.Sigmoid, bias=bglu[:, f:f + 1], scale=1.0,
                               r=[b_gacc[i], b_par], w=[b_sgl[i]])
                            V(tt_, out=sgl[i][:], in0=sgl[i][:], in1=y1f[i][:], op=ALU.mult, r=[b_sgl[i], b_y1f[i]], w=[b_sgl[i]])
                            V(tt_, out=yo[i][:], in0=sgl[i][:], in1=szs[i][:], op=ALU.mult, r=[b_sgl[i], b_szs[i]], w=[b_yo[i]])
                            t.dma("sync", yTd[1][f, :, tsl], yo[i][:], reads=[b_yo[i]], writes=[b_yTd[1]])
                t.barrier()

        with ExitStack() as pc2:
            qc = [sb(f"c2qc{i}", [128, 4, TT], BF16, pc2) for i in range(2)]
            szt = [sb(f"c2sz{i}", [128, 4, TT], F32, pc2) for i in range(2)]
            b_qc = [Buf(), Buf()]
            b_szt = [Buf(), Buf()]
            scp = [ps(f"c2scp{i}", [128, 512], F32, pc2) for i in range(2)]
            denp = ps("c2denp", [128, 512], F32, pc2)
            op_ = [ps(f"c2op{i}", [128, 512], F32, pc2) for i in range(4)]
            b_scp = [Buf(), Buf()]
            b_denp = Buf()
            b_op = [Buf() for _ in range(4)]
            pT = sb("c2pT", [128, 2, TT], BF16, pc2)
            rden = sb("c2rden", [128, TT], F32, pc2)
            otmp = sb("c2otmp", [128, TT], F32, pc2)
            yo = [sb(f"c2yo{i}", [128, 4, TT], BF16, pc2) for i in range(2)]
            b_pT, b_rden, b_otmp = Buf(), Buf(), Buf()
            b_yo = [Buf(), Buf()]
            it = 0
            for tt in range(1 if DBG else NTT):
                for hd in range(1 if DBG else 4):
                    i = it % 2
                    it += 1
                    tsl = slice(tt * TT, (tt + 1) * TT)
                    t.dma("sync", qc[i][:], qcTd[hd * 4:(hd + 1) * 4, :, tsl].rearrange("j p t -> p j t"),
                          reads=[b_qcTd], writes=[b_qc[i]])
                    t.dma("sync", szt[i][:], szd[2][hd * 4:(hd + 1) * 4, :, tsl].rearrange("j p t -> p j t"),
                          reads=[b_szd[2]], writes=[b_szt[i]])
                    for mt in range(2):
                        for j in range(4):
                            t.op("tensor", nc.tensor.matmul, scp[mt][:],
                                 lhsT=KcT[:, hd * 4 + j, mt * 128:(mt + 1) * 128], rhs=qc[i][:, j, :],
                                 start=(j == 0), stop=(j == 3), reads=[b_KcT, b_qc[i]], writes=[b_scp[mt]],
                                 inc=(j == 3))
                        t.op("scalar", nc.scalar.activation, out=pT[:, mt, :], in_=scp[mt][:], func=AF.Exp,
                             reads=[b_scp[mt]], writes=[b_pT])
                    for mt in range(2):
                        t.op("tensor", nc.tensor.matmul, denp[:], lhsT=onesb[:], rhs=pT[:, mt, :],
                             start=(mt == 0), stop=(mt == 1), reads=[b_pT, b_const], writes=[b_denp], inc=(mt == 1))
                    t.op("vector", nc.vector.reciprocal, rden[:], denp[:], reads=[b_denp], writes=[b_rden])
                    for j in range(4):
                        for mt in range(2):
                            t.op("tensor", nc.tensor.matmul, op_[j][:],
                                 lhsT=Vc[:, mt, hd * 512 + j * 128: hd * 512 + (j + 1) * 128], rhs=pT[:, mt, :],
                                 start=(mt == 0), stop=(mt == 1), reads=[b_Vc, b_pT], writes=[b_op[j]], inc=(mt == 1))
                        t.op("vector", nc.vector.tensor_tensor, out=otmp[:], in0=op_[j][:], in1=rden[:], op=ALU.mult,
                             reads=[b_op[j], b_rden], writes=[b_otmp])
                        t.op("vector", nc.vector.tensor_tensor, out=yo[i][:, j, :], in0=otmp[:], in1=szt[i][:, j, :],
                             op=ALU.mult, reads=[b_otmp, b_szt[i]], writes=[b_yo[i]])
                    t.dma("sync", yTd[2][hd * 4:(hd + 1) * 4, :, tsl].rearrange("j p t -> p j t"), yo[i][:],
                          reads=[b_yo[i]], writes=[b_yTd[2]])
            t.barrier()

        b_out = Buf("out")
        with ExitStack() as pf:
            mk_w(pf)
            yT = [sb(f"pfyT{i}", [128, 16, FT], BF16, pf) for i in range(1)]
            b_yT = [Buf()]
            sg = [sb(f"pfsg{i}", [128, 4, FT], F32, pf) for i in range(1)]
            b_sg = [Buf()]
            mrg32 = sb("pfmrg32", [128, 32, FT], F32, pf) if (ENABLE_A or ENABLE_S) else None
            mrgT = sb("pfmrgT", [128, 32, FT], BF16, pf)
            b_m32 = [Buf() for _ in range(32)]
            b_mT = Buf()
            acc = [ps(f"pfacc{i}", [128, 512], F32, pf) for i in range(4)]
            b_acc = [Buf() for _ in range(4)]
            oo = [sb(f"pfoo{i}", [128, 512], F32, pf) for i in range(2)]
            b_oo = [Buf(), Buf()]
            brs = [b for b in range(3) if (b == 0 and ENABLE_A) or (b == 1 and ENABLE_S) or b == 2]
            ctr = 0
            yc = 0
            for tt in range(1 if DBG else OWN // FT):
                tsl = slice(tt * FT, (tt + 1) * FT)
                for bi, b in enumerate(brs):
                    yi = 0
                    yc += 1
                    t.dma("sync", yT[yi][:], yTd[b][:, :, tsl].rearrange("c p t -> p c t"),
                          reads=[b_yTd[b]], writes=[b_yT[yi]])
                    for fb in range(8):
                        wt, bw = load_w(w_br[b], 16, fb * 512)
                        si = 0
                        t.dma("sync", sg[si][:], sgd[b][fb * 4:(fb + 1) * 4, :, tsl].rearrange("j p t -> p j t"),
                              reads=[b_sgd[b]], writes=[b_sg[si]])
                        for j in range(4):
                            a = ctr % 4
                            ctr += 1
                            f = fb * 4 + j
                            for ct in range(16):
                                t.op("tensor", nc.tensor.matmul, acc[a][:, 0:FT], lhsT=wt[:, ct, j * 128:(j + 1) * 128],
                                     rhs=yT[yi][:, ct, :], start=(ct == 0), stop=(ct == 15),
                                     reads=[bw, b_yT[yi]], writes=[b_acc[a]], inc=(ct == 15))
                            last = (bi == len(brs) - 1)
                            if bi == 0:
                                t.op("vector", nc.vector.tensor_tensor,
                                     out=(mrgT[:, f, :] if last else mrg32[:, f, :]),
                                     in0=acc[a][:, 0:FT], in1=sg[si][:, j, :], op=ALU.mult,
                                     reads=[b_acc[a], b_sg[si]], writes=[b_mT if last else b_m32[f]])
                            else:
                                t.op("vector", nc.vector.tensor_tensor, out=sg[si][:, j, :], in0=acc[a][:, 0:FT],
                                     in1=sg[si][:, j, :], op=ALU.mult,
                                     reads=[b_acc[a], b_sg[si]], writes=[b_sg[si]])
                                t.op("vector", nc.vector.tensor_tensor,
                                     out=(mrgT[:, f, :] if last else mrg32[:, f, :]),
                                     in0=sg[si][:, j, :], in1=mrg32[:, f, :], op=ALU.add,
                                     reads=[b_sg[si], b_m32[f]], writes=[b_mT if last else b_m32[f]])
                for cb in range(1 if DBG else 8):
                    wt, bw = load_w(w_out, 32, cb * 512)
                    for s in range(FT // 128):
                        a = ctr % 4
                        o = ctr % 2
                        ctr += 1
                        r0 = tt * FT + s * 128
                        t.dma("sync", oo[o][:], x[r0:r0 + 128, cb * 512:(cb + 1) * 512], writes=[b_oo[o]])
                        for ft in range(32):
                            t.op("tensor", nc.tensor.matmul, acc[a][:], lhsT=mrgT[:, ft, s * 128:(s + 1) * 128],
                                 rhs=wt[:, ft, :], start=(ft == 0), stop=(ft == 31),
                                 reads=[bw, b_mT], writes=[b_acc[a]], inc=(ft == 31))
                        t.op("vector", nc.vector.tensor_tensor, out=oo[o][:], in0=acc[a][:], in1=oo[o][:], op=ALU.add,
                             reads=[b_acc[a], b_oo[o]], writes=[b_oo[o]])
                        t.dma("sync", out[r0:r0 + 128, cb * 512:(cb + 1) * 512], oo[o][:],
                              reads=[b_oo[o]], writes=[b_out])
            t.wait_all("sync", [b_out])
            t.barrier()

        block = st.enter_context(nc.Block())
        t.replay(block)
    return nc


_NC = None


def _in_maps(inputs):
    f32 = lambda a: np.ascontiguousarray(np.asarray(a, dtype=np.float32))
    x = f32(inputs["x"])
    mem = f32(inputs["mem"])
    B, S, _ = x.shape
    xs = x.reshape(B * S // OWN, OWN, D)
    shared = {
        "w_in": f32(inputs["w_in"]),
        "w_mem_kv": f32(inputs["w_mem_kv"]),
        "w_br_a": f32(inputs["w_br_a"]),
        "w_br_s": f32(inputs["w_br_s"]),
        "w_br_c": f32(inputs["w_br_c"]),
        "w_out": f32(inputs["w_out"]),
        "gnB": f32(np.broadcast_to(f32(inputs["g_norm"])[None, :], (128, D))),
        "gmB": f32(np.broadcast_to(f32(inputs["g_mem"])[None, :], (128, D))),
        "qgc": f32(f32(inputs["q_gain_c"]).reshape(4, 128).T),
        "kgc": f32(f32(inputs["k_gain_c"]).reshape(4, 128).T),
    }
    shared["qga"] = f32(f32(inputs["q_gain_a"]).reshape(128, 1))
    shared["kga"] = f32(f32(inputs["k_gain_a"]).reshape(128, 1))

    a_re = f32(inputs["ssm_a_re"]); a_im = f32(inputs["ssm_a_im"]); ldt = f32(inputs["ssm_log_dt"])
    row = lambda v: f32(np.broadcast_to(v.reshape(1, 8192), (128, 8192)))
    shared["s_arep"] = row(a_re); shared["s_irep"] = row(a_im)
    shared["s_lrep"] = row(np.repeat(ldt, 64))
    col = lambda v: f32(v.reshape(64, 128).T)
    shared["s_acol"] = col(a_re); shared["s_icol"] = col(a_im); shared["s_lcol"] = col(np.repeat(ldt, 64))
    b_re = f32(inputs["ssm_b_re"]); b_im = f32(inputs["ssm_b_im"])
    c_re = f32(inputs["ssm_c_re"]); c_im = f32(inputs["ssm_c_im"])
    def arrB(b):
        o = np.zeros((128, 64, 128), np.float32)
        for pr in range(64):
            for gi in range(2):
                g = 2 * pr + gi
                k0 = (g % 8) * 16
                o[k0:k0 + 16, pr, gi * 64:(gi + 1) * 64] = b[g].T
        return f32(o.reshape(128, 8192))
    def arrC(cm):
        o = np.zeros((128, 64, 128), np.float32)
        for pr in range(64):
            for gi in range(2):
                g = 2 * pr + gi
                k0 = (g % 8) * 16
                o[gi * 64:(gi + 1) * 64, pr, k0:k0 + 16] = cm[g].T
        return f32(o.reshape(128, 8192))
    shared["s_bre"] = arrB(b_re); shared["s_bim"] = arrB(b_im)
    shared["s_cre"] = arrC(c_re); shared["s_cim"] = arrC(c_im)
    shared["s_dcol"] = f32(f32(inputs["ssm_d"]).reshape(16, 128).T)
    shared["s_bglu"] = f32(f32(inputs["b_glu"]).reshape(16, 128).T)
    shared["w_glu"] = f32(inputs["w_glu"])
    in_maps = []
    nper = S // OWN
    for c in range(8):
        b, pos = divmod(c, nper)
        m = dict(shared)
        m["x"] = np.ascontiguousarray(xs[c])
        m["mem"] = np.ascontiguousarray(mem[b])
        xp = np.zeros((OFF, D), np.float32)
        if pos > 0:
            xp[OFF - pos * OWN:] = x[b, :pos * OWN]
        m["xprev"] = xp
        vb = np.zeros((128, 32), np.float32)
        vb[:, :(OFF - pos * OWN) // 256] = -1.0e30
        m["vbB"] = vb
        in_maps.append(m)
    return in_maps, (B, S)


def kernel(**inputs):
    global _NC
    in_maps, (B, S) = _in_maps(inputs)
    if _NC is None:
        _NC = build_nc()
    res = run_bass_kernel_spmd(_NC, in_maps, core_ids=list(range(8)))
    o = np.stack([np.asarray(r["out"], dtype=np.float32) for r in res.results], axis=0)
    return o.reshape(B, S, D)
```
